# Optimizing a Trainium2 kernel written in Bass

```python
import jax
import jax.numpy as jnp
from jax import lax
import numpy as np

D_MODEL = 1024
BATCH = 2
SEQ = 16384
DEPTH = 1

GRID_W = 64
CTX_LEN = 256
CHUNK = 64
GLA_HEADS = 4
GLA_DK = 64
GLA_DV = 128
GLA_GATE_RANK = 16
GLA_GATE_TAU = 16.0
RET_HEADS = 4
RET_DK = 128
RET_DV = 128
RET_GAMMA_EXP0 = 5
ROPE_BASE = 10000.0
N_EXPERTS = 16
EC_CAPACITY_FACTOR = 2
EXPERT_FF = 1408
N_ADA = 6
EPS = 1e-6

GLA_QK = GLA_HEADS * GLA_DK
GLA_V = GLA_HEADS * GLA_DV
RET_QK = RET_HEADS * RET_DK
RET_V = RET_HEADS * RET_DV
MIX_WIDTH = GLA_V + RET_V
IN_WIDTHS = (GLA_QK, GLA_QK, GLA_V, 2 * GLA_GATE_RANK, GLA_V, RET_QK, RET_QK, RET_V, RET_V)
IN_WIDTH = sum(IN_WIDTHS)
IN_SPLIT_POINTS = tuple(int(p) for p in np.cumsum(IN_WIDTHS)[:-1])

kernel_name = 'hybrid_gla_retention_ec_moe_dit_block'

F32 = jnp.float32


def _rmsnorm(x, w):
    xf = x.astype(F32)
    y = xf * lax.rsqrt(jnp.mean(xf * xf, axis=-1, keepdims=True) + EPS)
    return (y * w).astype(x.dtype)


def _head_rmsnorm(o, w):
    B, T, H, d = o.shape
    of = o.astype(F32)
    y = of * lax.rsqrt(jnp.mean(of * of, axis=-1, keepdims=True) + EPS)
    return y.reshape(B, T, H * d) * w


def _head_layernorm(o, w):
    B, T, H, d = o.shape
    of = o.astype(F32)
    mu = jnp.mean(of, axis=-1, keepdims=True)
    var = jnp.mean(jnp.square(of - mu), axis=-1, keepdims=True)
    return ((of - mu) * lax.rsqrt(var + EPS)).reshape(B, T, H * d) * w


def _flip(a):
    return jnp.flip(a, axis=1)


def _to_chunks(a):
    B, T, H, d = a.shape
    return a.reshape(B, T // CHUNK, CHUNK, H, d).transpose(1, 0, 3, 2, 4)


def _from_chunks(a):
    N, B, H, C, d = a.shape
    return a.transpose(1, 0, 3, 2, 4).reshape(B, N * C, H, d)


def _grid_rope_angles(n_tok):
    rows = n_tok // GRID_W
    row = jnp.broadcast_to(jnp.arange(rows)[:, None], (rows, GRID_W)).reshape(-1).astype(F32)
    col = jnp.broadcast_to(jnp.arange(GRID_W)[None, :], (rows, GRID_W)).reshape(-1).astype(F32)
    n_freq = RET_DK // 4
    inv = ROPE_BASE ** (-jnp.arange(n_freq, dtype=F32) / n_freq)
    return jnp.concatenate([row[:, None] * inv, col[:, None] * inv], axis=-1)


def _apply_rope(a, ang):
    half = a.shape[-1] // 2
    cos = jnp.cos(ang)[None, :, None, :]
    sin = jnp.sin(ang)[None, :, None, :]
    a1, a2 = a[..., :half], a[..., half:]
    return jnp.concatenate([a1 * cos - a2 * sin, a1 * sin + a2 * cos], axis=-1)


def _gla_chunked(q, k, v, log_a, s0):
    mask = jnp.tril(jnp.ones((CHUNK, CHUNK), dtype=bool))[:, :, None]

    def step(s, xs):
        qc, kc, vc, gc = xs
        b = jnp.cumsum(gc, axis=2)
        b_end = b[:, :, -1:, :]
        decay = jnp.exp(jnp.where(mask, b[:, :, :, None, :] - b[:, :, None, :, :], -jnp.inf))
        scores = jnp.einsum('bhik,bhjk,bhijk->bhij', qc, kc, decay)
        o = jnp.einsum('bhij,bhjv->bhiv', scores, vc) + jnp.einsum('bhik,bhkv->bhiv', qc * jnp.exp(b), s)
        s = jnp.exp(b_end)[:, :, 0, :, None] * s + jnp.einsum('bhjk,bhjv->bhkv', kc * jnp.exp(b_end - b), vc)
        return s, o

    s_end, o = lax.scan(step, s0, (_to_chunks(q), _to_chunks(k), _to_chunks(v), _to_chunks(log_a)))
    return _from_chunks(o), s_end


def _retention_chunked(q, k, v, log_gamma, s0):
    pos = jnp.arange(CHUNK, dtype=F32)
    lg = log_gamma.astype(F32)[:, None, None]
    rel = pos[:, None] - pos[None, :]
    d_intra = jnp.where(rel >= 0, jnp.exp(lg * jnp.maximum(rel, 0.0)), 0.0)
    q_decay = jnp.exp(lg * (pos + 1.0)[:, None])
    k_decay = jnp.exp(lg * (CHUNK - 1.0 - pos)[:, None])
    c_decay = jnp.exp(lg * CHUNK)

    def step(s, xs):
        qc, kc, vc = xs
        scores = jnp.einsum('bhik,bhjk->bhij', qc, kc) * d_intra
        o = jnp.einsum('bhij,bhjv->bhiv', scores, vc) + jnp.einsum('bhik,bhkv->bhiv', qc * q_decay, s)
        s = c_decay * s + jnp.einsum('bhjk,bhjv->bhkv', kc * k_decay, vc)
        return s, o

    s_end, o = lax.scan(step, s0, (_to_chunks(q), _to_chunks(k), _to_chunks(v)))
    return _from_chunks(o), s_end


def _gla_final_state(k, v, log_a):
    b = jnp.cumsum(log_a, axis=1)
    w = jnp.exp(b[:, -1:] - b)
    return jnp.einsum('bthk,bthv->bhkv', k * w, v)


def _ret_final_state(k, v, log_gamma):
    T = k.shape[1]
    w = jnp.exp(log_gamma.astype(F32)[None, :] * jnp.arange(T - 1, -1, -1, dtype=F32)[:, None])
    return jnp.einsum('bthk,bthv->bhkv', k * w[None, :, :, None], v)


def _project(h, w_in, gla_gate_w, gla_gate_b, rope):
    B, T, _ = h.shape
    gq, gk, gv, gz, gg, rq, rk, rv, rg = jnp.split(h @ w_in, IN_SPLIT_POINTS, axis=-1)
    gq = gq.reshape(B, T, GLA_HEADS, GLA_DK) * (GLA_DK ** -0.5)
    gk = gk.reshape(B, T, GLA_HEADS, GLA_DK)
    gv = gv.reshape(B, T, GLA_HEADS, GLA_DV)
    pre = jnp.einsum('btdr,drk->btdk', gz.reshape(B, T, 2, GLA_GATE_RANK), gla_gate_w) + gla_gate_b
    log_a = (jax.nn.log_sigmoid(pre.astype(F32)) / GLA_GATE_TAU).reshape(B, T, 2, GLA_HEADS, GLA_DK)
    rq = rq.reshape(B, T, RET_HEADS, RET_DK)
    rk = rk.reshape(B, T, RET_HEADS, RET_DK) * (RET_DK ** -0.5)
    if rope is not None:
        rq = _apply_rope(rq, rope)
        rk = _apply_rope(rk, rope)
    rv = rv.reshape(B, T, RET_HEADS, RET_DV)
    return gq, gk, gv, log_a, gg, rq, rk, rv, rg


def _zero_states(B):
    g = jnp.zeros((B, GLA_HEADS, GLA_DK, GLA_DV), F32)
    r = jnp.zeros((B, RET_HEADS, RET_DK, RET_DV), F32)
    return (g, g, r, r)


def _token_mixer(h, w_in, gla_gate_w, gla_gate_b, ret_decay_logit, gla_norm_w, ret_norm_w, w_out, init_states, rope):
    gq, gk, gv, log_a, gg, rq, rk, rv, rg = _project(h, w_in, gla_gate_w, gla_gate_b, rope)
    log_gamma = jax.nn.log_sigmoid(ret_decay_logit.astype(F32))
    s_gf, s_gb, s_rf, s_rb = init_states
    o_gf, s_gf = _gla_chunked(gq, gk, gv, log_a[:, :, 0], s_gf)
    o_gb, s_gb = _gla_chunked(_flip(gq), _flip(gk), _flip(gv), _flip(log_a[:, :, 1]), s_gb)
    o_rf, s_rf = _retention_chunked(rq, rk, rv, log_gamma[0], s_rf)
    o_rb, s_rb = _retention_chunked(_flip(rq), _flip(rk), _flip(rv), log_gamma[1], s_rb)
    o_g = _head_rmsnorm(o_gf + _flip(o_gb), gla_norm_w) * jax.nn.silu(gg)
    o_r = _head_layernorm(o_rf + _flip(o_rb), ret_norm_w) * jax.nn.silu(rg)
    out = jnp.concatenate([o_g, o_r], axis=-1) @ w_out
    return out, (s_gf, s_gb, s_rf, s_rb)


def _context_states(hc, w_in, gla_gate_w, gla_gate_b, ret_decay_logit):
    _, gk, gv, log_a, _, _, rk, rv, _ = _project(hc, w_in, gla_gate_w, gla_gate_b, None)
    log_gamma = jax.nn.log_sigmoid(ret_decay_logit.astype(F32))
    s_gf = _gla_final_state(gk, gv, log_a[:, :, 0])
    s_gb = _gla_final_state(_flip(gk), _flip(gv), _flip(log_a[:, :, 1]))
    s_rf = _ret_final_state(rk, rv, log_gamma[0])
    s_rb = _ret_final_state(_flip(rk), _flip(rv), log_gamma[1])
    return (s_gf, s_gb, s_rf, s_rb)


def _expert_choice_ffn(h, w_router, w_gate, w_up, w_down):
    B, T, _ = h.shape
    cap = EC_CAPACITY_FACTOR * T // N_EXPERTS
    aff = jax.nn.softmax((h @ w_router).astype(F32), axis=-1)
    gate, idx = lax.top_k(jnp.swapaxes(aff, 1, 2), cap)
    bidx = jnp.arange(B)[:, None, None]
    xe = h[bidx, idx]
    a = jnp.einsum('becd,edf->becf', xe, w_gate)
    u = jnp.einsum('becd,edf->becf', xe, w_up)
    ye = jnp.einsum('becf,efd->becd', jax.nn.silu(a) * u, w_down) * gate[..., None]
    return jnp.zeros_like(h).at[bidx, idx].add(ye.astype(h.dtype))


def setup_inputs(seed: int = 0) -> dict:
    key = jax.random.key(seed)
    ks = jax.random.split(key, 24)
    n = lambda k, shape, s: jax.random.normal(k, shape, F32) * s
    logit0 = jnp.asarray(np.log(2.0 ** (RET_GAMMA_EXP0 + np.arange(RET_HEADS)) - 1.0), F32)
    return {
        'x': n(ks[0], (BATCH, SEQ, D_MODEL), 1.0),
        'c': n(ks[1], (BATCH, D_MODEL), 1.0),
        'ctx': n(ks[2], (BATCH, CTX_LEN, D_MODEL), 1.0),
        'c_ctx': n(ks[3], (D_MODEL,), 1.0),
        'w_ada': n(ks[4], (DEPTH, D_MODEL, N_ADA * D_MODEL), 0.02),
        'b_ada': n(ks[5], (DEPTH, N_ADA * D_MODEL), 0.02),
        'norm1_w': 1.0 + n(ks[6], (DEPTH, D_MODEL), 0.02),
        'w_in': n(ks[7], (DEPTH, D_MODEL, IN_WIDTH), D_MODEL ** -0.5),
        'gla_gate_w': n(ks[8], (DEPTH, 2, GLA_GATE_RANK, GLA_QK), GLA_GATE_RANK ** -0.5),
        'gla_gate_b': n(ks[9], (DEPTH, 2, GLA_QK), 0.1),
        'ret_decay_logit': logit0[None, None, :] + n(ks[10], (DEPTH, 2, RET_HEADS), 0.05),
        'gla_norm_w': 1.0 + n(ks[11], (DEPTH, GLA_V), 0.02),
        'ret_norm_w': 1.0 + n(ks[12], (DEPTH, RET_V), 0.02),
        'w_out': n(ks[13], (DEPTH, MIX_WIDTH, D_MODEL), MIX_WIDTH ** -0.5),
        'norm2_w': 1.0 + n(ks[14], (DEPTH, D_MODEL), 0.02),
        'w_router': n(ks[15], (DEPTH, D_MODEL, N_EXPERTS), D_MODEL ** -0.5),
        'w_exp_gate': n(ks[16], (DEPTH, N_EXPERTS, D_MODEL, EXPERT_FF), D_MODEL ** -0.5),
        'w_exp_up': n(ks[17], (DEPTH, N_EXPERTS, D_MODEL, EXPERT_FF), D_MODEL ** -0.5),
        'w_exp_down': n(ks[18], (DEPTH, N_EXPERTS, EXPERT_FF, D_MODEL), EXPERT_FF ** -0.5),
        'final_norm_w': 1.0 + n(ks[19], (D_MODEL,), 0.02),
    }


def reference(x, c, ctx, c_ctx, w_ada, b_ada, norm1_w, w_in, gla_gate_w, gla_gate_b, ret_decay_logit,
              gla_norm_w, ret_norm_w, w_out, norm2_w, w_router, w_exp_gate, w_exp_up, w_exp_down, final_norm_w):
    B, n_lat, D = x.shape
    rope = _grid_rope_angles(n_lat)
    for i in range(DEPTH):
        last = i == DEPTH - 1
        mod = (jax.nn.silu(c) @ w_ada[i] + b_ada[i]).reshape(B, N_ADA, D)[:, :, None, :]
        mod_c = (jax.nn.silu(c_ctx) @ w_ada[i] + b_ada[i]).reshape(N_ADA, D)
        hc = _rmsnorm(ctx, norm1_w[i]) * (1.0 + mod_c[1]) + mod_c[0]
        if last:
            ctx_states = _context_states(hc, w_in[i], gla_gate_w[i], gla_gate_b[i], ret_decay_logit[i])
        else:
            ctx_out, ctx_states = _token_mixer(hc, w_in[i], gla_gate_w[i], gla_gate_b[i], ret_decay_logit[i],
                                               gla_norm_w[i], ret_norm_w[i], w_out[i], _zero_states(B), None)
        h = _rmsnorm(x, norm1_w[i]) * (1.0 + mod[:, 1]) + mod[:, 0]
        x_out, _ = _token_mixer(h, w_in[i], gla_gate_w[i], gla_gate_b[i], ret_decay_logit[i],
                                gla_norm_w[i], ret_norm_w[i], w_out[i], ctx_states, rope)
        x = x + mod[:, 2] * x_out
        h2 = _rmsnorm(x, norm2_w[i]) * (1.0 + mod[:, 4]) + mod[:, 3]
        x = x + mod[:, 5] * _expert_choice_ffn(h2, w_router[i], w_exp_gate[i], w_exp_up[i], w_exp_down[i])
        if not last:
            ctx = ctx + mod_c[2] * ctx_out
            hc2 = _rmsnorm(ctx, norm2_w[i]) * (1.0 + mod_c[4]) + mod_c[3]
            ctx = ctx + mod_c[5] * _expert_choice_ffn(hc2, w_router[i], w_exp_gate[i], w_exp_up[i], w_exp_down[i])
    return _rmsnorm(x, final_norm_w)
```

```python
import math
from contextlib import ExitStack

import numpy as np
import ml_dtypes
import concourse.bass as bass
import concourse.mybir as mybir
from concourse.bass_utils import run_bass_kernel_spmd

F32 = mybir.dt.float32
BF16 = mybir.dt.bfloat16
I32 = mybir.dt.int32
ALU = mybir.AluOpType
AF = mybir.ActivationFunctionType
AX = mybir.AxisListType

D = 1024
NE = 16
FF = 1408
NF = FF // 128
INW = 3616
EPS = 1e-6
CTX = 256
RW = 1024 + 36

ENGS = ["pe", "act", "dve", "pool", "sp"]
KDMA = 8
BLK = 8192


class Op:
    __slots__ = ("eng", "fn", "dma", "waits", "sem", "val")


class Prog:
    def __init__(self, nc, es):
        self.nc = nc
        self.es = es
        self.ops = {e: [] for e in ENGS}
        self.last_w = {}
        self.readers = {}
        self.ncomp = {e: 0 for e in ENGS}
        self.ndma = {e: 0 for e in ENGS}
        self.waited = {e: {} for e in ENGS}
        self.pending = {e: [] for e in ENGS}
        self.csem = {}
        self.dsem = {}
        self.nsem = 0

    def _sem(self, name):
        self.nsem += 1
        return self.es.enter_context(self.nc.semaphore(name))

    def _csem(self, eng, i):
        k = (eng, i // BLK)
        if k not in self.csem:
            self.csem[k] = self._sem("c_%s_%d" % k)
        return self.csem[k], i % BLK + 1

    def _dsem(self, eng, i):
        k = (eng, i % KDMA)
        if k not in self.dsem:
            self.dsem[k] = self._sem("d_%s_%d" % k)
        return self.dsem[k], 16 * (i // KDMA + 1)

    def _want(self, op, sem, val):
        w = self.waited[op.eng]
        key = id(sem)
        if w.get(key, 0) >= val:
            return
        w[key] = val
        op.waits.append((sem, val))

    def add(self, eng, fn, reads=(), writes=(), dma=False):
        op = Op()
        op.eng, op.fn, op.dma, op.waits = eng, fn, dma, []
        for sem, val in self.pending[eng]:
            self._want(op, sem, val)
        self.pending[eng] = []
        deps = []
        for k in reads:
            w = self.last_w.get(k)
            if w is not None:
                deps.append(w)
        for k in writes:
            w = self.last_w.get(k)
            if w is not None:
                deps.append(w)
            deps.extend(self.readers.get(k, ()))
        for d in deps:
            if d.eng == eng and eng == "pe" and not d.dma and not dma:
                continue
            self._want(op, d.sem, d.val)
        if dma:
            i = self.ndma[eng]
            op.sem, op.val = self._dsem(eng, i)
            if i >= KDMA:
                self._want(op, op.sem, op.val - 16)
            self.ndma[eng] = i + 1
        else:
            i = self.ncomp[eng]
            op.sem, op.val = self._csem(eng, i)
            self.ncomp[eng] = i + 1
        for k in writes:
            self.last_w[k] = op
            self.readers[k] = []
        for k in reads:
            self.readers.setdefault(k, []).append(op)
        self.ops[eng].append(op)
        return op

    def _latest(self):
        out = []
        for e in ENGS:
            n = self.ncomp[e]
            if n:
                out.append(self._csem(e, n - 1))
            nd = self.ndma[e]
            for j in range(max(0, nd - KDMA), nd):
                out.append(self._dsem(e, j))
        return out

    def barrier(self):
        lat = self._latest()
        for e in ENGS:
            self.pending[e] = list(lat)

    def emit(self):
        final = self._latest()
        with self.nc.Block() as block:
            def run(ename):
                def body(e):
                    for op in self.ops[ename]:
                        for sem, val in op.waits:
                            e.wait_ge(sem, val)
                        ins = op.fn(e)
                        ins.then_inc(op.sem, 16 if op.dma else 1)
                    if ename == "sp":
                        for sem, val in final:
                            e.wait_ge(sem, val)
                return body
            block.tensor(run("pe"))
            block.scalar(run("act"))
            block.vector(run("dve"))
            block.gpsimd(run("pool"))
            block.sync(run("sp"))


def build_program(T, debug=False):
    NT = T // 128
    NTT = NT + 2
    CAP = 2 * T // NE
    NST = CAP // 128
    SC = min(512, CAP)
    NSC = CAP // SC
    nc = bass.Bass("TRN2", target_bir_lowering=False)
    es = ExitStack()
    P = Prog(nc, es)

    def din(name, shape, dt=F32):
        return nc.dram_tensor(name, list(shape), dt, kind="ExternalInput").ap()

    def dscr(name, shape, dt, dump=False):
        if debug and dump:
            return nc.dram_tensor(name, list(shape), dt, kind="ExternalOutput").ap()
        return nc.dram_tensor(name, list(shape), dt).ap()

    def sb(name, shape, dt=F32):
        return es.enter_context(nc.sbuf_tensor(name, list(shape), dt))

    def ps(name, shape, dt=F32):
        return es.enter_context(nc.psum_tensor(name, list(shape), dt))

    xin = din("xin", [NTT * 128, D])
    rope = din("rope", [NTT * 128, 128])
    scT = din("scT", [128, 16])
    w_ada = din("w_ada", [D, 6 * D])
    b_ada_bc = din("b_ada_bc", [128, 6 * D])
    n1_bc = din("n1_bc", [128, D])
    n2_bc = din("n2_bc", [128, D])
    fn_bc = din("fn_bc", [128, D])
    hn_bc = din("hn_bc", [128, D])
    w_in = din("w_in", [D, INW])
    gwaug = din("gwaug", [33, 512])
    rdl_bc = din("rdl_bc", [128, 8])
    w_out = din("w_out", [D, D])
    w_router = din("w_router", [D, NE])
    w_eg = din("w_eg", [NE, D, FF])
    w_eu = din("w_eu", [NE, D, FF])
    w_ed = din("w_ed", [NE, FF, D])
    cst = din("cst", [128, 1024])
    out = nc.dram_tensor("out", [T, D], F32, kind="ExternalOutput").ap()

    QT = [dscr("QT%d" % d, [NTT, 128, 768], BF16, True) for d in range(2)]
    KT = [dscr("KT%d" % d, [NTT, 128, 768], BF16, True) for d in range(2)]
    KK = [dscr("KK%d" % d, [NTT, 128, 768], BF16, True) for d in range(2)]
    VV = dscr("VV", [NTT, 128, 1024], BF16, True)
    SG = dscr("SG", [NT, 128, 1024], BF16, True)
    OF = dscr("OF", [NT, 128, 1024], F32, True)
    X1 = dscr("X1", [T, D], F32, True)
    H2 = dscr("H2", [NT, 128, RW], BF16, True)
    XE = [dscr("XE%d" % e, [CAP + 128, RW], BF16) for e in range(NE)]
    dbg_aff = dscr("dbg_aff", [128, NT * NE], F32, True) if debug else None
    dbg_idx = dscr("dbg_idx", [128, NT * NE], I32, True) if debug else None
    dbg_mod = dscr("dbg_mod", [6, 128, D], F32, True) if debug else None

    csb = sb("csb", [128, 1024])
    ident_b = sb("ident_b", [128, 128], BF16)
    triF_b = sb("triF_b", [128, 128], BF16)
    triB_b = sb("triB_b", [128, 128], BF16)
    triS_b = sb("triS_b", [128, 128], BF16)
    ones_b = sb("ones_b", [128, 128], BF16)
    A1 = [sb("A1_%d" % i, [128, D]) for i in range(2)]
    B1 = [sb("B1_%d" % i, [128, D]) for i in range(2)]
    A2 = sb("A2", [128, D])
    B2 = sb("B2", [128, D])
    M2 = sb("M2", [128, D])
    M5 = sb("M5", [128, D])
    ET = sb("ET", [128, NTT * 4])
    ER = sb("ER", [128, 8])
    DQ = sb("DQ", [128, 8])
    DK = sb("DK", [128, 8])
    affT = sb("affT", [128, NT * NE])
    idxT = sb("idxT", [128, NT * NE], I32)
    identF = csb[:, 0:128]
    triF = csb[:, 128:256]
    triB = csb[:, 256:384]
    triS = csb[:, 384:512]
    onesF = csb[:, 512:640]
    IOTA = 640

    P.add("sp", lambda e: e.dma_start(out=csb[:], in_=cst[:, :]), writes=["csb"], dma=True)
    for nm, dst, src in (("ident_b", ident_b, identF), ("triF_b", triF_b, triF), ("triB_b", triB_b, triB),
                         ("triS_b", triS_b, triS), ("ones_b", ones_b, onesF)):
        P.add("dve", lambda e, dst=dst, src=src: e.tensor_copy(out=dst[:], in_=src), reads=["csb"], writes=[nm])

    with ExitStack() as ph:
        def sbp(name, shape, dt=F32):
            return ph.enter_context(nc.sbuf_tensor(name, list(shape), dt))
        sc = sbp("sc", [128, 16])
        scs = sbp("scs", [128, 16])
        scbc = sbp("scbc", [128, 16, 128])
        wab = [sbp("wab%d" % i, [128, 8, 512]) for i in range(2)]
        bab = [sbp("bab%d" % i, [128, 512]) for i in range(2)]
        nw1 = sbp("nw1", [128, D])
        nw2 = sbp("nw2", [128, D])
        modt = [sbp("modt%d" % i, [128, 512]) for i in range(2)]
        rd = sbp("rd", [128, 8])
        rd2 = sbp("rd2", [128, 8])
        lg = sbp("lg", [128, 8])
        pm = [es.enter_context(nc.psum_tensor("pm%d" % i, [128, 512], F32)) for i in range(2)]

        P.add("sp", lambda e: e.dma_start(out=sc[:], in_=scT[:, :]), writes=["sc"], dma=True)
        P.add("sp", lambda e: e.dma_start(out=nw1[:], in_=n1_bc[:, :]), writes=["nw1"], dma=True)
        P.add("sp", lambda e: e.dma_start(out=nw2[:], in_=n2_bc[:, :]), writes=["nw2"], dma=True)
        P.add("sp", lambda e: e.dma_start(out=rd[:], in_=rdl_bc[:, :]), writes=["rd"], dma=True)
        P.add("act", lambda e: e.activation(out=scs[:], in_=sc[:], func=AF.Silu), reads=["sc"], writes=["scs"])
        P.add("dve", lambda e: e.tensor_copy(out=scbc[:], in_=scs[:].unsqueeze(2).to_broadcast([128, 16, 128])),
              reads=["scs"], writes=["scbc"])
        P.add("act", lambda e: e.activation(out=rd2[:], in_=rd[:], func=AF.Exp, scale=-1.0), reads=["rd"], writes=["rd2"])
        P.add("act", lambda e: e.activation(out=lg[:], in_=rd2[:], func=AF.Ln, bias=1.0), reads=["rd2"], writes=["lg"])
        for d in range(2):
            cq = IOTA + 3 + d
            ck = IOTA + 1 + d
            P.add("act", lambda e, d=d, cq=cq: e.activation(out=DQ[:, d * 4:(d + 1) * 4], in_=lg[:, d * 4:(d + 1) * 4], func=AF.Exp,
                                                           scale=csb[:, cq:cq + 1]), reads=["lg", "csb"], writes=["DQ%d" % d])
            P.add("act", lambda e, d=d, ck=ck: e.activation(out=DK[:, d * 4:(d + 1) * 4], in_=lg[:, d * 4:(d + 1) * 4], func=AF.Exp,
                                                           scale=csb[:, ck:ck + 1], bias=csb[:, IOTA + 6:IOTA + 7]),
                  reads=["lg", "csb"], writes=["DK%d" % d])
        P.add("act", lambda e: e.activation(out=ER[:], in_=lg[:], func=AF.Exp, scale=-128.0), reads=["lg"], writes=["ER"])

        jobs = [(n, 0) for n in range(12)] + [(n, 1) for n in range(4)]
        for ji, (n, which) in enumerate(jobs):
            bi = ji % 2
            for k in range(8):
                P.add("sp", lambda e, n=n, k=k, bi=bi: e.dma_start(out=wab[bi][:, k, :], in_=w_ada[k * 128:(k + 1) * 128, n * 512:(n + 1) * 512]),
                      writes=["wab%d" % bi], dma=True)
            P.add("sp", lambda e, n=n, bi=bi: e.dma_start(out=bab[bi][:], in_=b_ada_bc[:, n * 512:(n + 1) * 512]),
                  writes=["bab%d" % bi], dma=True)
            for k in range(8):
                P.add("pe", lambda e, k=k, bi=bi, which=which: e.matmul(pm[bi][:], lhsT=scbc[:, which * 8 + k, :], rhs=wab[bi][:, k, :],
                                                                        start=(k == 0), stop=(k == 7)),
                      reads=["scbc", "wab%d" % bi], writes=["pm%d" % bi])
            P.add("dve", lambda e, bi=bi: e.tensor_tensor(out=modt[bi][:], in0=pm[bi][:], in1=bab[bi][:], op=ALU.add),
                  reads=["pm%d" % bi, "bab%d" % bi], writes=["modt%d" % bi])
            m, half = n // 2, n % 2
            cs = slice(half * 512, (half + 1) * 512)
            if debug and which == 0:
                P.add("sp", lambda e, bi=bi, m=m, cs=cs: e.dma_start(out=dbg_mod[m, :, cs], in_=modt[bi][:]),
                      reads=["modt%d" % bi], writes=["dbgmod"], dma=True)
            if m == 0:
                P.add("dve", lambda e, bi=bi, cs=cs, which=which: e.tensor_copy(out=B1[which][:, cs], in_=modt[bi][:]),
                      reads=["modt%d" % bi], writes=["B1_%d" % which])
            elif m == 1:
                P.add("dve", lambda e, bi=bi, cs=cs, which=which: e.scalar_tensor_tensor(out=A1[which][:, cs], in0=modt[bi][:], scalar=1.0, in1=nw1[:, cs],
                                                                                         op0=ALU.add, op1=ALU.mult),
                      reads=["modt%d" % bi, "nw1"], writes=["A1_%d" % which])
            elif m == 2:
                P.add("dve", lambda e, bi=bi, cs=cs: e.tensor_copy(out=M2[:, cs], in_=modt[bi][:]), reads=["modt%d" % bi], writes=["M2"])
            elif m == 3:
                P.add("dve", lambda e, bi=bi, cs=cs: e.tensor_copy(out=B2[:, cs], in_=modt[bi][:]), reads=["modt%d" % bi], writes=["B2"])
            elif m == 4:
                P.add("dve", lambda e, bi=bi, cs=cs: e.scalar_tensor_tensor(out=A2[:, cs], in0=modt[bi][:], scalar=1.0, in1=nw2[:, cs],
                                                                            op0=ALU.add, op1=ALU.mult),
                      reads=["modt%d" % bi, "nw2"], writes=["A2"])
            else:
                P.add("dve", lambda e, bi=bi, cs=cs: e.tensor_copy(out=M5[:, cs], in_=modt[bi][:]), reads=["modt%d" % bi], writes=["M5"])
        P.barrier()

    PA = ps("PA", [128, 1024], BF16)
    PQ = [ps("PQ%d" % i, [128, 512]) for i in range(5)]

    with ExitStack() as ph:
        def sbp(name, shape, dt=F32):
            return ph.enter_context(nc.sbuf_tensor(name, list(shape), dt))
        win = sbp("win", [128, 8, INW], BF16)
        gw = sbp("gw", [33, 512], BF16)
        gzaug = sbp("gzaug", [33, 128], BF16)
        xt = [sbp("xt%d" % i, [128, D]) for i in range(2)]
        rp = [sbp("rp%d" % i, [128, 128]) for i in range(2)]
        junk = sbp("junk", [128, D], BF16)
        ss = sbp("ss", [128, 4])
        htmp = sbp("htmp", [128, D])
        hb = sbp("hb", [128, D], BF16)
        hT = sbp("hT", [128, D], BF16)
        t1 = sbp("t1", [128, 512])
        spl = sbp("spl", [128, 512])
        epos = sbp("epos", [128, 512])
        eneg = sbp("eneg", [128, 512])
        rr = sbp("rr", [128, 512])
        ra = sbp("ra", [128, 256])
        rb = sbp("rb", [128, 256])
        qtok = [[sbp("qtok%d_%d" % (d, i), [128, 768], BF16) for i in range(2)] for d in range(2)]
        ktok = [[sbp("ktok%d_%d" % (d, i), [128, 768], BF16) for i in range(2)] for d in range(2)]
        qts = [[sbp("qts%d_%d" % (d, i), [128, 768], BF16) for i in range(2)] for d in range(2)]
        kts = [[sbp("kts%d_%d" % (d, i), [128, 768], BF16) for i in range(2)] for d in range(2)]
        vvt = [sbp("vvt%d" % i, [128, 1024], BF16) for i in range(2)]
        sgt = [sbp("sgt%d" % i, [128, 1024], BF16) for i in range(2)]

        for k in range(8):
            for hf in range(2):
                P.add("pool", lambda e, k=k, hf=hf: e.dma_start(out=win[:, k, hf * 1808:(hf + 1) * 1808],
                                                               in_=w_in[k * 128:(k + 1) * 128, hf * 1808:(hf + 1) * 1808]),
                      writes=["win"], dma=True)
        P.add("pool", lambda e: e.dma_start(out=gw[:], in_=gwaug[:, :]), writes=["gw"], dma=True)
        P.add("dve", lambda e: e.memset(gzaug[:], 1.0), writes=["gzaug"])

        for tt in range(NTT):
            b = tt % 2
            isctx = tt < 2
            ci = 1 if isctx else 0
            X = "xt%d" % b
            P.add("sp", lambda e, tt=tt, b=b: e.dma_start(out=xt[b][:], in_=xin[tt * 128:(tt + 1) * 128, :]), writes=[X], dma=True)
            P.add("sp", lambda e, tt=tt, b=b: e.dma_start(out=rp[b][:], in_=rope[tt * 128:(tt + 1) * 128, :]), writes=["rp%d" % b], dma=True)
            P.add("act", lambda e, b=b: e.activation(out=junk[:], in_=xt[b][:], func=AF.Square, accum_out=ss[:, 0:1]),
                  reads=[X], writes=["junk", "ss0"])
            P.add("act", lambda e: e.activation(out=ss[:, 1:2], in_=ss[:, 0:1], func=AF.Sqrt, scale=1.0 / D, bias=csb[:, IOTA + 7:IOTA + 8]),
                  reads=["ss0", "csb"], writes=["ss1"])
            P.add("dve", lambda e: e.reciprocal(out=ss[:, 2:3], in_=ss[:, 1:2]), reads=["ss1"], writes=["ss2"])
            P.add("dve", lambda e, b=b, ci=ci: e.scalar_tensor_tensor(out=htmp[:], in0=xt[b][:], scalar=ss[:, 2:3], in1=A1[ci][:],
                                                                      op0=ALU.mult, op1=ALU.mult),
                  reads=[X, "ss2", "A1_%d" % ci], writes=["htmp"])
            P.add("dve", lambda e, ci=ci: e.tensor_tensor(out=hb[:], in0=htmp[:], in1=B1[ci][:], op=ALU.add),
                  reads=["htmp", "B1_%d" % ci], writes=["hb"])
            for k in range(8):
                P.add("pe", lambda e, k=k: e.transpose(out=PA[:, k * 128:(k + 1) * 128], in_=hb[:, k * 128:(k + 1) * 128], identity=ident_b[:]),
                      reads=["hb", "ident_b"], writes=["PA"])
            P.add("act", lambda e: e.activation(out=hT[:], in_=PA[:], func=AF.Copy), reads=["PA"], writes=["hT"])

            for k in range(8):
                P.add("pe", lambda e, k=k: e.matmul(PQ[4][0:32, 0:128], lhsT=win[:, k, 3584:3616], rhs=hT[:, k * 128:(k + 1) * 128],
                                                    start=(k == 0), stop=(k == 7)), reads=["win", "hT"], writes=["PQ4"])
            P.add("act", lambda e: e.activation(out=gzaug[0:32, :], in_=PQ[4][0:32, 0:128], func=AF.Copy), reads=["PQ4"], writes=["gzaug"])
            P.add("pe", lambda e: e.matmul(PQ[4][:], lhsT=gzaug[:], rhs=gw[:], start=True, stop=True), reads=["gzaug", "gw"], writes=["PQ4"])
            P.add("act", lambda e: e.activation(out=t1[:], in_=PQ[4][:], func=AF.Exp, scale=-1.0), reads=["PQ4"], writes=["t1"])
            P.add("act", lambda e: e.activation(out=spl[:], in_=t1[:], func=AF.Ln, bias=1.0), reads=["t1"], writes=["spl"])
            P.add("pe", lambda e: e.matmul(PQ[4][:, 0:256], lhsT=triF, rhs=spl[:, 0:256], start=True, stop=True), reads=["spl", "csb"], writes=["PQ4"])
            P.add("pe", lambda e: e.matmul(PQ[4][:, 256:512], lhsT=triB, rhs=spl[:, 256:512], start=True, stop=True), reads=["spl", "csb"], writes=["PQ4"])
            for d in range(2):
                for blk in range(2):
                    j = d * 2 + blk
                    P.add("pe", lambda e, d=d, blk=blk, j=j: e.matmul(PQ[3][:, j:j + 1], lhsT=spl[:, d * 256 + blk * 128: d * 256 + (blk + 1) * 128],
                                                                       rhs=csb[:, 512:513], start=True, stop=True),
                          reads=["spl", "csb"], writes=["PQ3"])
            P.add("act", lambda e: e.activation(out=epos[:], in_=PQ[4][:], func=AF.Exp, scale=-1.0 / 16), reads=["PQ4"], writes=["epos"])
            P.add("act", lambda e: e.activation(out=eneg[:], in_=PQ[4][:], func=AF.Exp, scale=1.0 / 16), reads=["PQ4"], writes=["eneg"])
            P.add("act", lambda e, tt=tt: e.activation(out=ET[:, tt * 4:(tt + 1) * 4], in_=PQ[3][:, 0:4], func=AF.Exp, scale=-1.0 / 16),
                  reads=["PQ3"], writes=["ET"])

            def inproj(g, pq):
                for k in range(8):
                    P.add("pe", lambda e, k=k, g=g, pq=pq: e.matmul(PQ[pq][:], lhsT=hT[:, k * 128:(k + 1) * 128], rhs=win[:, k, g * 512:(g + 1) * 512],
                                                                    start=(k == 0), stop=(k == 7)), reads=["win", "hT"], writes=["PQ%d" % pq])

            QK = [("qtok%d_%d" % (d, b), "ktok%d_%d" % (d, b)) for d in range(2)]
            inproj(0, 0)
            for d in range(2):
                P.add("dve", lambda e, d=d, b=b: e.scalar_tensor_tensor(out=qtok[d][b][:, 0:256], in0=PQ[0][:, 0:256], scalar=0.125,
                                                                        in1=epos[:, d * 256:(d + 1) * 256], op0=ALU.mult, op1=ALU.mult),
                      reads=["PQ0", "epos"], writes=[QK[d][0]])
                P.add("dve", lambda e, d=d, b=b: e.tensor_tensor(out=ktok[d][b][:, 0:256], in0=PQ[0][:, 256:512], in1=eneg[:, d * 256:(d + 1) * 256], op=ALU.mult),
                      reads=["PQ0", "eneg"], writes=[QK[d][1]])
            inproj(1, 1)
            P.add("act", lambda e, b=b: e.activation(out=vvt[b][:, 0:512], in_=PQ[1][:], func=AF.Copy), reads=["PQ1"], writes=["vvt%d" % b])
            if not isctx:
                inproj(2, 2)
                P.add("act", lambda e, b=b: e.activation(out=sgt[b][:, 0:512], in_=PQ[2][:], func=AF.Silu), reads=["PQ2"], writes=["sgt%d" % b])
            for g, pq, isq in ((3, 0, True), (4, 1, False)):
                inproj(g, pq)
                pv = "PQ%d" % pq
                cosb = lambda b=b: rp[b][:, 0:64].unsqueeze(1).to_broadcast([128, 4, 64])
                sinb = lambda b=b: rp[b][:, 64:128].unsqueeze(1).to_broadcast([128, 4, 64])
                a1 = lambda pq=pq: PQ[pq][:].rearrange("p (h c) -> p h c", h=4)[:, :, 0:64]
                a2 = lambda pq=pq: PQ[pq][:].rearrange("p (h c) -> p h c", h=4)[:, :, 64:128]
                r3 = lambda t: t[:].rearrange("p (h c) -> p h c", h=4)
                rr3 = lambda: rr[:].rearrange("p (h c) -> p h c", h=4)
                R = "rp%d" % b
                P.add("dve", lambda e, a1=a1, cosb=cosb: e.tensor_tensor(out=r3(ra), in0=a1(), in1=cosb(), op=ALU.mult), reads=[pv, R], writes=["ra"])
                P.add("dve", lambda e, a2=a2, sinb=sinb: e.tensor_tensor(out=r3(rb), in0=a2(), in1=sinb(), op=ALU.mult), reads=[pv, R], writes=["rb"])
                P.add("dve", lambda e: e.tensor_tensor(out=rr3()[:, :, 0:64], in0=r3(ra), in1=r3(rb), op=ALU.subtract), reads=["ra", "rb"], writes=["rr"])
                P.add("dve", lambda e, a1=a1, sinb=sinb: e.tensor_tensor(out=r3(ra), in0=a1(), in1=sinb(), op=ALU.mult), reads=[pv, R, "rr"], writes=["ra"])
                P.add("dve", lambda e, a2=a2, cosb=cosb: e.tensor_tensor(out=r3(rb), in0=a2(), in1=cosb(), op=ALU.mult), reads=[pv, R, "rr"], writes=["rb"])
                P.add("dve", lambda e: e.tensor_tensor(out=rr3()[:, :, 64:128], in0=r3(ra), in1=r3(rb), op=ALU.add), reads=["ra", "rb"], writes=["rr"])
                for d in range(2):
                    tab = DQ if isq else DK
                    dst = qtok if isq else ktok
                    P.add("dve", lambda e, d=d, tab=tab, dst=dst, b=b: e.tensor_tensor(
                        out=dst[d][b][:, 256:768].rearrange("p (h c) -> p h c", h=4), in0=rr[:].rearrange("p (h c) -> p h c", h=4),
                        in1=tab[:, d * 4:(d + 1) * 4].unsqueeze(2).to_broadcast([128, 4, 128]), op=ALU.mult),
                        reads=["rr", ("DQ%d" if isq else "DK%d") % d], writes=[QK[d][0 if isq else 1]])
            inproj(5, 2)
            P.add("act", lambda e, b=b: e.activation(out=vvt[b][:, 512:1024], in_=PQ[2][:], func=AF.Copy), reads=["PQ2"], writes=["vvt%d" % b])
            if not isctx:
                inproj(6, 0)
                P.add("act", lambda e, b=b: e.activation(out=sgt[b][:, 512:1024], in_=PQ[0][:], func=AF.Silu), reads=["PQ0"], writes=["sgt%d" % b])
            for d in range(2):
                for (src, srcn, dst, dstn, dr) in ((qtok[d][b], QK[d][0], qts[d][b], "qts%d_%d" % (d, b), QT[d]),
                                                   (ktok[d][b], QK[d][1], kts[d][b], "kts%d_%d" % (d, b), KT[d])):
                    for blk in range(6):
                        P.add("pe", lambda e, src=src, blk=blk: e.transpose(out=PA[:, blk * 128:(blk + 1) * 128], in_=src[:, blk * 128:(blk + 1) * 128],
                                                                             identity=ident_b[:]), reads=[srcn, "ident_b"], writes=["PA"])
                    P.add("act", lambda e, dst=dst: e.activation(out=dst[:], in_=PA[:, 0:768], func=AF.Copy), reads=["PA"], writes=[dstn])
                    P.add("sp", lambda e, dst=dst, dr=dr, tt=tt: e.dma_start(out=dr[tt, :, :], in_=dst[:]), reads=[dstn], writes=["DR_qk"], dma=True)
                P.add("sp", lambda e, d=d, b=b, tt=tt: e.dma_start(out=KK[d][tt, :, :], in_=ktok[d][b][:]), reads=[QK[d][1]], writes=["DR_kk"], dma=True)
            P.add("sp", lambda e, b=b, tt=tt: e.dma_start(out=VV[tt, :, :], in_=vvt[b][:]), reads=["vvt%d" % b], writes=["DR_vv"], dma=True)
            if not isctx:
                P.add("sp", lambda e, b=b, tt=tt: e.dma_start(out=SG[tt - 2, :, :], in_=sgt[b][:]), reads=["sgt%d" % b], writes=["DR_sg"], dma=True)
        P.barrier()

    with ExitStack() as ph:
        def sbp(name, shape, dt=F32):
            return ph.enter_context(nc.sbuf_tensor(name, list(shape), dt))
        wo = sbp("wo", [128, 8, D], BF16)
        wr = sbp("wr", [128, 8, NE], BF16)
        hnw = sbp("hnw", [128, D])
        qT = [sbp("qT%d" % i, [128, 768], BF16) for i in range(2)]
        kT = [sbp("kT%d" % i, [128, 768], BF16) for i in range(2)]
        kk = [sbp("kk%d" % i, [128, 768], BF16) for i in range(2)]
        vv = [sbp("vv%d" % i, [128, 1024], BF16) for i in range(2)]
        S = sbp("S", [128, 768])
        Sb = sbp("Sb", [128, 768], BF16)
        Tt = [sbp("Tt%d" % i, [128, 128]) for i in range(2)]
        PTs = [sbp("PTs%d" % i, [128, 128], BF16) for i in range(4)]
        ofs = [sbp("ofs%d" % i, [128, D]) for i in range(2)]
        osum = sbp("osum", [128, D])
        sq = sbp("sq", [128, D])
        st = sbp("st", [128, 64])
        sgl = [sbp("sgl%d" % i, [128, D], BF16) for i in range(2)]
        xl = [sbp("xl%d" % i, [128, D]) for i in range(2)]
        yb = sbp("yb", [128, D], BF16)
        yT = sbp("yT", [128, D], BF16)
        x1t = [sbp("x1t%d" % i, [128, D]) for i in range(2)]
        junk2 = sbp("junk2", [128, D], BF16)
        h2r = [sbp("h2r%d" % i, [128, RW], BF16) for i in range(2)]
        h2T = sbp("h2T", [128, D], BF16)
        ex = sbp("ex", [128, NE])

        for k in range(8):
            P.add("pool", lambda e, k=k: e.dma_start(out=wo[:, k, :], in_=w_out[k * 128:(k + 1) * 128, :]), writes=["wo"], dma=True)
            P.add("pool", lambda e, k=k: e.dma_start(out=wr[:, k, :], in_=w_router[k * 128:(k + 1) * 128, :]), writes=["wr"], dma=True)
        P.add("sp", lambda e: e.dma_start(out=hnw[:], in_=hn_bc[:, :]), writes=["hnw"], dma=True)

        step = 0
        for d in range(2):
            P.add("dve", lambda e: e.memset(S[:], 0.0), writes=["S%d" % i for i in range(6)])
            P.add("dve", lambda e: e.memset(Sb[:], 0.0), writes=["Sb%d" % i for i in range(6)])
            seq = list(range(NTT)) if d == 0 else [1, 0] + list(range(NTT - 1, 1, -1))
            maskb = triF_b if d == 0 else triB_b
            maskn = "triF_b" if d == 0 else "triB_b"
            for tt in seq:
                b = step % 2
                step += 1
                isctx = tt < 2
                lt = tt - 2
                if not isctx:
                    P.add("sp", lambda e, d=d, tt=tt, b=b: e.dma_start(out=qT[b][:], in_=QT[d][tt, :, :]), reads=["DR_qk"], writes=["qT%d" % b], dma=True)
                    P.add("sp", lambda e, d=d, tt=tt, b=b: e.dma_start(out=kT[b][:], in_=KT[d][tt, :, :]), reads=["DR_qk"], writes=["kT%d" % b], dma=True)
                P.add("sp", lambda e, d=d, tt=tt, b=b: e.dma_start(out=kk[b][:], in_=KK[d][tt, :, :]), reads=["DR_kk"], writes=["kk%d" % b], dma=True)
                P.add("sp", lambda e, tt=tt, b=b: e.dma_start(out=vv[b][:], in_=VV[tt, :, :]), reads=["DR_vv"], writes=["vv%d" % b], dma=True)
                if not isctx and d == 1:
                    P.add("sp", lambda e, lt=lt, b=b: e.dma_start(out=ofs[b][:], in_=OF[lt, :, :]), reads=["DR_of"], writes=["ofs%d" % b], dma=True)
                    P.add("sp", lambda e, lt=lt, b=b: e.dma_start(out=sgl[b][:], in_=SG[lt, :, :]), reads=["DR_sg"], writes=["sgl%d" % b], dma=True)
                    P.add("sp", lambda e, tt=tt, b=b: e.dma_start(out=xl[b][:], in_=xin[tt * 128:(tt + 1) * 128, :]), writes=["xl%d" % b], dma=True)
                hi = 0
                for blk in range(6):
                    if blk < 2:
                        heads = [(2 * blk + j, 64 * j, 64, (2 * blk + j) * 64) for j in range(2)]
                    else:
                        heads = [(4 + blk - 2, 0, 128, 256 + (blk - 2) * 128)]
                    bs = slice(blk * 128, (blk + 1) * 128)
                    if not isctx:
                        for (h, po, dk, kc) in heads:
                            pq = hi % 2
                            pt = hi % 4
                            hi += 1
                            P.add("pe", lambda e, b=b, po=po, dk=dk, bs=bs, pq=pq: e.matmul(PQ[pq][:, 0:128], lhsT=kT[b][po:po + dk, bs], rhs=qT[b][po:po + dk, bs],
                                                                                          start=True, stop=True),
                                  reads=["kT%d" % b, "qT%d" % b], writes=["PQ%d" % pq])
                            P.add("dve", lambda e, pq=pq, pt=pt, maskb=maskb: e.tensor_tensor(out=PTs[pt][:], in0=PQ[pq][:, 0:128], in1=maskb[:], op=ALU.mult),
                                  reads=["PQ%d" % pq, maskn], writes=["PTs%d" % pt])
                            ob = 2 + h // 4
                            oc = slice((h % 4) * 128, (h % 4 + 1) * 128)
                            P.add("pe", lambda e, b=b, pt=pt, h=h, ob=ob, oc=oc: e.matmul(PQ[ob][:, oc], lhsT=PTs[pt][:], rhs=vv[b][:, h * 128:(h + 1) * 128],
                                                                                        start=True, stop=False),
                                  reads=["PTs%d" % pt, "vv%d" % b], writes=["PQ%d" % ob])
                            P.add("pe", lambda e, b=b, po=po, dk=dk, bs=bs, ob=ob, oc=oc: e.matmul(PQ[ob][:, oc], lhsT=qT[b][po:po + dk, bs], rhs=Sb[po:po + dk, bs],
                                                                                                 start=False, stop=True),
                                  reads=["qT%d" % b, "Sb%d" % blk], writes=["PQ%d" % ob])
                    pu = blk % 2
                    pun = "pm%d" % pu
                    for (h, po, dk, kc) in heads:
                        P.add("pe", lambda e, b=b, po=po, dk=dk, kc=kc, h=h, pu=pu: e.matmul(pm[pu][po:po + dk, 0:128], lhsT=kk[b][:, kc:kc + dk], rhs=vv[b][:, h * 128:(h + 1) * 128],
                                                                                           start=True, stop=True),
                              reads=["kk%d" % b, "vv%d" % b], writes=[pun])
                    if blk < 2:
                        eap = lambda tt=tt, d=d, blk=blk: ET[:, tt * 4 + d * 2 + blk: tt * 4 + d * 2 + blk + 1]
                        en = "ET"
                    else:
                        eap = lambda d=d, blk=blk: ER[:, d * 4 + blk - 2: d * 4 + blk - 1]
                        en = "ER"
                    P.add("dve", lambda e, pu=pu, bs=bs: e.tensor_tensor(out=Tt[pu][:], in0=pm[pu][:, 0:128], in1=S[:, bs], op=ALU.add),
                          reads=[pun, "S%d" % blk], writes=["Tt%d" % pu])
                    P.add("act", lambda e, pu=pu, bs=bs, eap=eap: e.activation(out=S[:, bs], in_=Tt[pu][:], func=AF.Copy, scale=eap()),
                          reads=["Tt%d" % pu, en], writes=["S%d" % blk])
                    P.add("dve", lambda e, pu=pu, bs=bs, eap=eap: e.tensor_scalar(out=Sb[:, bs], in0=Tt[pu][:], scalar1=eap(), scalar2=None, op0=ALU.mult),
                          reads=["Tt%d" % pu, en], writes=["Sb%d" % blk])
                if isctx:
                    continue
                if d == 0:
                    for hb_ in range(2):
                        P.add("act", lambda e, b=b, hb_=hb_: e.activation(out=ofs[b][:, hb_ * 512:(hb_ + 1) * 512], in_=PQ[2 + hb_][:], func=AF.Copy),
                              reads=["PQ%d" % (2 + hb_)], writes=["ofs%d" % b])
                    P.add("sp", lambda e, b=b, lt=lt: e.dma_start(out=OF[lt, :, :], in_=ofs[b][:]), reads=["ofs%d" % b], writes=["DR_of"], dma=True)
                    continue
                for hb_ in range(2):
                    cs = slice(hb_ * 512, (hb_ + 1) * 512)
                    P.add("dve", lambda e, b=b, hb_=hb_, cs=cs: e.tensor_tensor(out=osum[:, cs], in0=PQ[2 + hb_][:], in1=ofs[b][:, cs], op=ALU.add),
                          reads=["PQ%d" % (2 + hb_), "ofs%d" % b], writes=["osum"])
                o3 = lambda: osum[:].rearrange("p (h c) -> p h c", h=8)
                P.add("dve", lambda e: e.tensor_reduce(out=st[:, 0:8], in_=o3(), axis=AX.X, op=ALU.add), reads=["osum"], writes=["st_s1"])
                P.add("dve", lambda e: e.tensor_tensor(out=sq[:], in0=osum[:], in1=osum[:], op=ALU.mult), reads=["osum"], writes=["sq"])
                P.add("dve", lambda e: e.tensor_reduce(out=st[:, 8:16], in_=sq[:].rearrange("p (h c) -> p h c", h=8), axis=AX.X, op=ALU.add),
                      reads=["sq"], writes=["st_s2"])
                P.add("dve", lambda e: e.tensor_scalar(out=st[:, 16:24], in0=st[:, 0:8], scalar1=1.0 / 128, scalar2=None, op0=ALU.mult), reads=["st_s1"], writes=["st_m"])
                P.add("dve", lambda e: e.memset(st[:, 16:20], 0.0), reads=["st_m"], writes=["st_m"])
                P.add("dve", lambda e: e.tensor_tensor(out=st[:, 24:32], in0=st[:, 16:24], in1=st[:, 16:24], op=ALU.mult), reads=["st_m"], writes=["st_mm"])
                P.add("dve", lambda e: e.scalar_tensor_tensor(out=st[:, 32:40], in0=st[:, 8:16], scalar=1.0 / 128, in1=st[:, 24:32], op0=ALU.mult, op1=ALU.subtract),
                      reads=["st_s2", "st_mm"], writes=["st_v"])
                P.add("act", lambda e: e.activation(out=st[:, 40:48], in_=st[:, 32:40], func=AF.Sqrt, bias=csb[:, IOTA + 7:IOTA + 8]), reads=["st_v", "csb"], writes=["st_sd"])
                P.add("dve", lambda e: e.reciprocal(out=st[:, 48:56], in_=st[:, 40:48]), reads=["st_sd"], writes=["st_r"])
                P.add("dve", lambda e: e.scalar_tensor_tensor(out=st[:, 56:64], in0=st[:, 16:24], scalar=-1.0, in1=st[:, 48:56], op0=ALU.mult, op1=ALU.mult),
                      reads=["st_m", "st_r"], writes=["st_sh"])
                P.add("dve", lambda e: e.tensor_tensor(out=sq[:].rearrange("p (h c) -> p h c", h=8), in0=o3(),
                                                       in1=st[:, 48:56].unsqueeze(2).to_broadcast([128, 8, 128]), op=ALU.mult), reads=["osum", "st_r", "sq"], writes=["sq"])
                P.add("dve", lambda e: e.tensor_tensor(out=o3(), in0=sq[:].rearrange("p (h c) -> p h c", h=8),
                                                       in1=st[:, 56:64].unsqueeze(2).to_broadcast([128, 8, 128]), op=ALU.add), reads=["sq", "st_sh"], writes=["osum"])
                P.add("dve", lambda e: e.tensor_tensor(out=sq[:], in0=osum[:], in1=hnw[:], op=ALU.mult), reads=["osum", "hnw"], writes=["sq"])
                P.add("dve", lambda e, b=b: e.tensor_tensor(out=yb[:], in0=sq[:], in1=sgl[b][:], op=ALU.mult), reads=["sq", "sgl%d" % b], writes=["yb"])
                for k in range(8):
                    P.add("pe", lambda e, k=k: e.transpose(out=PA[:, k * 128:(k + 1) * 128], in_=yb[:, k * 128:(k + 1) * 128], identity=ident_b[:]),
                          reads=["yb", "ident_b"], writes=["PA"])
                P.add("act", lambda e: e.activation(out=yT[:], in_=PA[:], func=AF.Copy), reads=["PA"], writes=["yT"])
                for nch in range(2):
                    for k in range(8):
                        P.add("pe", lambda e, k=k, nch=nch: e.matmul(PQ[2 + nch][:], lhsT=yT[:, k * 128:(k + 1) * 128], rhs=wo[:, k, nch * 512:(nch + 1) * 512],
                                                                     start=(k == 0), stop=(k == 7)), reads=["yT", "wo"], writes=["PQ%d" % (2 + nch)])
                    cs = slice(nch * 512, (nch + 1) * 512)
                    P.add("dve", lambda e, nch=nch, cs=cs: e.tensor_tensor(out=sq[:, cs], in0=PQ[2 + nch][:], in1=M2[:, cs], op=ALU.mult),
                          reads=["PQ%d" % (2 + nch), "M2"], writes=["sq"])
                P.add("dve", lambda e, b=b: e.tensor_tensor(out=x1t[b][:], in0=sq[:], in1=xl[b][:], op=ALU.add), reads=["sq", "xl%d" % b], writes=["x1t%d" % b])
                P.add("sp", lambda e, b=b, lt=lt: e.dma_start(out=X1[lt * 128:(lt + 1) * 128, :], in_=x1t[b][:]), reads=["x1t%d" % b], writes=["DR_x1"], dma=True)
                P.add("act", lambda e, b=b: e.activation(out=junk2[:], in_=x1t[b][:], func=AF.Square, accum_out=st[:, 0:1]), reads=["x1t%d" % b, "st_s1"], writes=["junk2", "st_s1"])
                P.add("act", lambda e: e.activation(out=st[:, 1:2], in_=st[:, 0:1], func=AF.Sqrt, scale=1.0 / D, bias=csb[:, IOTA + 7:IOTA + 8]), reads=["st_s1", "csb"], writes=["st_q1"])
                P.add("dve", lambda e: e.reciprocal(out=st[:, 2:3], in_=st[:, 1:2]), reads=["st_q1"], writes=["st_q2"])
                P.add("dve", lambda e, b=b: e.scalar_tensor_tensor(out=sq[:], in0=x1t[b][:], scalar=st[:, 2:3], in1=A2[:], op0=ALU.mult, op1=ALU.mult),
                      reads=["x1t%d" % b, "st_q2", "A2", "sq"], writes=["sq"])
                P.add("dve", lambda e, b=b: e.tensor_tensor(out=h2r[b][:, 0:D], in0=sq[:], in1=B2[:], op=ALU.add), reads=["sq", "B2"], writes=["h2r%d" % b])
                for k in range(8):
                    P.add("pe", lambda e, k=k, b=b: e.transpose(out=PA[:, k * 128:(k + 1) * 128], in_=h2r[b][:, k * 128:(k + 1) * 128], identity=ident_b[:]),
                          reads=["h2r%d" % b, "ident_b"], writes=["PA"])
                P.add("act", lambda e: e.activation(out=h2T[:], in_=PA[:], func=AF.Copy), reads=["PA"], writes=["h2T"])
                for k in range(8):
                    P.add("pe", lambda e, k=k: e.matmul(PQ[4][:, 0:NE], lhsT=h2T[:, k * 128:(k + 1) * 128], rhs=wr[:, k, :], start=(k == 0), stop=(k == 7)),
                          reads=["h2T", "wr"], writes=["PQ4"])
                P.add("dve", lambda e: e.tensor_reduce(out=st[:, 4:5], in_=PQ[4][:, 0:NE], axis=AX.X, op=ALU.max), reads=["PQ4"], writes=["st_mx"])
                P.add("dve", lambda e: e.tensor_scalar(out=st[:, 5:6], in0=st[:, 4:5], scalar1=-1.0, scalar2=None, op0=ALU.mult), reads=["st_mx"], writes=["st_nmx"])
                P.add("act", lambda e: e.activation(out=ex[:], in_=PQ[4][:, 0:NE], func=AF.Exp, bias=st[:, 5:6], accum_out=st[:, 6:7]), reads=["PQ4", "st_nmx"], writes=["ex", "st_sm"])
                P.add("dve", lambda e: e.reciprocal(out=st[:, 7:8], in_=st[:, 6:7]), reads=["st_sm"], writes=["st_rs"])
                P.add("dve", lambda e, lt=lt: e.tensor_scalar(out=affT[:, lt * NE:(lt + 1) * NE], in0=ex[:], scalar1=st[:, 7:8], scalar2=None, op0=ALU.mult),
                      reads=["ex", "st_rs"], writes=["affT"])
                meta = lambda b=b: h2r[b][:, D:RW].bitcast(F32)
                P.add("dve", lambda e, lt=lt, meta=meta: e.tensor_copy(out=meta()[:, 0:NE], in_=affT[:, lt * NE:(lt + 1) * NE]), reads=["affT"], writes=["h2r%d" % b])
                P.add("dve", lambda e, lt=lt, meta=meta: e.tensor_scalar(out=meta()[:, NE:NE + 2], in0=csb[:, IOTA:IOTA + 2], scalar1=float(lt * 128), scalar2=None, op0=ALU.add),
                      reads=["csb"], writes=["h2r%d" % b])
                P.add("sp", lambda e, b=b, lt=lt: e.dma_start(out=H2[lt, :, :], in_=h2r[b][:]), reads=["h2r%d" % b], writes=["DR_h2"], dma=True)
        P.barrier()

    with ExitStack() as ph:
        def sbp(name, shape, dt=F32):
            return ph.enter_context(nc.sbuf_tensor(name, list(shape), dt))
        NTE = NT * NE
        lo = sbp("lo", [128, NE])
        hi_ = sbp("hi", [128, NE])
        mid = sbp("mid", [128, NE])
        cmp_ = sbp("cmp", [128, NTE])
        cnt = sbp("cnt", [128, NE])
        ge = sbp("ge", [128, NE])
        d1 = sbp("d1", [128, NE])
        d2 = sbp("d2", [128, NE])
        maskb_ = sbp("maskb", [128, NTE], BF16)
        wn = sbp("wn", [128, NTE])
        tots = sbp("tots", [128, NTE])
        offs = sbp("offs", [128, NTE])
        pos = sbp("pos", [128, NTE])
        sel = sbp("sel", [128, NTE])
        hrow = [sbp("hrow%d" % i, [128, RW], BF16) for i in range(2)]

        a3 = lambda t: t[:].rearrange("p (t e) -> p t e", e=NE)
        P.add("dve", lambda e: e.memset(lo[:], 0.0), writes=["lo"])
        P.add("dve", lambda e: e.memset(hi_[:], 2.0), writes=["hi"])
        for it in range(40):
            P.add("dve", lambda e: e.tensor_tensor(out=mid[:], in0=lo[:], in1=hi_[:], op=ALU.add), reads=["lo", "hi"], writes=["mid"])
            P.add("dve", lambda e: e.tensor_scalar(out=mid[:], in0=mid[:], scalar1=0.5, scalar2=None, op0=ALU.mult), reads=["mid"], writes=["mid"])
            P.add("dve", lambda e: e.tensor_tensor(out=a3(cmp_), in0=a3(affT), in1=mid[:].unsqueeze(1).to_broadcast([128, NT, NE]), op=ALU.is_ge),
                  reads=["affT", "mid"], writes=["cmp"])
            P.add("dve", lambda e: e.tensor_reduce(out=cnt[:], in_=cmp_[:].rearrange("p (t e) -> p e t", e=NE), axis=AX.X, op=ALU.add), reads=["cmp"], writes=["cnt"])
            P.add("pe", lambda e: e.matmul(PQ[0][:, 0:NE], lhsT=onesF, rhs=cnt[:], start=True, stop=True), reads=["cnt", "csb"], writes=["PQ0"])
            P.add("dve", lambda e: e.tensor_scalar(out=ge[:], in0=PQ[0][:, 0:NE], scalar1=float(CAP) - 0.5, scalar2=None, op0=ALU.is_ge), reads=["PQ0"], writes=["ge"])
            P.add("dve", lambda e: e.tensor_tensor(out=d1[:], in0=mid[:], in1=lo[:], op=ALU.subtract), reads=["mid", "lo"], writes=["d1"])
            P.add("dve", lambda e: e.tensor_tensor(out=d2[:], in0=hi_[:], in1=mid[:], op=ALU.subtract), reads=["mid", "hi"], writes=["d2"])
            P.add("dve", lambda e: e.tensor_tensor(out=d1[:], in0=d1[:], in1=ge[:], op=ALU.mult), reads=["d1", "ge"], writes=["d1"])
            P.add("dve", lambda e: e.tensor_tensor(out=d2[:], in0=d2[:], in1=ge[:], op=ALU.mult), reads=["d2", "ge"], writes=["d2"])
            P.add("dve", lambda e: e.tensor_tensor(out=lo[:], in0=lo[:], in1=d1[:], op=ALU.add), reads=["lo", "d1"], writes=["lo"])
            P.add("dve", lambda e: e.tensor_tensor(out=hi_[:], in0=mid[:], in1=d2[:], op=ALU.add), reads=["mid", "d2"], writes=["hi"])
        P.add("dve", lambda e: e.tensor_tensor(out=a3(cmp_), in0=a3(affT), in1=lo[:].unsqueeze(1).to_broadcast([128, NT, NE]), op=ALU.is_ge),
              reads=["affT", "lo"], writes=["cmp"])
        P.add("dve", lambda e: e.tensor_copy(out=maskb_[:], in_=cmp_[:]), reads=["cmp"], writes=["maskb"])
        for c0 in range(0, NTE, 512):
            c1 = min(NTE, c0 + 512)
            w = c1 - c0
            P.add("pe", lambda e, c0=c0, c1=c1, w=w: e.matmul(PQ[0][:, 0:w], lhsT=triS_b[:], rhs=maskb_[:, c0:c1], start=True, stop=True),
                  reads=["maskb", "triS_b"], writes=["PQ0"])
            P.add("pe", lambda e, c0=c0, c1=c1, w=w: e.matmul(PQ[1][:, 0:w], lhsT=ones_b[:], rhs=maskb_[:, c0:c1], start=True, stop=True),
                  reads=["maskb", "ones_b"], writes=["PQ1"])
            P.add("act", lambda e, c0=c0, c1=c1, w=w: e.activation(out=wn[:, c0:c1], in_=PQ[0][:, 0:w], func=AF.Copy), reads=["PQ0"], writes=["wn"])
            P.add("act", lambda e, c0=c0, c1=c1, w=w: e.activation(out=tots[:, c0:c1], in_=PQ[1][:, 0:w], func=AF.Copy), reads=["PQ1"], writes=["tots"])
        P.add("dve", lambda e: e.memset(offs[:, 0:NE], 0.0), writes=["offs"])
        for t in range(1, NT):
            P.add("dve", lambda e, t=t: e.tensor_tensor(out=offs[:, t * NE:(t + 1) * NE], in0=offs[:, (t - 1) * NE:t * NE], in1=tots[:, (t - 1) * NE:t * NE], op=ALU.add),
                  reads=["offs", "tots"], writes=["offs"])
        P.add("dve", lambda e: e.tensor_tensor(out=pos[:], in0=wn[:], in1=offs[:], op=ALU.add), reads=["wn", "offs"], writes=["pos"])
        P.add("dve", lambda e: e.tensor_scalar(out=sel[:], in0=pos[:], scalar1=float(CAP) - 0.5, scalar2=None, op0=ALU.is_lt), reads=["pos"], writes=["sel"])
        P.add("dve", lambda e: e.tensor_tensor(out=sel[:], in0=sel[:], in1=cmp_[:], op=ALU.mult), reads=["sel", "cmp"], writes=["sel"])
        P.add("dve", lambda e: e.tensor_scalar(out=pos[:], in0=pos[:], scalar1=csb[:, IOTA + 5:IOTA + 6], scalar2=None, op0=ALU.subtract), reads=["pos", "csb"], writes=["pos"])
        P.add("dve", lambda e: e.tensor_tensor(out=pos[:], in0=pos[:], in1=sel[:], op=ALU.mult), reads=["pos", "sel"], writes=["pos"])
        P.add("dve", lambda e: e.tensor_scalar(out=pos[:], in0=pos[:], scalar1=csb[:, IOTA + 5:IOTA + 6], scalar2=None, op0=ALU.add), reads=["pos", "csb"], writes=["pos"])
        P.add("dve", lambda e: e.tensor_copy(out=idxT[:], in_=pos[:]), reads=["pos"], writes=["idxT"])
        if debug:
            P.add("sp", lambda e: e.dma_start(out=dbg_aff[:, :], in_=affT[:]), reads=["affT"], writes=["dbg1"], dma=True)
            P.add("sp", lambda e: e.dma_start(out=dbg_idx[:, :], in_=idxT[:]), reads=["idxT"], writes=["dbg2"], dma=True)
        for lt in range(NT):
            b = lt % 2
            P.add("sp", lambda e, lt=lt, b=b: e.dma_start(out=hrow[b][:], in_=H2[lt, :, :]), reads=["DR_h2"], writes=["hrow%d" % b], dma=True)
            for ex_ in range(NE):
                P.add("pool", lambda e, lt=lt, b=b, ex_=ex_: e.indirect_dma_start(
                    out=XE[ex_][:, :], out_offset=bass.IndirectOffsetOnAxis(ap=idxT[:, lt * NE + ex_: lt * NE + ex_ + 1], axis=0),
                    in_=hrow[b][:], in_offset=None), reads=["hrow%d" % b, "idxT"], writes=["XE%d" % ex_], dma=True)
        P.barrier()

    with ExitStack() as ph:
        def sbp(name, shape, dt=F32):
            return ph.enter_context(nc.sbuf_tensor(name, list(shape), dt))
        wg = sbp("wg", [128, 8, FF], BF16)
        wu = sbp("wu", [128, 8, FF], BF16)
        wd = sbp("wd", [128, NF, D], BF16)
        xrow = [sbp("xrow%d" % i, [128, RW], BF16) for i in range(2)]
        xeT = sbp("xeT", [128, 8, CAP], BF16)
        gateT = sbp("gateT", [128, NST])
        tokI = sbp("tokI", [128, NST], I32)
        sgx = sbp("sgx", [128, SC])
        hidT = sbp("hidT", [128, NF, SC], BF16)
        ysb = [sbp("ysb%d" % i, [128, D]) for i in range(2)]
        yi = 0
        for ex_ in range(NE):
            for k in range(8):
                P.add("pool", lambda e, ex_=ex_, k=k: e.dma_start(out=wg[:, k, :], in_=w_eg[ex_, k * 128:(k + 1) * 128, :]), writes=["wg"], dma=True)
                P.add("pool", lambda e, ex_=ex_, k=k: e.dma_start(out=wu[:, k, :], in_=w_eu[ex_, k * 128:(k + 1) * 128, :]), writes=["wu"], dma=True)
            for f in range(NF):
                P.add("pool", lambda e, ex_=ex_, f=f: e.dma_start(out=wd[:, f, :], in_=w_ed[ex_, f * 128:(f + 1) * 128, :]), writes=["wd"], dma=True)
            for s_ in range(NST):
                b = s_ % 2
                P.add("sp", lambda e, ex_=ex_, s_=s_, b=b: e.dma_start(out=xrow[b][:], in_=XE[ex_][s_ * 128:(s_ + 1) * 128, :]),
                      reads=["XE%d" % ex_], writes=["xrow%d" % b], dma=True)
                for k in range(8):
                    P.add("pe", lambda e, k=k, b=b: e.transpose(out=PA[:, k * 128:(k + 1) * 128], in_=xrow[b][:, k * 128:(k + 1) * 128], identity=ident_b[:]),
                          reads=["xrow%d" % b, "ident_b"], writes=["PA"])
                P.add("act", lambda e, s_=s_: e.activation(out=xeT[:, :, s_ * 128:(s_ + 1) * 128], in_=PA[:].rearrange("p (k c) -> p k c", k=8), func=AF.Copy),
                      reads=["PA"], writes=["xeT"])
                mt = lambda b=b: xrow[b][:, D:RW].bitcast(F32)
                P.add("dve", lambda e, s_=s_, mt=mt, ex_=ex_: e.tensor_copy(out=gateT[:, s_:s_ + 1], in_=mt()[:, ex_:ex_ + 1]), reads=["xrow%d" % b], writes=["gateT"])
                P.add("dve", lambda e, s_=s_, mt=mt: e.tensor_copy(out=tokI[:, s_:s_ + 1], in_=mt()[:, NE:NE + 1]), reads=["xrow%d" % b], writes=["tokI"])
            for sc_ in range(NSC):
                ss_ = slice(sc_ * SC, (sc_ + 1) * SC)
                for f in range(NF):
                    pg = f % 2
                    for k in range(8):
                        P.add("pe", lambda e, k=k, f=f, pg=pg, ss_=ss_: e.matmul(PQ[pg][:, 0:SC], lhsT=wg[:, k, f * 128:(f + 1) * 128], rhs=xeT[:, k, ss_],
                                                                               start=(k == 0), stop=(k == 7)), reads=["wg", "xeT"], writes=["PQ%d" % pg])
                    for k in range(8):
                        P.add("pe", lambda e, k=k, f=f, pg=pg, ss_=ss_: e.matmul(PQ[2 + pg][:, 0:SC], lhsT=wu[:, k, f * 128:(f + 1) * 128], rhs=xeT[:, k, ss_],
                                                                               start=(k == 0), stop=(k == 7)), reads=["wu", "xeT"], writes=["PQ%d" % (2 + pg)])
                    P.add("act", lambda e, pg=pg: e.activation(out=sgx[:], in_=PQ[pg][:, 0:SC], func=AF.Silu), reads=["PQ%d" % pg], writes=["sgx"])
                    P.add("dve", lambda e, pg=pg, f=f: e.tensor_tensor(out=hidT[:, f, :], in0=sgx[:], in1=PQ[2 + pg][:, 0:SC], op=ALU.mult),
                          reads=["sgx", "PQ%d" % (2 + pg)], writes=["hidT"])
                for sl in range(SC // 128):
                    s_ = sc_ * (SC // 128) + sl
                    yb_ = yi % 2
                    yi += 1
                    for nch in range(2):
                        pp = "pm%d" % nch
                        for f in range(NF):
                            P.add("pe", lambda e, f=f, sl=sl, nch=nch: e.matmul(pm[nch][:], lhsT=hidT[:, f, sl * 128:(sl + 1) * 128], rhs=wd[:, f, nch * 512:(nch + 1) * 512],
                                                                               start=(f == 0), stop=(f == NF - 1)), reads=["hidT", "wd"], writes=[pp])
                        cs = slice(nch * 512, (nch + 1) * 512)
                        P.add("dve", lambda e, nch=nch, cs=cs, s_=s_, yb_=yb_: e.scalar_tensor_tensor(out=ysb[yb_][:, cs], in0=pm[nch][:], scalar=gateT[:, s_:s_ + 1], in1=M5[:, cs],
                                                                                                    op0=ALU.mult, op1=ALU.mult),
                              reads=[pp, "gateT", "M5"], writes=["ysb%d" % yb_])
                    P.add("pool", lambda e, s_=s_, yb_=yb_: e.indirect_dma_start(
                        out=X1[:, :], out_offset=bass.IndirectOffsetOnAxis(ap=tokI[:, s_:s_ + 1], axis=0),
                        in_=ysb[yb_][:], in_offset=None, compute_op=ALU.add), reads=["ysb%d" % yb_, "tokI", "DR_x1"], writes=["DR_x1"], dma=True)
        P.barrier()

    with ExitStack() as ph:
        def sbp(name, shape, dt=F32):
            return ph.enter_context(nc.sbuf_tensor(name, list(shape), dt))
        fnw = sbp("fnw", [128, D])
        xf = [sbp("xf%d" % i, [128, D]) for i in range(2)]
        of_ = [sbp("of%d" % i, [128, D]) for i in range(2)]
        junk3 = sbp("junk3", [128, D], BF16)
        s5 = sbp("s5", [128, 4])
        P.add("sp", lambda e: e.dma_start(out=fnw[:], in_=fn_bc[:, :]), writes=["fnw"], dma=True)
        for lt in range(NT):
            b = lt % 2
            P.add("sp", lambda e, lt=lt, b=b: e.dma_start(out=xf[b][:], in_=X1[lt * 128:(lt + 1) * 128, :]), reads=["DR_x1"], writes=["xf%d" % b], dma=True)
            P.add("act", lambda e, b=b: e.activation(out=junk3[:], in_=xf[b][:], func=AF.Square, accum_out=s5[:, 0:1]), reads=["xf%d" % b], writes=["junk3", "s5a"])
            P.add("act", lambda e: e.activation(out=s5[:, 1:2], in_=s5[:, 0:1], func=AF.Sqrt, scale=1.0 / D, bias=csb[:, IOTA + 7:IOTA + 8]), reads=["s5a", "csb"], writes=["s5b"])
            P.add("dve", lambda e: e.reciprocal(out=s5[:, 2:3], in_=s5[:, 1:2]), reads=["s5b"], writes=["s5c"])
            P.add("dve", lambda e, b=b: e.scalar_tensor_tensor(out=of_[b][:], in0=xf[b][:], scalar=s5[:, 2:3], in1=fnw[:], op0=ALU.mult, op1=ALU.mult),
                  reads=["xf%d" % b, "s5c", "fnw"], writes=["of%d" % b])
            P.add("sp", lambda e, lt=lt, b=b: e.dma_start(out=out[lt * 128:(lt + 1) * 128, :], in_=of_[b][:]), reads=["of%d" % b], writes=["OUT"], dma=True)

    P.emit()
    es.close()
    return nc


def _consts(T):
    CAP = 2 * T // NE
    c = np.zeros((128, 1024), np.float32)
    p = np.arange(128)
    c[:, 0:128] = np.eye(128)
    c[:, 128:256] = (p[:, None] <= p[None, :])
    c[:, 256:384] = (p[:, None] >= p[None, :])
    c[:, 384:512] = (p[:, None] < p[None, :])
    c[:, 512:640] = 1.0
    c[:, 640] = p
    c[:, 641] = p + 1
    c[:, 642] = 128 - p
    c[:, 643] = -(p + 1)
    c[:, 644] = -(128 - p)
    c[:, 645] = CAP + p
    c[:, 646] = math.log(128.0 ** -0.5)
    c[:, 647] = EPS
    return c


def _rope_table(T):
    rows = T // 64
    row = np.repeat(np.arange(rows), 64).astype(np.float32)
    col = np.tile(np.arange(64), rows).astype(np.float32)
    n_freq = 32
    inv = (np.float32(10000.0) ** (-np.arange(n_freq, dtype=np.float32) / n_freq)).astype(np.float32)
    ang = np.concatenate([row[:, None] * inv, col[:, None] * inv], axis=-1).astype(np.float32)
    tab = np.zeros((CTX + T, 128), np.float32)
    tab[:CTX, 0:64] = 1.0
    tab[CTX:, 0:64] = np.cos(ang)
    tab[CTX:, 64:128] = np.sin(ang)
    return tab


def make_inputs(inp, b, T):
    f = lambda a: np.ascontiguousarray(np.asarray(a, dtype=np.float32))
    x = f(inp["x"])[b, :T]
    ctx = f(inp["ctx"])[b]
    c = f(inp["c"])[b]
    cc = f(inp["c_ctx"])
    bc = lambda v: np.ascontiguousarray(np.broadcast_to(np.asarray(v, np.float32).reshape(1, -1), (128, np.asarray(v).size)))
    w_in = f(inp["w_in"])[0]
    w_in_r = np.concatenate([w_in[:, 0:512], w_in[:, 512:1024], w_in[:, 1056:1568], w_in[:, 1568:2080], w_in[:, 2080:2592],
                             w_in[:, 2592:3104], w_in[:, 3104:3616], w_in[:, 1024:1056]], axis=1)
    gw = f(inp["gla_gate_w"])[0]
    gb = f(inp["gla_gate_b"])[0]
    gwaug = np.zeros((33, 512), np.float32)
    gwaug[0:16, 0:256] = gw[0]
    gwaug[16:32, 256:512] = gw[1]
    gwaug[32, 0:256] = gb[0]
    gwaug[32, 256:512] = gb[1]
    scT = np.zeros((128, 16), np.float32)
    scT[:, 0:8] = c.reshape(8, 128).T
    scT[:, 8:16] = cc.reshape(8, 128).T
    return {
        "xin": np.ascontiguousarray(np.concatenate([ctx, x], axis=0)),
        "rope": _rope_table(T),
        "scT": scT,
        "w_ada": f(inp["w_ada"])[0],
        "b_ada_bc": bc(f(inp["b_ada"])[0]),
        "n1_bc": bc(f(inp["norm1_w"])[0]),
        "n2_bc": bc(f(inp["norm2_w"])[0]),
        "fn_bc": bc(f(inp["final_norm_w"])),
        "hn_bc": bc(np.concatenate([f(inp["gla_norm_w"])[0], f(inp["ret_norm_w"])[0]])),
        "w_in": np.ascontiguousarray(w_in_r),
        "gwaug": gwaug,
        "rdl_bc": bc(f(inp["ret_decay_logit"])[0].reshape(-1)),
        "w_out": f(inp["w_out"])[0],
        "w_router": f(inp["w_router"])[0],
        "w_eg": f(inp["w_exp_gate"])[0],
        "w_eu": f(inp["w_exp_up"])[0],
        "w_ed": f(inp["w_exp_down"])[0],
        "cst": _consts(T),
    }


def kernel(**inputs):
    T = 16384
    B = 2
    nc = build_program(T)
    in_maps = [make_inputs(inputs, b, T) for b in range(B)]
    res = run_bass_kernel_spmd(nc, in_maps, core_ids=list(range(B)))
    return np.stack([np.asarray(res.results[b]["out"], dtype=np.float32).reshape(T, D) for b in range(B)], axis=0)
```

```python
import math
from contextlib import ExitStack

import numpy as np
import ml_dtypes
import concourse.bass as bass
import concourse.mybir as mybir
from concourse.bass_utils import run_bass_kernel_spmd

F32 = mybir.dt.float32
BF16 = mybir.dt.bfloat16
I32 = mybir.dt.int32
ALU = mybir.AluOpType
AF = mybir.ActivationFunctionType
AX = mybir.AxisListType

D = 1024
NE = 16
FF = 1408
NF = FF // 128
INW = 3616
EPS = 1e-6
CTX = 256
RW = 1024 + 52
MG = 1024
MHI = 1024 + 48
MLO = 1024 + 49

ENGS = ["pe", "act", "dve", "pool", "sp"]
KDMA = 8
BLK = 8192


class Op:
    __slots__ = ("eng", "fn", "dma", "waits", "sem", "val")


class Prog:
    def __init__(self, nc, es):
        self.nc = nc
        self.es = es
        self.ops = {e: [] for e in ENGS}
        self.last_w = {}
        self.readers = {}
        self.ncomp = {e: 0 for e in ENGS}
        self.ndma = {e: 0 for e in ENGS}
        self.waited = {e: {} for e in ENGS}
        self.pending = {e: [] for e in ENGS}
        self.csem = {}
        self.dsem = {}
        self.nsem = 0

    def _sem(self, name):
        self.nsem += 1
        return self.es.enter_context(self.nc.semaphore(name))

    def _csem(self, eng, i):
        k = (eng, i // BLK)
        if k not in self.csem:
            self.csem[k] = self._sem("c_%s_%d" % k)
        return self.csem[k], i % BLK + 1

    def _dsem(self, eng, i):
        k = (eng, i % KDMA)
        if k not in self.dsem:
            self.dsem[k] = self._sem("d_%s_%d" % k)
        return self.dsem[k], 16 * (i // KDMA + 1)

    def _want(self, op, sem, val):
        w = self.waited[op.eng]
        key = id(sem)
        if w.get(key, 0) >= val:
            return
        w[key] = val
        op.waits.append((sem, val))

    def add(self, eng, fn, reads=(), writes=(), dma=False):
        if getattr(self, "muted", False):
            return Op()
        op = Op()
        op.eng, op.fn, op.dma, op.waits = eng, fn, dma, []
        for sem, val in self.pending[eng]:
            self._want(op, sem, val)
        self.pending[eng] = []
        deps = []
        for k in reads:
            w = self.last_w.get(k)
            if w is not None:
                deps.append(w)
        for k in writes:
            w = self.last_w.get(k)
            if w is not None:
                deps.append(w)
            deps.extend(self.readers.get(k, ()))
        for d in deps:
            if d.eng == eng and eng == "pe" and not d.dma and not dma:
                continue
            self._want(op, d.sem, d.val)
        if dma:
            i = self.ndma[eng]
            op.sem, op.val = self._dsem(eng, i)
            if i >= KDMA:
                self._want(op, op.sem, op.val - 16)
            self.ndma[eng] = i + 1
        else:
            i = self.ncomp[eng]
            op.sem, op.val = self._csem(eng, i)
            self.ncomp[eng] = i + 1
        for k in writes:
            self.last_w[k] = op
            self.readers[k] = []
        for k in reads:
            self.readers.setdefault(k, []).append(op)
        self.ops[eng].append(op)
        return op

    def add_cc(self, fn, reads=(), writes=()):
        if getattr(self, "muted", False):
            return Op()
        op = self.add("pool", fn, reads, writes)
        self.ncomp["pool"] -= 1
        op.sem, op.val = self._sem("cc%d" % self.nsem), 1
        op.dma = None
        self.cc_ops = getattr(self, "cc_ops", []) + [op]
        return op

    def _latest(self):
        out = []
        for e in ENGS:
            n = self.ncomp[e]
            if n:
                out.append(self._csem(e, n - 1))
            nd = self.ndma[e]
            for j in range(max(0, nd - KDMA), nd):
                out.append(self._dsem(e, j))
        for op in getattr(self, "cc_ops", []):
            out.append((op.sem, op.val))
        return out

    def barrier(self):
        if getattr(self, "muted", False):
            return
        lat = self._latest()
        for e in ENGS:
            self.pending[e] = list(lat)

    def emit(self):
        final = self._latest()
        with self.nc.Block() as block:
            def run(ename):
                def body(e):
                    for op in self.ops[ename]:
                        for sem, val in op.waits:
                            e.wait_ge(sem, val)
                        ins = op.fn(e)
                        if op.dma is None:
                            ins.then_inc(op.sem)
                        else:
                            ins.then_inc(op.sem, 16 if op.dma else 1)
                    if ename == "sp":
                        for sem, val in final:
                            e.wait_ge(sem, val)
                return body
            block.tensor(run("pe"))
            block.scalar(run("act"))
            block.vector(run("dve"))
            block.gpsimd(run("pool"))
            block.sync(run("sp"))


class _Stop(Exception):
    pass


def build_program(T, debug=False, stop_after=99.0):
    NT = T // 128
    NTT = NT + 2
    NTO = NT // 4
    TQ = T // 4
    CAP = 2 * T // NE
    CAPL = max(128, ((3 * CAP // 8) + 127) // 128 * 128)
    NST = CAPL // 128
    SC = 384 if CAPL % 384 == 0 else 128
    NSC = CAPL // SC
    WC = 928
    nc = bass.Bass("TRN2", target_bir_lowering=False)
    es = ExitStack()
    P = Prog(nc, es)

    def din(name, shape, dt=F32):
        return nc.dram_tensor(name, list(shape), dt, kind="ExternalInput").ap()

    def dscr(name, shape, dt, dump=False, **kw):
        if debug and dump:
            return nc.dram_tensor(name, list(shape), dt, kind="ExternalOutput").ap()
        return nc.dram_tensor(name, list(shape), dt, **kw).ap()

    def sb(name, shape, dt=F32):
        return es.enter_context(nc.sbuf_tensor(name, list(shape), dt))

    def ps(name, shape, dt=F32):
        return es.enter_context(nc.psum_tensor(name, list(shape), dt))

    xin = din("xin", [NTT * 128, D])
    xrot = din("xrot", [T, D])
    rope = din("rope", [NTT * 128, 128])
    scT = din("scT", [128, 16])
    w_ada = din("w_ada", [D, 6 * D])
    b_ada_bc = din("b_ada_bc", [128, 6 * D])
    n1_bc = din("n1_bc", [128, D])
    n2_bc = din("n2_bc", [128, D])
    fn_bc = din("fn_bc", [128, D])
    hn_bc = din("hn_bc", [128, 256])
    w_in = din("w_in", [D, WC])
    gwaug = din("gwaug", [33, 128])
    rdl_bc = din("rdl_bc", [128, 2])
    w_out = din("w_out", [D, D])
    w_router = din("w_router", [D, NE])
    w_eg = din("w_eg", [NE, D, FF])
    w_eu = din("w_eu", [NE, D, FF])
    w_ed = din("w_ed", [NE, FF, D])
    cst = din("cst", [128, 1024])
    yidx_d = din("yidx", [128, NT * 4], I32)
    out = nc.dram_tensor("out", [TQ, D], F32, kind="ExternalOutput").ap()

    QKT = dscr("QKT", [NTT, 128, 1024], BF16)
    KK = dscr("KK", [NTT, 128, 512], BF16)
    VV = dscr("VV", [NTT, 128, 256], BF16)
    SG = dscr("SG", [NT, 128, 256], BF16)
    OF = dscr("OF", [NT, 128, 256], F32)
    YL = dscr("YL", [T, 256], BF16)
    YA = dscr("YA", [8 * T, 256], BF16, addr_space="Shared")
    X1 = dscr("X1", [TQ + 128, D], F32, True)
    H2 = dscr("H2", [NTO, 128, RW], BF16)
    XE = [dscr("XE%d" % e, [CAPL + 128, RW], BF16) for e in range(NE)]
    dbg_aff = dscr("dbg_aff", [128, NT * NE], F32, True) if debug else None
    dbg_idx = dscr("dbg_idx", [128, NTO * NE], I32, True) if debug else None

    csb = sb("csb", [128, 1024])
    ident_b = sb("ident_b", [128, 128], BF16)
    triF_b = sb("triF_b", [128, 128], BF16)
    triB_b = sb("triB_b", [128, 128], BF16)
    triS_b = sb("triS_b", [128, 128], BF16)
    ones_b = sb("ones_b", [128, 128], BF16)
    M5 = sb("M5", [128, D])
    ET = sb("ET", [128, NTT * 2])
    ER = sb("ER", [128, 2])
    DQ = sb("DQ", [128, 2])
    DK = sb("DK", [128, 2])
    identF = csb[:, 0:128]
    triF = csb[:, 128:256]
    triB = csb[:, 256:384]
    triS = csb[:, 384:512]
    onesF = csb[:, 512:640]
    IOTA = 640
    EPSC = csb[:, IOTA + 7:IOTA + 8]

    es03 = ExitStack()
    es01 = ExitStack()
    A2 = es03.enter_context(nc.sbuf_tensor("A2", [128, D], F32))
    B2 = es03.enter_context(nc.sbuf_tensor("B2", [128, D], F32))
    M2 = es03.enter_context(nc.sbuf_tensor("M2", [128, D], F32))
    affT = es03.enter_context(nc.sbuf_tensor("affT", [128, NT * NE], F32))
    idxT = es03.enter_context(nc.sbuf_tensor("idxT", [128, NTO * NE], I32))
    yidx = es03.enter_context(nc.sbuf_tensor("yidx_s", [128, NT * 4], I32))
    xinit = es03.enter_context(nc.sbuf_tensor("xinit", [128, RW], BF16))
    A1 = [es01.enter_context(nc.sbuf_tensor("A1_%d" % i, [128, D], F32)) for i in range(2)]
    B1 = [es01.enter_context(nc.sbuf_tensor("B1_%d" % i, [128, D], F32)) for i in range(2)]

    def chk(n):
        if n > stop_after:
            P.muted = True

    P.add("sp", lambda e: e.dma_start(out=csb[:], in_=cst[:, :]), writes=["csb"], dma=True)
    P.add("sp", lambda e: e.dma_start(out=yidx[:], in_=yidx_d[:, :]), writes=["yidx"], dma=True)
    for nm, dst, src in (("ident_b", ident_b, identF), ("triF_b", triF_b, triF), ("triB_b", triB_b, triB),
                         ("triS_b", triS_b, triS), ("ones_b", ones_b, onesF)):
        P.add("dve", lambda e, dst=dst, src=src: e.tensor_copy(out=dst[:], in_=src), reads=["csb"], writes=[nm])
    P.add("dve", lambda e: e.memset(xinit[:], 0.0), writes=["xinit"])
    P.add("dve", lambda e: e.memset(xinit[:, MHI:MHI + 1], float(NTO)), reads=["xinit"], writes=["xinit"])
    P.add("dve", lambda e: e.tensor_copy(out=xinit[:, MLO:MLO + 1], in_=csb[:, IOTA:IOTA + 1]), reads=["xinit", "csb"], writes=["xinit"])
    P.add("pool", lambda e: e.dma_start(out=X1[TQ:TQ + 128, :], in_=xinit[:, 0:D]), reads=["xinit"], writes=["DR_x1"], dma=True)
    for ex_ in range(NE):
        for s_ in range(NST):
            P.add("sp", lambda e, ex_=ex_, s_=s_: e.dma_start(out=XE[ex_][s_ * 128:(s_ + 1) * 128, :], in_=xinit[:]),
                  reads=["xinit"], writes=["XE%d" % ex_], dma=True)

    pm = [es.enter_context(nc.psum_tensor("pm%d" % i, [128, 512], F32)) for i in range(2)]

    with ExitStack() as ph:
        def sbp(name, shape, dt=F32):
            return ph.enter_context(nc.sbuf_tensor(name, list(shape), dt))
        sc = sbp("sc", [128, 16])
        scs = sbp("scs", [128, 16])
        scbc = sbp("scbc", [128, 16, 128])
        wab = [sbp("wab%d" % i, [128, 8, 512]) for i in range(2)]
        bab = [sbp("bab%d" % i, [128, 512]) for i in range(2)]
        nw1 = sbp("nw1", [128, D])
        nw2 = sbp("nw2", [128, D])
        modt = [sbp("modt%d" % i, [128, 512]) for i in range(2)]
        rd = sbp("rd", [128, 2])
        rd2 = sbp("rd2", [128, 2])
        lg = sbp("lg", [128, 2])

        P.add("sp", lambda e: e.dma_start(out=sc[:], in_=scT[:, :]), writes=["sc"], dma=True)
        P.add("sp", lambda e: e.dma_start(out=nw1[:], in_=n1_bc[:, :]), writes=["nw1"], dma=True)
        P.add("sp", lambda e: e.dma_start(out=nw2[:], in_=n2_bc[:, :]), writes=["nw2"], dma=True)
        P.add("sp", lambda e: e.dma_start(out=rd[:], in_=rdl_bc[:, :]), writes=["rd"], dma=True)
        P.add("act", lambda e: e.activation(out=scs[:], in_=sc[:], func=AF.Silu), reads=["sc"], writes=["scs"])
        P.add("dve", lambda e: e.tensor_copy(out=scbc[:], in_=scs[:].unsqueeze(2).to_broadcast([128, 16, 128])),
              reads=["scs"], writes=["scbc"])
        P.add("act", lambda e: e.activation(out=rd2[:], in_=rd[:], func=AF.Exp, scale=-1.0), reads=["rd"], writes=["rd2"])
        P.add("act", lambda e: e.activation(out=lg[:], in_=rd2[:], func=AF.Ln, bias=1.0), reads=["rd2"], writes=["lg"])
        for d in range(2):
            cq = IOTA + 3 + d
            ck = IOTA + 1 + d
            P.add("act", lambda e, d=d, cq=cq: e.activation(out=DQ[:, d:d + 1], in_=lg[:, d:d + 1], func=AF.Exp, scale=csb[:, cq:cq + 1]),
                  reads=["lg", "csb"], writes=["DQ%d" % d])
            P.add("act", lambda e, d=d, ck=ck: e.activation(out=DK[:, d:d + 1], in_=lg[:, d:d + 1], func=AF.Exp, scale=csb[:, ck:ck + 1],
                                                           bias=csb[:, IOTA + 6:IOTA + 7]), reads=["lg", "csb"], writes=["DK%d" % d])
        P.add("act", lambda e: e.activation(out=ER[:], in_=lg[:], func=AF.Exp, scale=-128.0), reads=["lg"], writes=["ER"])

        jobs = [(n, 0) for n in range(12)] + [(n, 1) for n in range(4)]
        for ji, (n, which) in enumerate(jobs):
            bi = ji % 2
            for k in range(8):
                P.add("sp", lambda e, n=n, k=k, bi=bi: e.dma_start(out=wab[bi][:, k, :], in_=w_ada[k * 128:(k + 1) * 128, n * 512:(n + 1) * 512]),
                      writes=["wab%d" % bi], dma=True)
            P.add("sp", lambda e, n=n, bi=bi: e.dma_start(out=bab[bi][:], in_=b_ada_bc[:, n * 512:(n + 1) * 512]),
                  writes=["bab%d" % bi], dma=True)
            for k in range(8):
                P.add("pe", lambda e, k=k, bi=bi, which=which: e.matmul(pm[bi][:], lhsT=scbc[:, which * 8 + k, :], rhs=wab[bi][:, k, :],
                                                                        start=(k == 0), stop=(k == 7)),
                      reads=["scbc", "wab%d" % bi], writes=["pm%d" % bi])
            P.add("dve", lambda e, bi=bi: e.tensor_tensor(out=modt[bi][:], in0=pm[bi][:], in1=bab[bi][:], op=ALU.add),
                  reads=["pm%d" % bi, "bab%d" % bi], writes=["modt%d" % bi])
            m, half = n // 2, n % 2
            cs = slice(half * 512, (half + 1) * 512)
            if m == 0:
                P.add("dve", lambda e, bi=bi, cs=cs, which=which: e.tensor_copy(out=B1[which][:, cs], in_=modt[bi][:]),
                      reads=["modt%d" % bi], writes=["B1_%d" % which])
            elif m == 1:
                P.add("dve", lambda e, bi=bi, cs=cs, which=which: e.scalar_tensor_tensor(out=A1[which][:, cs], in0=modt[bi][:], scalar=1.0, in1=nw1[:, cs],
                                                                                         op0=ALU.add, op1=ALU.mult),
                      reads=["modt%d" % bi, "nw1"], writes=["A1_%d" % which])
            elif m == 2:
                P.add("dve", lambda e, bi=bi, cs=cs: e.tensor_copy(out=M2[:, cs], in_=modt[bi][:]), reads=["modt%d" % bi], writes=["M2"])
            elif m == 3:
                P.add("dve", lambda e, bi=bi, cs=cs: e.tensor_copy(out=B2[:, cs], in_=modt[bi][:]), reads=["modt%d" % bi], writes=["B2"])
            elif m == 4:
                P.add("dve", lambda e, bi=bi, cs=cs: e.scalar_tensor_tensor(out=A2[:, cs], in0=modt[bi][:], scalar=1.0, in1=nw2[:, cs],
                                                                            op0=ALU.add, op1=ALU.mult),
                      reads=["modt%d" % bi, "nw2"], writes=["A2"])
            else:
                P.add("dve", lambda e, bi=bi, cs=cs: e.tensor_copy(out=M5[:, cs], in_=modt[bi][:]), reads=["modt%d" % bi], writes=["M5"])
        P.barrier()

    chk(1)
    PA = ps("PA", [128, 1024], BF16)
    PQ = [ps("PQ%d" % i, [128, 512]) for i in range(5)]

    with ExitStack() as ph:
        def sbp(name, shape, dt=F32):
            return ph.enter_context(nc.sbuf_tensor(name, list(shape), dt))
        win = sbp("win", [128, 8, WC], BF16)
        gw = sbp("gw", [33, 128], BF16)
        gzaug = sbp("gzaug", [33, 128], BF16)
        xt = [sbp("xt%d" % i, [128, D]) for i in range(2)]
        rp = [sbp("rp%d" % i, [128, 128]) for i in range(2)]
        junk = sbp("junk", [128, D], BF16)
        ss = sbp("ss", [128, 4])
        htmp = sbp("htmp", [128, D])
        hb = sbp("hb", [128, D], BF16)
        hT = sbp("hT", [128, D], BF16)
        t1 = sbp("t1", [128, 128])
        spl = sbp("spl", [128, 192])
        epos = sbp("epos", [128, 128])
        eneg = sbp("eneg", [128, 128])
        rr = sbp("rr", [128, 256])
        ra = sbp("ra", [128, 128])
        rb = sbp("rb", [128, 128])
        qtok = [sbp("qtok%d" % i, [128, 512], BF16) for i in range(2)]
        ktok = [sbp("ktok%d" % i, [128, 512], BF16) for i in range(2)]
        qkts = [sbp("qkts%d" % i, [128, 1024], BF16) for i in range(2)]
        vvt = [sbp("vvt%d" % i, [128, 256], BF16) for i in range(2)]
        sgt = [sbp("sgt%d" % i, [128, 256], BF16) for i in range(2)]

        for k in range(8):
            P.add("pool", lambda e, k=k: e.dma_start(out=win[:, k, :], in_=w_in[k * 128:(k + 1) * 128, :]), writes=["win"], dma=True)
        P.add("pool", lambda e: e.dma_start(out=gw[:], in_=gwaug[:, :]), writes=["gw"], dma=True)
        P.add("dve", lambda e: e.memset(gzaug[:], 1.0), writes=["gzaug"])
        P.add("dve", lambda e: e.memset(spl[:], 0.0), writes=["spl"])
        for i in range(2):
            P.add("dve", lambda e, i=i: e.memset(qtok[i][:], 0.0), writes=["qtok%d" % i])
            P.add("dve", lambda e, i=i: e.memset(ktok[i][:], 0.0), writes=["ktok%d" % i])

        for tt in range(NTT):
            b = tt % 2
            isctx = tt < 2
            ci = 1 if isctx else 0
            X = "xt%d" % b
            QN, KN = "qtok%d" % b, "ktok%d" % b
            P.add("sp", lambda e, tt=tt, b=b: e.dma_start(out=xt[b][:], in_=xin[tt * 128:(tt + 1) * 128, :]), writes=[X], dma=True)
            P.add("sp", lambda e, tt=tt, b=b: e.dma_start(out=rp[b][:], in_=rope[tt * 128:(tt + 1) * 128, :]), writes=["rp%d" % b], dma=True)
            P.add("act", lambda e, b=b: e.activation(out=junk[:], in_=xt[b][:], func=AF.Square, accum_out=ss[:, 0:1]),
                  reads=[X], writes=["junk", "ss0"])
            P.add("act", lambda e: e.activation(out=ss[:, 1:2], in_=ss[:, 0:1], func=AF.Sqrt, scale=1.0 / D, bias=EPSC),
                  reads=["ss0", "csb"], writes=["ss1"])
            P.add("dve", lambda e: e.reciprocal(out=ss[:, 2:3], in_=ss[:, 1:2]), reads=["ss1"], writes=["ss2"])
            P.add("dve", lambda e, b=b, ci=ci: e.scalar_tensor_tensor(out=htmp[:], in0=xt[b][:], scalar=ss[:, 2:3], in1=A1[ci][:],
                                                                      op0=ALU.mult, op1=ALU.mult),
                  reads=[X, "ss2", "A1_%d" % ci], writes=["htmp"])
            P.add("dve", lambda e, ci=ci: e.tensor_tensor(out=hb[:], in0=htmp[:], in1=B1[ci][:], op=ALU.add),
                  reads=["htmp", "B1_%d" % ci], writes=["hb"])
            for k in range(8):
                P.add("pe", lambda e, k=k: e.transpose(out=PA[:, k * 128:(k + 1) * 128], in_=hb[:, k * 128:(k + 1) * 128], identity=ident_b[:]),
                      reads=["hb", "ident_b"], writes=["PA"])
            P.add("act", lambda e: e.activation(out=hT[:], in_=PA[:], func=AF.Copy), reads=["PA"], writes=["hT"])
            chk(1.1)

            for k in range(8):
                P.add("pe", lambda e, k=k: e.matmul(PQ[4][0:32, 0:128], lhsT=win[:, k, 896:928], rhs=hT[:, k * 128:(k + 1) * 128],
                                                    start=(k == 0), stop=(k == 7)), reads=["win", "hT"], writes=["PQ4"])
            P.add("act", lambda e: e.activation(out=gzaug[0:32, :], in_=PQ[4][0:32, 0:128], func=AF.Copy), reads=["PQ4"], writes=["gzaug"])
            P.add("pe", lambda e: e.matmul(PQ[4][:, 0:128], lhsT=gzaug[:], rhs=gw[:], start=True, stop=True), reads=["gzaug", "gw"], writes=["PQ4"])
            P.add("act", lambda e: e.activation(out=t1[:], in_=PQ[4][:, 0:128], func=AF.Exp, scale=-1.0), reads=["PQ4"], writes=["t1"])
            P.add("act", lambda e: e.activation(out=spl[:, 0:128], in_=t1[:], func=AF.Ln, bias=1.0), reads=["t1", "spl"], writes=["spl"])
            P.add("pe", lambda e: e.matmul(PQ[4][:, 0:64], lhsT=triF, rhs=spl[:, 0:64], start=True, stop=True), reads=["spl", "csb"], writes=["PQ4"])
            P.add("pe", lambda e: e.matmul(PQ[4][:, 64:128], lhsT=triB, rhs=spl[:, 64:128], start=True, stop=True), reads=["spl", "csb"], writes=["PQ4"])
            for d in range(2):
                P.add("pe", lambda e, d=d: e.matmul(PQ[3][:, d:d + 1], lhsT=spl[:, d * 64:d * 64 + 128], rhs=csb[:, 512:513], start=True, stop=True),
                      reads=["spl", "csb"], writes=["PQ3"])
            P.add("act", lambda e: e.activation(out=epos[:], in_=PQ[4][:, 0:128], func=AF.Exp, scale=-1.0 / 16), reads=["PQ4"], writes=["epos"])
            P.add("act", lambda e: e.activation(out=eneg[:], in_=PQ[4][:, 0:128], func=AF.Exp, scale=1.0 / 16), reads=["PQ4"], writes=["eneg"])
            P.add("act", lambda e, tt=tt: e.activation(out=ET[0:64, tt * 2:(tt + 1) * 2], in_=PQ[3][0:64, 0:2], func=AF.Exp, scale=-1.0 / 16),
                  reads=["PQ3"], writes=["ET"])

            chk(1.2)
            for k in range(8):
                P.add("pe", lambda e, k=k: e.matmul(PQ[0][:, 0:384], lhsT=hT[:, k * 128:(k + 1) * 128], rhs=win[:, k, 0:384], start=(k == 0), stop=(k == 7)),
                      reads=["win", "hT"], writes=["PQ0"])
            for d in range(2):
                P.add("dve", lambda e, d=d, b=b: e.scalar_tensor_tensor(out=qtok[b][:, d * 256:d * 256 + 64], in0=PQ[0][:, 0:64], scalar=0.125,
                                                                        in1=epos[:, d * 64:(d + 1) * 64], op0=ALU.mult, op1=ALU.mult),
                      reads=["PQ0", "epos"], writes=[QN])
                P.add("dve", lambda e, d=d, b=b: e.tensor_tensor(out=ktok[b][:, d * 256:d * 256 + 64], in0=PQ[0][:, 64:128], in1=eneg[:, d * 64:(d + 1) * 64], op=ALU.mult),
                      reads=["PQ0", "eneg"], writes=[KN])
            qk3 = lambda: PQ[0][:, 128:384].rearrange("p (h c) -> p h c", h=2)
            cosb = lambda b=b: rp[b][:, 0:64].unsqueeze(1).to_broadcast([128, 2, 64])
            sinb = lambda b=b: rp[b][:, 64:128].unsqueeze(1).to_broadcast([128, 2, 64])
            r3 = lambda t: t[:].rearrange("p (h c) -> p h c", h=2)
            R = "rp%d" % b
            P.add("dve", lambda e, cosb=cosb: e.tensor_tensor(out=r3(ra), in0=qk3()[:, :, 0:64], in1=cosb(), op=ALU.mult), reads=["PQ0", R], writes=["ra"])
            P.add("dve", lambda e, sinb=sinb: e.tensor_tensor(out=r3(rb), in0=qk3()[:, :, 64:128], in1=sinb(), op=ALU.mult), reads=["PQ0", R], writes=["rb"])
            P.add("dve", lambda e: e.tensor_tensor(out=r3(rr)[:, :, 0:64], in0=r3(ra), in1=r3(rb), op=ALU.subtract), reads=["ra", "rb"], writes=["rr"])
            P.add("dve", lambda e, sinb=sinb: e.tensor_tensor(out=r3(ra), in0=qk3()[:, :, 0:64], in1=sinb(), op=ALU.mult), reads=["PQ0", R, "rr"], writes=["ra"])
            P.add("dve", lambda e, cosb=cosb: e.tensor_tensor(out=r3(rb), in0=qk3()[:, :, 64:128], in1=cosb(), op=ALU.mult), reads=["PQ0", R, "rr"], writes=["rb"])
            P.add("dve", lambda e: e.tensor_tensor(out=r3(rr)[:, :, 64:128], in0=r3(ra), in1=r3(rb), op=ALU.add), reads=["ra", "rb"], writes=["rr"])
            for d in range(2):
                P.add("dve", lambda e, d=d, b=b: e.tensor_scalar(out=qtok[b][:, d * 256 + 128:(d + 1) * 256], in0=rr[:, 0:128], scalar1=DQ[:, d:d + 1], scalar2=None, op0=ALU.mult),
                      reads=["rr", "DQ%d" % d], writes=[QN])
                P.add("dve", lambda e, d=d, b=b: e.tensor_scalar(out=ktok[b][:, d * 256 + 128:(d + 1) * 256], in0=rr[:, 128:256], scalar1=DK[:, d:d + 1], scalar2=None, op0=ALU.mult),
                      reads=["rr", "DK%d" % d], writes=[KN])
            chk(1.3)
            for k in range(8):
                P.add("pe", lambda e, k=k: e.matmul(PQ[1][:], lhsT=hT[:, k * 128:(k + 1) * 128], rhs=win[:, k, 384:896], start=(k == 0), stop=(k == 7)),
                      reads=["win", "hT"], writes=["PQ1"])
            P.add("act", lambda e, b=b: e.activation(out=vvt[b][:], in_=PQ[1][:, 0:256], func=AF.Copy), reads=["PQ1"], writes=["vvt%d" % b])
            if not isctx:
                P.add("act", lambda e, b=b: e.activation(out=sgt[b][:], in_=PQ[1][:, 256:512], func=AF.Silu), reads=["PQ1"], writes=["sgt%d" % b])
            chk(1.4)
            for d in range(2):
                for j, (src, sn) in enumerate(((qtok[b], QN), (ktok[b], KN))):
                    for blk in range(2):
                        c0 = d * 512 + j * 256 + blk * 128
                        P.add("pe", lambda e, src=src, d=d, blk=blk, c0=c0: e.transpose(out=PA[:, c0:c0 + 128], in_=src[:, d * 256 + blk * 128: d * 256 + (blk + 1) * 128],
                                                                                      identity=ident_b[:]), reads=[sn, "ident_b"], writes=["PA"])
            P.add("act", lambda e, b=b: e.activation(out=qkts[b][:], in_=PA[:], func=AF.Copy), reads=["PA"], writes=["qkts%d" % b])
            P.add("sp", lambda e, b=b, tt=tt: e.dma_start(out=QKT[tt, :, :], in_=qkts[b][:]), reads=["qkts%d" % b], writes=["DR_qk"], dma=True)
            P.add("sp", lambda e, b=b, tt=tt: e.dma_start(out=KK[tt, :, :], in_=ktok[b][:]), reads=[KN], writes=["DR_kk"], dma=True)
            P.add("sp", lambda e, b=b, tt=tt: e.dma_start(out=VV[tt, :, :], in_=vvt[b][:]), reads=["vvt%d" % b], writes=["DR_vv"], dma=True)
            if not isctx:
                P.add("sp", lambda e, b=b, tt=tt: e.dma_start(out=SG[tt - 2, :, :], in_=sgt[b][:]), reads=["sgt%d" % b], writes=["DR_sg"], dma=True)
        P.barrier()
    es01.close()
    chk(2)

    with ExitStack() as ph:
        def sbp(name, shape, dt=F32):
            return ph.enter_context(nc.sbuf_tensor(name, list(shape), dt))
        hnw = sbp("hnw", [128, 256])
        qk = [sbp("qk%d" % i, [128, 512], BF16) for i in range(2)]
        kk = [sbp("kk%d" % i, [128, 256], BF16) for i in range(2)]
        vv = [sbp("vv%d" % i, [128, 256], BF16) for i in range(2)]
        S = sbp("S", [128, 256])
        Sb = sbp("Sb", [128, 256], BF16)
        Tt = [sbp("Tt%d" % i, [128, 128]) for i in range(2)]
        PTs = [sbp("PTs%d" % i, [128, 128], BF16) for i in range(2)]
        ofs = [sbp("ofs%d" % i, [128, 256]) for i in range(2)]
        osum = sbp("osum", [128, 256])
        sq = sbp("sq", [128, 256])
        st = sbp("st", [128, 64])
        sgl = [sbp("sgl%d" % i, [128, 256], BF16) for i in range(2)]
        yb = [sbp("yb%d" % i, [128, 256], BF16) for i in range(2)]

        P.add("sp", lambda e: e.dma_start(out=hnw[:], in_=hn_bc[:, :]), writes=["hnw"], dma=True)
        step = 0
        for d in range(2):
            P.add("dve", lambda e: e.memset(S[:], 0.0), writes=["S0", "S1"])
            P.add("dve", lambda e: e.memset(Sb[:], 0.0), writes=["Sb0", "Sb1"])
            seq = list(range(NTT)) if d == 0 else [1, 0] + list(range(NTT - 1, 1, -1))
            maskb = triF_b if d == 0 else triB_b
            maskn = "triF_b" if d == 0 else "triB_b"
            for tt in seq:
                b = step % 2
                step += 1
                isctx = tt < 2
                lt = tt - 2
                if not isctx:
                    P.add("sp", lambda e, d=d, tt=tt, b=b: e.dma_start(out=qk[b][:], in_=QKT[tt, :, d * 512:(d + 1) * 512]), reads=["DR_qk"], writes=["qk%d" % b], dma=True)
                P.add("sp", lambda e, d=d, tt=tt, b=b: e.dma_start(out=kk[b][:], in_=KK[tt, :, d * 256:(d + 1) * 256]), reads=["DR_kk"], writes=["kk%d" % b], dma=True)
                P.add("sp", lambda e, tt=tt, b=b: e.dma_start(out=vv[b][:], in_=VV[tt, :, :]), reads=["DR_vv"], writes=["vv%d" % b], dma=True)
                if not isctx and d == 1:
                    P.add("sp", lambda e, lt=lt, b=b: e.dma_start(out=ofs[b][:], in_=OF[lt, :, :]), reads=["DR_of"], writes=["ofs%d" % b], dma=True)
                    P.add("sp", lambda e, lt=lt, b=b: e.dma_start(out=sgl[b][:], in_=SG[lt, :, :]), reads=["DR_sg"], writes=["sgl%d" % b], dma=True)
                for hh in range(2):
                    dk = 64 if hh == 0 else 128
                    qc = slice(hh * 128, (hh + 1) * 128)
                    kc_ = slice(256 + hh * 128, 256 + (hh + 1) * 128)
                    hs = slice(hh * 128, (hh + 1) * 128)
                    ktc = slice(0, 64) if hh == 0 else slice(128, 256)
                    if not isctx:
                        P.add("pe", lambda e, b=b, dk=dk, qc=qc, kc_=kc_, hh=hh: e.matmul(PQ[hh][:, 0:128], lhsT=qk[b][0:dk, kc_], rhs=qk[b][0:dk, qc], start=True, stop=True),
                              reads=["qk%d" % b], writes=["PQ%d" % hh])
                        P.add("dve", lambda e, hh=hh, maskb=maskb: e.tensor_tensor(out=PTs[hh][:], in0=PQ[hh][:, 0:128], in1=maskb[:], op=ALU.mult),
                              reads=["PQ%d" % hh, maskn], writes=["PTs%d" % hh])
                        P.add("pe", lambda e, b=b, hh=hh, hs=hs: e.matmul(PQ[2][:, hs], lhsT=PTs[hh][:], rhs=vv[b][:, hs], start=True, stop=False),
                              reads=["PTs%d" % hh, "vv%d" % b], writes=["PQ2"])
                        P.add("pe", lambda e, b=b, dk=dk, qc=qc, hs=hs: e.matmul(PQ[2][:, hs], lhsT=qk[b][0:dk, qc], rhs=Sb[0:dk, hs], start=False, stop=True),
                              reads=["qk%d" % b, "Sb%d" % hh], writes=["PQ2"])
                    pun = "pm%d" % hh
                    P.add("pe", lambda e, b=b, dk=dk, ktc=ktc, hs=hs, hh=hh: e.matmul(pm[hh][0:dk, 0:128], lhsT=kk[b][:, ktc], rhs=vv[b][:, hs], start=True, stop=True),
                          reads=["kk%d" % b, "vv%d" % b], writes=[pun])
                    if hh == 0:
                        eap = lambda tt=tt, d=d: ET[0:64, tt * 2 + d: tt * 2 + d + 1]
                        en = "ET"
                    else:
                        eap = lambda d=d: ER[:, d:d + 1]
                        en = "ER"
                    P.add("dve", lambda e, hh=hh, dk=dk, hs=hs: e.tensor_tensor(out=Tt[hh][0:dk, :], in0=pm[hh][0:dk, 0:128], in1=S[0:dk, hs], op=ALU.add),
                          reads=[pun, "S%d" % hh], writes=["Tt%d" % hh])
                    P.add("act", lambda e, hh=hh, dk=dk, hs=hs, eap=eap: e.activation(out=S[0:dk, hs], in_=Tt[hh][0:dk, :], func=AF.Copy, scale=eap()),
                          reads=["Tt%d" % hh, en], writes=["S%d" % hh])
                    P.add("dve", lambda e, hh=hh, dk=dk, hs=hs, eap=eap: e.tensor_scalar(out=Sb[0:dk, hs], in0=Tt[hh][0:dk, :], scalar1=eap(), scalar2=None, op0=ALU.mult),
                          reads=["Tt%d" % hh, en], writes=["Sb%d" % hh])
                if isctx:
                    continue
                if d == 0:
                    P.add("act", lambda e, b=b: e.activation(out=ofs[b][:], in_=PQ[2][:, 0:256], func=AF.Copy), reads=["PQ2"], writes=["ofs%d" % b])
                    P.add("sp", lambda e, b=b, lt=lt: e.dma_start(out=OF[lt, :, :], in_=ofs[b][:]), reads=["ofs%d" % b], writes=["DR_of"], dma=True)
                    continue
                P.add("dve", lambda e, b=b: e.tensor_tensor(out=osum[:], in0=PQ[2][:, 0:256], in1=ofs[b][:], op=ALU.add), reads=["PQ2", "ofs%d" % b], writes=["osum"])
                o3 = lambda: osum[:].rearrange("p (h c) -> p h c", h=2)
                s3 = lambda: sq[:].rearrange("p (h c) -> p h c", h=2)
                P.add("dve", lambda e: e.tensor_reduce(out=st[:, 0:2], in_=o3(), axis=AX.X, op=ALU.add), reads=["osum"], writes=["st_s1"])
                P.add("dve", lambda e: e.tensor_tensor(out=sq[:], in0=osum[:], in1=osum[:], op=ALU.mult), reads=["osum"], writes=["sq"])
                P.add("dve", lambda e: e.tensor_reduce(out=st[:, 8:10], in_=s3(), axis=AX.X, op=ALU.add), reads=["sq"], writes=["st_s2"])
                P.add("dve", lambda e: e.tensor_scalar(out=st[:, 16:18], in0=st[:, 0:2], scalar1=1.0 / 128, scalar2=None, op0=ALU.mult), reads=["st_s1"], writes=["st_m"])
                P.add("dve", lambda e: e.memset(st[:, 16:17], 0.0), reads=["st_m"], writes=["st_m"])
                P.add("dve", lambda e: e.tensor_tensor(out=st[:, 24:26], in0=st[:, 16:18], in1=st[:, 16:18], op=ALU.mult), reads=["st_m"], writes=["st_mm"])
                P.add("dve", lambda e: e.scalar_tensor_tensor(out=st[:, 32:34], in0=st[:, 8:10], scalar=1.0 / 128, in1=st[:, 24:26], op0=ALU.mult, op1=ALU.subtract),
                      reads=["st_s2", "st_mm"], writes=["st_v"])
                P.add("act", lambda e: e.activation(out=st[:, 40:42], in_=st[:, 32:34], func=AF.Sqrt, bias=EPSC), reads=["st_v", "csb"], writes=["st_sd"])
                P.add("dve", lambda e: e.reciprocal(out=st[:, 48:50], in_=st[:, 40:42]), reads=["st_sd"], writes=["st_r"])
                P.add("dve", lambda e: e.scalar_tensor_tensor(out=st[:, 56:58], in0=st[:, 16:18], scalar=-1.0, in1=st[:, 48:50], op0=ALU.mult, op1=ALU.mult),
                      reads=["st_m", "st_r"], writes=["st_sh"])
                P.add("dve", lambda e: e.tensor_tensor(out=s3(), in0=o3(), in1=st[:, 48:50].unsqueeze(2).to_broadcast([128, 2, 128]), op=ALU.mult),
                      reads=["osum", "st_r", "sq"], writes=["sq"])
                P.add("dve", lambda e: e.tensor_tensor(out=o3(), in0=s3(), in1=st[:, 56:58].unsqueeze(2).to_broadcast([128, 2, 128]), op=ALU.add),
                      reads=["sq", "st_sh"], writes=["osum"])
                P.add("dve", lambda e: e.tensor_tensor(out=sq[:], in0=osum[:], in1=hnw[:], op=ALU.mult), reads=["osum", "hnw"], writes=["sq"])
                P.add("dve", lambda e, b=b: e.tensor_tensor(out=yb[b][:], in0=sq[:], in1=sgl[b][:], op=ALU.mult), reads=["sq", "sgl%d" % b], writes=["yb%d" % b])
                P.add("sp", lambda e, b=b, lt=lt: e.dma_start(out=YL[lt * 128:(lt + 1) * 128, :], in_=yb[b][:]), reads=["yb%d" % b], writes=["DR_yl"], dma=True)
        P.barrier()

    chk(3)
    P.add_cc(lambda e: e.collective_compute("AllGather", ALU.bypass, replica_groups=[list(range(8))], ins=[YL[:, :]], outs=[YA[:, :]]),
             reads=["DR_yl"], writes=["DR_ya"])
    P.barrier()

    chk(4)
    with ExitStack() as ph:
        def sbp(name, shape, dt=F32):
            return ph.enter_context(nc.sbuf_tensor(name, list(shape), dt))
        wo = sbp("wo", [128, 8, D], BF16)
        wr = sbp("wr", [128, 8, NE], BF16)
        yg = [sbp("yg%d" % i, [128, D], BF16) for i in range(2)]
        xl = [sbp("xl%d" % i, [128, D]) for i in range(2)]
        yT = sbp("yT", [128, D], BF16)
        sqB = sbp("sq2", [128, D])
        stB = sbp("st2", [128, 16])
        x1t = [sbp("x1t%d" % i, [128, D]) for i in range(2)]
        junk2 = sbp("junk2", [128, D], BF16)
        h2r = [sbp("h2r%d" % i, [128, RW], BF16) for i in range(2)]
        h2T = sbp("h2T", [128, D], BF16)
        ex = sbp("ex", [128, NE])
        gtmp = sbp("gtmp", [128, NE])
        for k in range(8):
            P.add("pool", lambda e, k=k: e.dma_start(out=wo[:, k, :], in_=w_out[k * 128:(k + 1) * 128, :]), writes=["wo"], dma=True)
            P.add("pool", lambda e, k=k: e.dma_start(out=wr[:, k, :], in_=w_router[k * 128:(k + 1) * 128, :]), writes=["wr"], dma=True)
        for i in range(2):
            P.add("dve", lambda e, i=i: e.memset(h2r[i][:, D:RW], 0.0), writes=["h2r%d" % i])
        for it in range(NT):
            b = it % 2
            own = it < NTO
            for r in range(4):
                P.add("pool", lambda e, it=it, r=r, b=b: e.indirect_dma_start(
                    out=yg[b][:, r * 256:(r + 1) * 256], out_offset=None, in_=YA[:, :],
                    in_offset=bass.IndirectOffsetOnAxis(ap=yidx[:, it * 4 + r: it * 4 + r + 1], axis=0)),
                    reads=["DR_ya", "yidx"], writes=["yg%d" % b], dma=True)
            P.add("sp", lambda e, it=it, b=b: e.dma_start(out=xl[b][:], in_=xrot[it * 128:(it + 1) * 128, :]), writes=["xl%d" % b], dma=True)
            for k in range(8):
                P.add("pe", lambda e, k=k, b=b: e.transpose(out=PA[:, k * 128:(k + 1) * 128], in_=yg[b][:, k * 128:(k + 1) * 128], identity=ident_b[:]),
                      reads=["yg%d" % b, "ident_b"], writes=["PA"])
            P.add("act", lambda e: e.activation(out=yT[:], in_=PA[:], func=AF.Copy), reads=["PA"], writes=["yT"])
            for nch in range(2):
                for k in range(8):
                    P.add("pe", lambda e, k=k, nch=nch: e.matmul(PQ[2 + nch][:], lhsT=yT[:, k * 128:(k + 1) * 128], rhs=wo[:, k, nch * 512:(nch + 1) * 512],
                                                                 start=(k == 0), stop=(k == 7)), reads=["yT", "wo"], writes=["PQ%d" % (2 + nch)])
                cs = slice(nch * 512, (nch + 1) * 512)
                P.add("dve", lambda e, nch=nch, cs=cs: e.tensor_tensor(out=sqB[:, cs], in0=PQ[2 + nch][:], in1=M2[:, cs], op=ALU.mult),
                      reads=["PQ%d" % (2 + nch), "M2"], writes=["sq"])
            P.add("dve", lambda e, b=b: e.tensor_tensor(out=x1t[b][:], in0=sqB[:], in1=xl[b][:], op=ALU.add), reads=["sq", "xl%d" % b], writes=["x1t%d" % b])
            if own:
                P.add("sp", lambda e, b=b, it=it: e.dma_start(out=X1[it * 128:(it + 1) * 128, :], in_=x1t[b][:]), reads=["x1t%d" % b], writes=["DR_x1"], dma=True)
            P.add("act", lambda e, b=b: e.activation(out=junk2[:], in_=x1t[b][:], func=AF.Square, accum_out=stB[:, 0:1]), reads=["x1t%d" % b], writes=["junk2", "st_s1"])
            P.add("act", lambda e: e.activation(out=stB[:, 1:2], in_=stB[:, 0:1], func=AF.Sqrt, scale=1.0 / D, bias=EPSC), reads=["st_s1", "csb"], writes=["st_q1"])
            P.add("dve", lambda e: e.reciprocal(out=stB[:, 2:3], in_=stB[:, 1:2]), reads=["st_q1"], writes=["st_q2"])
            P.add("dve", lambda e, b=b: e.scalar_tensor_tensor(out=sqB[:], in0=x1t[b][:], scalar=stB[:, 2:3], in1=A2[:], op0=ALU.mult, op1=ALU.mult),
                  reads=["x1t%d" % b, "st_q2", "A2", "sq"], writes=["sq"])
            H = "h2r%d" % b
            P.add("dve", lambda e, b=b: e.tensor_tensor(out=h2r[b][:, 0:D], in0=sqB[:], in1=B2[:], op=ALU.add), reads=["sq", "B2"], writes=[H])
            for k in range(8):
                P.add("pe", lambda e, k=k, b=b: e.transpose(out=PA[:, k * 128:(k + 1) * 128], in_=h2r[b][:, k * 128:(k + 1) * 128], identity=ident_b[:]),
                      reads=[H, "ident_b"], writes=["PA"])
            P.add("act", lambda e: e.activation(out=h2T[:], in_=PA[:], func=AF.Copy), reads=["PA"], writes=["h2T"])
            for k in range(8):
                P.add("pe", lambda e, k=k: e.matmul(PQ[4][:, 0:NE], lhsT=h2T[:, k * 128:(k + 1) * 128], rhs=wr[:, k, :], start=(k == 0), stop=(k == 7)),
                      reads=["h2T", "wr"], writes=["PQ4"])
            P.add("dve", lambda e: e.tensor_reduce(out=stB[:, 4:5], in_=PQ[4][:, 0:NE], axis=AX.X, op=ALU.max), reads=["PQ4"], writes=["st_mx"])
            P.add("dve", lambda e: e.tensor_scalar(out=stB[:, 5:6], in0=stB[:, 4:5], scalar1=-1.0, scalar2=None, op0=ALU.mult), reads=["st_mx"], writes=["st_nmx"])
            P.add("act", lambda e: e.activation(out=ex[:], in_=PQ[4][:, 0:NE], func=AF.Exp, bias=stB[:, 5:6], accum_out=stB[:, 6:7]), reads=["PQ4", "st_nmx"], writes=["ex", "st_sm"])
            P.add("dve", lambda e: e.reciprocal(out=stB[:, 7:8], in_=stB[:, 6:7]), reads=["st_sm"], writes=["st_rs"])
            asl = slice(it * NE, (it + 1) * NE)
            P.add("dve", lambda e, asl=asl: e.tensor_scalar(out=affT[:, asl], in0=ex[:], scalar1=stB[:, 7:8], scalar2=None, op0=ALU.mult),
                  reads=["ex", "st_rs"], writes=["affT"])
            if own:
                P.add("dve", lambda e, b=b, asl=asl: e.tensor_copy(out=h2r[b][:, MG:MG + 16], in_=affT[:, asl]), reads=["affT"], writes=[H])
                P.add("dve", lambda e, b=b, asl=asl: e.tensor_tensor(out=gtmp[:], in0=affT[:, asl], in1=h2r[b][:, MG:MG + 16], op=ALU.subtract), reads=["affT", H], writes=["gtmp"])
                P.add("dve", lambda e, b=b: e.tensor_copy(out=h2r[b][:, MG + 16:MG + 32], in_=gtmp[:]), reads=["gtmp"], writes=[H])
                P.add("dve", lambda e, b=b: e.tensor_tensor(out=gtmp[:], in0=gtmp[:], in1=h2r[b][:, MG + 16:MG + 32], op=ALU.subtract), reads=["gtmp", H], writes=["gtmp"])
                P.add("dve", lambda e, b=b: e.tensor_copy(out=h2r[b][:, MG + 32:MG + 48], in_=gtmp[:]), reads=["gtmp"], writes=[H])
                P.add("dve", lambda e, b=b, it=it: e.memset(h2r[b][:, MHI:MHI + 1], float(it)), reads=[H], writes=[H])
                P.add("dve", lambda e, b=b: e.tensor_copy(out=h2r[b][:, MLO:MLO + 1], in_=csb[:, IOTA:IOTA + 1]), reads=[H, "csb"], writes=[H])
                P.add("sp", lambda e, b=b, it=it: e.dma_start(out=H2[it, :, :], in_=h2r[b][:]), reads=[H], writes=["DR_h2"], dma=True)
        P.barrier()

    chk(5)
    with ExitStack() as ph:
        def sbp(name, shape, dt=F32):
            return ph.enter_context(nc.sbuf_tensor(name, list(shape), dt))
        NTE = NT * NE
        NOE = NTO * NE
        lo = sbp("lo", [128, NE])
        hi_ = sbp("hi", [128, NE])
        mid = sbp("mid", [128, NE])
        cmp_ = sbp("cmp", [128, NTE])
        cnt = sbp("cnt", [128, NE])
        ge = sbp("ge", [128, NE])
        d1 = sbp("d1", [128, NE])
        d2 = sbp("d2", [128, NE])
        maskb_ = sbp("maskb", [128, NOE], BF16)
        wn = sbp("wn", [128, NOE])
        tots = sbp("tots", [128, NOE])
        offs = sbp("offs", [128, NOE])
        pos = sbp("pos", [128, NOE])
        sel = sbp("sel", [128, NOE])
        hrow = [sbp("hrow%d" % i, [128, RW], BF16) for i in range(2)]

        a3 = lambda t: t[:].rearrange("p (t e) -> p t e", e=NE)
        P.add("dve", lambda e: e.memset(lo[:], 0.0), writes=["lo"])
        P.add("dve", lambda e: e.memset(hi_[:], 2.0), writes=["hi"])
        for it in range(40):
            P.add("dve", lambda e: e.tensor_tensor(out=mid[:], in0=lo[:], in1=hi_[:], op=ALU.add), reads=["lo", "hi"], writes=["mid"])
            P.add("dve", lambda e: e.tensor_scalar(out=mid[:], in0=mid[:], scalar1=0.5, scalar2=None, op0=ALU.mult), reads=["mid"], writes=["mid"])
            P.add("dve", lambda e: e.tensor_tensor(out=a3(cmp_), in0=a3(affT), in1=mid[:].unsqueeze(1).to_broadcast([128, NT, NE]), op=ALU.is_ge),
                  reads=["affT", "mid"], writes=["cmp"])
            P.add("dve", lambda e: e.tensor_reduce(out=cnt[:], in_=cmp_[:].rearrange("p (t e) -> p e t", e=NE), axis=AX.X, op=ALU.add), reads=["cmp"], writes=["cnt"])
            P.add("pe", lambda e: e.matmul(PQ[0][:, 0:NE], lhsT=onesF, rhs=cnt[:], start=True, stop=True), reads=["cnt", "csb"], writes=["PQ0"])
            P.add("dve", lambda e: e.tensor_scalar(out=ge[:], in0=PQ[0][:, 0:NE], scalar1=float(CAP) - 0.5, scalar2=None, op0=ALU.is_ge), reads=["PQ0"], writes=["ge"])
            P.add("dve", lambda e: e.tensor_tensor(out=d1[:], in0=mid[:], in1=lo[:], op=ALU.subtract), reads=["mid", "lo"], writes=["d1"])
            P.add("dve", lambda e: e.tensor_tensor(out=d2[:], in0=hi_[:], in1=mid[:], op=ALU.subtract), reads=["mid", "hi"], writes=["d2"])
            P.add("dve", lambda e: e.tensor_tensor(out=d1[:], in0=d1[:], in1=ge[:], op=ALU.mult), reads=["d1", "ge"], writes=["d1"])
            P.add("dve", lambda e: e.tensor_tensor(out=d2[:], in0=d2[:], in1=ge[:], op=ALU.mult), reads=["d2", "ge"], writes=["d2"])
            P.add("dve", lambda e: e.tensor_tensor(out=lo[:], in0=lo[:], in1=d1[:], op=ALU.add), reads=["lo", "d1"], writes=["lo"])
            P.add("dve", lambda e: e.tensor_tensor(out=hi_[:], in0=mid[:], in1=d2[:], op=ALU.add), reads=["mid", "d2"], writes=["hi"])
        P.add("dve", lambda e: e.tensor_tensor(out=cmp_[:, 0:NOE].rearrange("p (t e) -> p t e", e=NE), in0=affT[:, 0:NOE].rearrange("p (t e) -> p t e", e=NE),
                                               in1=lo[:].unsqueeze(1).to_broadcast([128, NTO, NE]), op=ALU.is_ge),
              reads=["affT", "lo"], writes=["cmp"])
        P.add("dve", lambda e: e.tensor_copy(out=maskb_[:], in_=cmp_[:, 0:NOE]), reads=["cmp"], writes=["maskb"])
        for c0 in range(0, NOE, 512):
            c1 = min(NOE, c0 + 512)
            w = c1 - c0
            P.add("pe", lambda e, c0=c0, c1=c1, w=w: e.matmul(PQ[0][:, 0:w], lhsT=triS_b[:], rhs=maskb_[:, c0:c1], start=True, stop=True),
                  reads=["maskb", "triS_b"], writes=["PQ0"])
            P.add("pe", lambda e, c0=c0, c1=c1, w=w: e.matmul(PQ[1][:, 0:w], lhsT=ones_b[:], rhs=maskb_[:, c0:c1], start=True, stop=True),
                  reads=["maskb", "ones_b"], writes=["PQ1"])
            P.add("act", lambda e, c0=c0, c1=c1, w=w: e.activation(out=wn[:, c0:c1], in_=PQ[0][:, 0:w], func=AF.Copy), reads=["PQ0"], writes=["wn"])
            P.add("act", lambda e, c0=c0, c1=c1, w=w: e.activation(out=tots[:, c0:c1], in_=PQ[1][:, 0:w], func=AF.Copy), reads=["PQ1"], writes=["tots"])
        P.add("dve", lambda e: e.memset(offs[:, 0:NE], 0.0), writes=["offs"])
        for t in range(1, NTO):
            P.add("dve", lambda e, t=t: e.tensor_tensor(out=offs[:, t * NE:(t + 1) * NE], in0=offs[:, (t - 1) * NE:t * NE], in1=tots[:, (t - 1) * NE:t * NE], op=ALU.add),
                  reads=["offs", "tots"], writes=["offs"])
        P.add("dve", lambda e: e.tensor_tensor(out=pos[:], in0=wn[:], in1=offs[:], op=ALU.add), reads=["wn", "offs"], writes=["pos"])
        P.add("dve", lambda e: e.tensor_scalar(out=sel[:], in0=pos[:], scalar1=float(CAPL) - 0.5, scalar2=None, op0=ALU.is_lt), reads=["pos"], writes=["sel"])
        P.add("dve", lambda e: e.tensor_tensor(out=sel[:], in0=sel[:], in1=cmp_[:, 0:NOE], op=ALU.mult), reads=["sel", "cmp"], writes=["sel"])
        P.add("dve", lambda e: e.tensor_scalar(out=pos[:], in0=pos[:], scalar1=csb[:, IOTA + 5:IOTA + 6], scalar2=None, op0=ALU.subtract), reads=["pos", "csb"], writes=["pos"])
        P.add("dve", lambda e: e.tensor_tensor(out=pos[:], in0=pos[:], in1=sel[:], op=ALU.mult), reads=["pos", "sel"], writes=["pos"])
        P.add("dve", lambda e: e.tensor_scalar(out=pos[:], in0=pos[:], scalar1=csb[:, IOTA + 5:IOTA + 6], scalar2=None, op0=ALU.add), reads=["pos", "csb"], writes=["pos"])
        P.add("dve", lambda e: e.tensor_copy(out=idxT[:], in_=pos[:]), reads=["pos"], writes=["idxT"])
        if debug:
            P.add("sp", lambda e: e.dma_start(out=dbg_aff[:, :], in_=affT[:]), reads=["affT"], writes=["dbg1"], dma=True)
            P.add("sp", lambda e: e.dma_start(out=dbg_idx[:, :], in_=idxT[:]), reads=["idxT"], writes=["dbg2"], dma=True)
        for lt in range(NTO):
            b = lt % 2
            P.add("sp", lambda e, lt=lt, b=b: e.dma_start(out=hrow[b][:], in_=H2[lt, :, :]), reads=["DR_h2"], writes=["hrow%d" % b], dma=True)
            for ex_ in range(NE):
                P.add("pool", lambda e, lt=lt, b=b, ex_=ex_: e.indirect_dma_start(
                    out=XE[ex_][:, :], out_offset=bass.IndirectOffsetOnAxis(ap=idxT[:, lt * NE + ex_: lt * NE + ex_ + 1], axis=0),
                    in_=hrow[b][:], in_offset=None), reads=["hrow%d" % b, "idxT"], writes=["XE%d" % ex_], dma=True)
        P.barrier()
    es03.close()
    chk(6)

    with ExitStack() as ph:
        def sbp(name, shape, dt=F32):
            return ph.enter_context(nc.sbuf_tensor(name, list(shape), dt))
        wg = [sbp("wg%d" % i, [128, 8, FF], BF16) for i in range(2)]
        wu = [sbp("wu%d" % i, [128, 8, FF], BF16) for i in range(2)]
        wd = [sbp("wd%d" % i, [128, NF, D], BF16) for i in range(2)]
        xrow = [sbp("xrow%d" % i, [128, RW], BF16) for i in range(2)]
        xeT = sbp("xeT", [128, 8, CAPL], BF16)
        gateT = sbp("gateT", [128, NST])
        gt2 = sbp("gt2", [128, 4])
        tokI = sbp("tokI", [128, NST], I32)
        sgx = sbp("sgx", [128, SC])
        hidT = sbp("hidT", [128, NF, SC], BF16)
        ysb = [sbp("ysb%d" % i, [128, D]) for i in range(2)]

        def load_w(ex_):
            wb = ex_ % 2
            for k in range(8):
                P.add("pool", lambda e, ex_=ex_, k=k, wb=wb: e.dma_start(out=wg[wb][:, k, :], in_=w_eg[ex_, k * 128:(k + 1) * 128, :]), writes=["wg%d" % wb], dma=True)
                P.add("pool", lambda e, ex_=ex_, k=k, wb=wb: e.dma_start(out=wu[wb][:, k, :], in_=w_eu[ex_, k * 128:(k + 1) * 128, :]), writes=["wu%d" % wb], dma=True)
            for f in range(NF):
                P.add("pool", lambda e, ex_=ex_, f=f, wb=wb: e.dma_start(out=wd[wb][:, f, :], in_=w_ed[ex_, f * 128:(f + 1) * 128, :]), writes=["wd%d" % wb], dma=True)

        load_w(0)
        yi = 0
        for ex_ in range(NE):
            wb = ex_ % 2
            if ex_ + 1 < NE:
                load_w(ex_ + 1)
            for s_ in range(NST):
                b = s_ % 2
                XR = "xrow%d" % b
                P.add("sp", lambda e, ex_=ex_, s_=s_, b=b: e.dma_start(out=xrow[b][:], in_=XE[ex_][s_ * 128:(s_ + 1) * 128, :]),
                      reads=["XE%d" % ex_], writes=[XR], dma=True)
                for k in range(8):
                    P.add("pe", lambda e, k=k, b=b: e.transpose(out=PA[:, k * 128:(k + 1) * 128], in_=xrow[b][:, k * 128:(k + 1) * 128], identity=ident_b[:]),
                          reads=[XR, "ident_b"], writes=["PA"])
                P.add("act", lambda e, s_=s_: e.activation(out=xeT[:, :, s_ * 128:(s_ + 1) * 128], in_=PA[:].rearrange("p (k c) -> p k c", k=8), func=AF.Copy),
                      reads=["PA"], writes=["xeT"])
                P.add("dve", lambda e, b=b, ex_=ex_: e.tensor_tensor(out=gt2[:, 0:1], in0=xrow[b][:, MG + ex_:MG + ex_ + 1], in1=xrow[b][:, MG + 16 + ex_:MG + 17 + ex_], op=ALU.add),
                      reads=[XR], writes=["gt2a"])
                P.add("dve", lambda e, b=b, ex_=ex_, s_=s_: e.tensor_tensor(out=gateT[:, s_:s_ + 1], in0=gt2[:, 0:1], in1=xrow[b][:, MG + 32 + ex_:MG + 33 + ex_], op=ALU.add),
                      reads=[XR, "gt2a"], writes=["gateT"])
                P.add("dve", lambda e, b=b: e.scalar_tensor_tensor(out=gt2[:, 1:2], in0=xrow[b][:, MHI:MHI + 1], scalar=128.0, in1=xrow[b][:, MLO:MLO + 1], op0=ALU.mult, op1=ALU.add),
                      reads=[XR], writes=["gt2b"])
                P.add("dve", lambda e, s_=s_: e.tensor_copy(out=tokI[:, s_:s_ + 1], in_=gt2[:, 1:2]), reads=["gt2b"], writes=["tokI"])
            for sc_ in range(NSC):
                ss_ = slice(sc_ * SC, (sc_ + 1) * SC)
                for f in range(NF):
                    pg = f % 2
                    for k in range(8):
                        P.add("pe", lambda e, k=k, f=f, pg=pg, ss_=ss_, wb=wb: e.matmul(PQ[pg][:, 0:SC], lhsT=wg[wb][:, k, f * 128:(f + 1) * 128], rhs=xeT[:, k, ss_],
                                                                                      start=(k == 0), stop=(k == 7)), reads=["wg%d" % wb, "xeT"], writes=["PQ%d" % pg])
                    for k in range(8):
                        P.add("pe", lambda e, k=k, f=f, pg=pg, ss_=ss_, wb=wb: e.matmul(PQ[2 + pg][:, 0:SC], lhsT=wu[wb][:, k, f * 128:(f + 1) * 128], rhs=xeT[:, k, ss_],
                                                                                      start=(k == 0), stop=(k == 7)), reads=["wu%d" % wb, "xeT"], writes=["PQ%d" % (2 + pg)])
                    P.add("act", lambda e, pg=pg: e.activation(out=sgx[:], in_=PQ[pg][:, 0:SC], func=AF.Silu), reads=["PQ%d" % pg], writes=["sgx"])
                    P.add("dve", lambda e, pg=pg, f=f: e.tensor_tensor(out=hidT[:, f, :], in0=sgx[:], in1=PQ[2 + pg][:, 0:SC], op=ALU.mult),
                          reads=["sgx", "PQ%d" % (2 + pg)], writes=["hidT"])
                for sl in range(SC // 128):
                    s_ = sc_ * (SC // 128) + sl
                    yb_ = yi % 2
                    yi += 1
                    for nch in range(2):
                        pp = "pm%d" % nch
                        for f in range(NF):
                            P.add("pe", lambda e, f=f, sl=sl, nch=nch, wb=wb: e.matmul(pm[nch][:], lhsT=hidT[:, f, sl * 128:(sl + 1) * 128], rhs=wd[wb][:, f, nch * 512:(nch + 1) * 512],
                                                                                      start=(f == 0), stop=(f == NF - 1)), reads=["hidT", "wd%d" % wb], writes=[pp])
                        cs = slice(nch * 512, (nch + 1) * 512)
                        P.add("dve", lambda e, nch=nch, cs=cs, s_=s_, yb_=yb_: e.scalar_tensor_tensor(out=ysb[yb_][:, cs], in0=pm[nch][:], scalar=gateT[:, s_:s_ + 1], in1=M5[:, cs],
                                                                                                    op0=ALU.mult, op1=ALU.mult),
                              reads=[pp, "gateT", "M5"], writes=["ysb%d" % yb_])
                    P.add("pool", lambda e, s_=s_, yb_=yb_: e.indirect_dma_start(
                        out=X1[:, :], out_offset=bass.IndirectOffsetOnAxis(ap=tokI[:, s_:s_ + 1], axis=0),
                        in_=ysb[yb_][:], in_offset=None, compute_op=ALU.add), reads=["ysb%d" % yb_, "tokI", "DR_x1"], writes=["DR_x1"], dma=True)
        P.barrier()

    chk(7)
    with ExitStack() as ph:
        def sbp(name, shape, dt=F32):
            return ph.enter_context(nc.sbuf_tensor(name, list(shape), dt))
        fnw = sbp("fnw", [128, D])
        xf = [sbp("xf%d" % i, [128, D]) for i in range(2)]
        of_ = [sbp("of%d" % i, [128, D]) for i in range(2)]
        junk3 = sbp("junk3", [128, D], BF16)
        s5 = sbp("s5", [128, 4])
        P.add("sp", lambda e: e.dma_start(out=fnw[:], in_=fn_bc[:, :]), writes=["fnw"], dma=True)
        for lt in range(NTO):
            b = lt % 2
            P.add("sp", lambda e, lt=lt, b=b: e.dma_start(out=xf[b][:], in_=X1[lt * 128:(lt + 1) * 128, :]), reads=["DR_x1"], writes=["xf%d" % b], dma=True)
            P.add("act", lambda e, b=b: e.activation(out=junk3[:], in_=xf[b][:], func=AF.Square, accum_out=s5[:, 0:1]), reads=["xf%d" % b], writes=["junk3", "s5a"])
            P.add("act", lambda e: e.activation(out=s5[:, 1:2], in_=s5[:, 0:1], func=AF.Sqrt, scale=1.0 / D, bias=EPSC), reads=["s5a", "csb"], writes=["s5b"])
            P.add("dve", lambda e: e.reciprocal(out=s5[:, 2:3], in_=s5[:, 1:2]), reads=["s5b"], writes=["s5c"])
            P.add("dve", lambda e, b=b: e.scalar_tensor_tensor(out=of_[b][:], in0=xf[b][:], scalar=s5[:, 2:3], in1=fnw[:], op0=ALU.mult, op1=ALU.mult),
                  reads=["xf%d" % b, "s5c", "fnw"], writes=["of%d" % b])
            P.add("sp", lambda e, lt=lt, b=b: e.dma_start(out=out[lt * 128:(lt + 1) * 128, :], in_=of_[b][:]), reads=["of%d" % b], writes=["OUT"], dma=True)

    P.emit()
    es.close()
    return nc


def _capl(T):
    CAP = 2 * T // NE
    return max(128, ((3 * CAP // 8) + 127) // 128 * 128)


def _consts(T):
    c = np.zeros((128, 1024), np.float32)
    p = np.arange(128)
    c[:, 0:128] = np.eye(128)
    c[:, 128:256] = (p[:, None] <= p[None, :])
    c[:, 256:384] = (p[:, None] >= p[None, :])
    c[:, 384:512] = (p[:, None] < p[None, :])
    c[:, 512:640] = 1.0
    c[:, 640] = p
    c[:, 641] = p + 1
    c[:, 642] = 128 - p
    c[:, 643] = -(p + 1)
    c[:, 644] = -(128 - p)
    c[:, 645] = _capl(T) + p
    c[:, 646] = math.log(128.0 ** -0.5)
    c[:, 647] = EPS
    return c


def _rope_table(T):
    rows = T // 64
    row = np.repeat(np.arange(rows), 64).astype(np.float32)
    col = np.tile(np.arange(64), rows).astype(np.float32)
    n_freq = 32
    inv = (np.float32(10000.0) ** (-np.arange(n_freq, dtype=np.float32) / n_freq)).astype(np.float32)
    ang = np.concatenate([row[:, None] * inv, col[:, None] * inv], axis=-1).astype(np.float32)
    tab = np.zeros((CTX + T, 128), np.float32)
    tab[:CTX, 0:64] = 1.0
    tab[CTX:, 0:64] = np.cos(ang)
    tab[CTX:, 64:128] = np.sin(ang)
    return tab


def make_shared(inp, T):
    f = lambda a: np.ascontiguousarray(np.asarray(a, dtype=np.float32))
    bc = lambda v: np.ascontiguousarray(np.broadcast_to(np.asarray(v, np.float32).reshape(1, -1), (128, np.asarray(v).size)))
    w_out = f(inp["w_out"])[0]
    perm = np.concatenate([np.concatenate([np.arange(r * 128, (r + 1) * 128), 512 + np.arange(r * 128, (r + 1) * 128)]) for r in range(4)])
    return {
        "rope": _rope_table(T),
        "w_ada": f(inp["w_ada"])[0],
        "b_ada_bc": bc(f(inp["b_ada"])[0]),
        "n1_bc": bc(f(inp["norm1_w"])[0]),
        "n2_bc": bc(f(inp["norm2_w"])[0]),
        "fn_bc": bc(f(inp["final_norm_w"])),
        "w_out": np.ascontiguousarray(w_out[perm]),
        "w_router": f(inp["w_router"])[0],
        "w_eg": f(inp["w_exp_gate"])[0],
        "w_eu": f(inp["w_exp_up"])[0],
        "w_ed": f(inp["w_exp_down"])[0],
        "cst": _consts(T),
    }


def make_inputs(inp, core, T, shared):
    f = lambda a: np.ascontiguousarray(np.asarray(a, dtype=np.float32))
    bc = lambda v: np.ascontiguousarray(np.broadcast_to(np.asarray(v, np.float32).reshape(1, -1), (128, np.asarray(v).size)))
    b, q = core // 4, core % 4
    NT = T // 128
    NTO = NT // 4
    x = f(inp["x"])[b, :T]
    ctx = f(inp["ctx"])[b]
    c = f(inp["c"])[b]
    cc = f(inp["c_ctx"])
    w_in = f(inp["w_in"])[0]
    h64 = slice(q * 64, (q + 1) * 64)
    h128 = slice(q * 128, (q + 1) * 128)
    w_in_r = np.concatenate([w_in[:, 0:256][:, h64], w_in[:, 256:512][:, h64], w_in[:, 1568:2080][:, h128], w_in[:, 2080:2592][:, h128],
                             w_in[:, 512:1024][:, h128], w_in[:, 2592:3104][:, h128], w_in[:, 1056:1568][:, h128], w_in[:, 3104:3616][:, h128],
                             w_in[:, 1024:1056]], axis=1)
    gw = f(inp["gla_gate_w"])[0]
    gb = f(inp["gla_gate_b"])[0]
    gwaug = np.zeros((33, 128), np.float32)
    gwaug[0:16, 0:64] = gw[0][:, h64]
    gwaug[16:32, 64:128] = gw[1][:, h64]
    gwaug[32, 0:64] = gb[0][h64]
    gwaug[32, 64:128] = gb[1][h64]
    scT = np.zeros((128, 16), np.float32)
    scT[:, 0:8] = c.reshape(8, 128).T
    scT[:, 8:16] = cc.reshape(8, 128).T
    rdl = f(inp["ret_decay_logit"])[0]
    order = (np.arange(NT) + q * NTO) % NT
    xrot = np.ascontiguousarray(x.reshape(NT, 128, D)[order].reshape(T, D))
    yidx = np.zeros((128, NT * 4), np.int32)
    for r in range(4):
        yidx[:, r::4] = ((4 * b + r) * T + order[None, :] * 128 + np.arange(128)[:, None]).astype(np.int32)
    d = dict(shared)
    d.update({
        "xin": np.ascontiguousarray(np.concatenate([ctx, x], axis=0)),
        "xrot": xrot,
        "scT": scT,
        "hn_bc": bc(np.concatenate([f(inp["gla_norm_w"])[0][h128], f(inp["ret_norm_w"])[0][h128]])),
        "w_in": np.ascontiguousarray(w_in_r),
        "gwaug": gwaug,
        "rdl_bc": bc(np.array([rdl[0, q], rdl[1, q]], np.float32)),
        "yidx": yidx,
    })
    return d


def kernel(**inputs):
    T = 16384
    B = 2
    nc = build_program(T)
    shared = make_shared(inputs, T)
    in_maps = [make_inputs(inputs, core, T, shared) for core in range(8)]
    res = run_bass_kernel_spmd(nc, in_maps, core_ids=list(range(8)))
    TQ = T // 4
    full = np.zeros((B, T, D), np.float32)
    for core in range(8):
        b, q = core // 4, core % 4
        full[b, q * TQ:(q + 1) * TQ] = np.asarray(res.results[core]["out"], dtype=np.float32).reshape(TQ, D)
    return full
```

```python
import math
from contextlib import ExitStack

import numpy as np
import ml_dtypes
import concourse.bass as bass
import concourse.mybir as mybir
from concourse.bass_utils import run_bass_kernel_spmd

F32 = mybir.dt.float32
BF16 = mybir.dt.bfloat16
I32 = mybir.dt.int32
ALU = mybir.AluOpType
AF = mybir.ActivationFunctionType
AX = mybir.AxisListType

D = 1024
NE = 16
FF = 1408
NF = FF // 128
INW = 3616
EPS = 1e-6
CTX = 256
RW = 1024 + 52
MG = 1024
MHI = 1024 + 48
MLO = 1024 + 49

ENGS = ["pe", "act", "dve", "pool", "sp"]
KDMA = 8
BLK = 8192


class Op:
    __slots__ = ("eng", "fn", "dma", "waits", "sem", "val")


class Prog:
    def __init__(self, nc, es):
        self.nc = nc
        self.es = es
        self.ops = {e: [] for e in ENGS}
        self.last_w = {}
        self.readers = {}
        self.ncomp = {e: 0 for e in ENGS}
        self.ndma = {e: 0 for e in ENGS}
        self.waited = {e: {} for e in ENGS}
        self.pending = {e: [] for e in ENGS}
        self.csem = {}
        self.dsem = {}
        self.nsem = 0

    def _sem(self, name):
        self.nsem += 1
        return self.es.enter_context(self.nc.semaphore(name))

    def _csem(self, eng, i):
        k = (eng, i // BLK)
        if k not in self.csem:
            self.csem[k] = self._sem("c_%s_%d" % k)
        return self.csem[k], i % BLK + 1

    def _dsem(self, eng, i):
        k = (eng, i % KDMA)
        if k not in self.dsem:
            self.dsem[k] = self._sem("d_%s_%d" % k)
        return self.dsem[k], 16 * (i // KDMA + 1)

    def _want(self, op, sem, val):
        w = self.waited[op.eng]
        key = id(sem)
        if w.get(key, 0) >= val:
            return
        w[key] = val
        op.waits.append((sem, val))

    def add(self, eng, fn, reads=(), writes=(), dma=False):
        if getattr(self, "muted", False):
            return Op()
        op = Op()
        op.eng, op.fn, op.dma, op.waits = eng, fn, dma, []
        for sem, val in self.pending[eng]:
            self._want(op, sem, val)
        self.pending[eng] = []
        deps = []
        for k in reads:
            w = self.last_w.get(k)
            if w is not None:
                deps.append(w)
        for k in writes:
            w = self.last_w.get(k)
            if w is not None:
                deps.append(w)
            deps.extend(self.readers.get(k, ()))
        for d in deps:
            if d.eng == eng and eng == "pe" and not d.dma and not dma:
                continue
            self._want(op, d.sem, d.val)
        if dma:
            i = self.ndma[eng]
            op.sem, op.val = self._dsem(eng, i)
            if i >= KDMA:
                self._want(op, op.sem, op.val - 16)
            self.ndma[eng] = i + 1
        else:
            i = self.ncomp[eng]
            op.sem, op.val = self._csem(eng, i)
            self.ncomp[eng] = i + 1
        for k in writes:
            self.last_w[k] = op
            self.readers[k] = []
        for k in reads:
            self.readers.setdefault(k, []).append(op)
        self.ops[eng].append(op)
        return op

    def add_cc(self, fn, reads=(), writes=()):
        if getattr(self, "muted", False):
            return Op()
        op = self.add("pool", fn, reads, writes)
        self.ncomp["pool"] -= 1
        op.sem, op.val = self._sem("cc%d" % self.nsem), 1
        op.dma = None
        self.cc_ops = getattr(self, "cc_ops", []) + [op]
        return op

    def _latest(self):
        out = []
        for e in ENGS:
            n = self.ncomp[e]
            if n:
                out.append(self._csem(e, n - 1))
            nd = self.ndma[e]
            for j in range(max(0, nd - KDMA), nd):
                out.append(self._dsem(e, j))
        for op in getattr(self, "cc_ops", []):
            out.append((op.sem, op.val))
        return out

    def barrier(self):
        if getattr(self, "muted", False):
            return
        lat = self._latest()
        for e in ENGS:
            self.pending[e] = list(lat)

    def emit(self):
        final = self._latest()
        with self.nc.Block() as block:
            def run(ename):
                def body(e):
                    for op in self.ops[ename]:
                        for sem, val in op.waits:
                            e.wait_ge(sem, val)
                        ins = op.fn(e)
                        if op.dma is None:
                            ins.then_inc(op.sem)
                        else:
                            ins.then_inc(op.sem, 16 if op.dma else 1)
                    if ename == "sp":
                        for sem, val in final:
                            e.wait_ge(sem, val)
                return body
            block.tensor(run("pe"))
            block.scalar(run("act"))
            block.vector(run("dve"))
            block.gpsimd(run("pool"))
            block.sync(run("sp"))


class _Stop(Exception):
    pass


def build_program(T, debug=False, stop_after=99.0):
    NT = T // 128
    NTT = NT + 2
    NTO = NT // 4
    TQ = T // 4
    CAP = 2 * T // NE
    CAPL = max(128, ((3 * CAP // 8) + 127) // 128 * 128)
    NST = CAPL // 128
    SC = 384 if CAPL % 384 == 0 else 128
    NSC = CAPL // SC
    WC = 928
    nc = bass.Bass("TRN2", target_bir_lowering=False)
    es = ExitStack()
    P = Prog(nc, es)

    def din(name, shape, dt=F32):
        return nc.dram_tensor(name, list(shape), dt, kind="ExternalInput").ap()

    def dscr(name, shape, dt, dump=False, **kw):
        if debug and dump:
            return nc.dram_tensor(name, list(shape), dt, kind="ExternalOutput").ap()
        return nc.dram_tensor(name, list(shape), dt, **kw).ap()

    def sb(name, shape, dt=F32):
        return es.enter_context(nc.sbuf_tensor(name, list(shape), dt))

    def ps(name, shape, dt=F32):
        return es.enter_context(nc.psum_tensor(name, list(shape), dt))

    xin = din("xin", [NTT * 128, D])
    xrot = din("xrot", [T, D])
    rope = din("rope", [NTT * 128 + 1, 128])
    scT = din("scT", [128, 16])
    w_ada = din("w_ada", [D + 1, 6 * D])
    b_ada_bc = din("b_ada_bc", [129, 6 * D])
    n1_bc = din("n1_bc", [128, D])
    n2_bc = din("n2_bc", [128, D])
    fn_bc = din("fn_bc", [128, D])
    hn_bc = din("hn_bc", [128, 256])
    w_in = din("w_in", [D, WC])
    gwaug = din("gwaug", [33, 128])
    rdl_bc = din("rdl_bc", [128, 2])
    w_out = din("w_out", [D + 1, D])
    w_router = din("w_router", [D, NE])
    w_eg = din("w_eg", [NE, D, FF])
    w_eu = din("w_eu", [NE, D, FF])
    w_ed = din("w_ed", [NE, FF, D])
    cst = din("cst", [128, 1024])
    yidx_d = din("yidx", [128, NT * 4], I32)
    out = nc.dram_tensor("out", [TQ, D], F32, kind="ExternalOutput").ap()

    QKT = dscr("QKT", [NTT, 128, 1024], BF16)
    KK = dscr("KK", [NTT, 128, 512], BF16)
    VV = dscr("VV", [NTT, 128, 256], BF16)
    SG = dscr("SG", [NT, 128, 256], BF16)
    OF = dscr("OF", [NT, 128, 256], F32)
    YL = dscr("YL", [T, 256], BF16)
    YA = dscr("YA", [8 * T, 256], BF16, addr_space="Shared")
    X1 = dscr("X1", [TQ + 128, D], F32, True)
    H2 = dscr("H2", [NTO, 128, RW], BF16)
    XE = [dscr("XE%d" % e, [CAPL + 128, RW], BF16) for e in range(NE)]
    dbg_aff = dscr("dbg_aff", [128, NT * NE], F32, True) if debug else None
    dbg_idx = dscr("dbg_idx", [128, NTO * NE], I32, True) if debug else None

    csb = sb("csb", [128, 1024])
    ident_b = sb("ident_b", [128, 128], BF16)
    triF_b = sb("triF_b", [128, 128], BF16)
    triB_b = sb("triB_b", [128, 128], BF16)
    triS_b = sb("triS_b", [128, 128], BF16)
    ones_b = sb("ones_b", [128, 128], BF16)
    M5 = sb("M5", [128, D])
    ET = sb("ET", [128, NTT * 2])
    ER = sb("ER", [128, 2])
    DQ = sb("DQ", [128, 2])
    DK = sb("DK", [128, 2])
    identF = csb[:, 0:128]
    triF = csb[:, 128:256]
    triB = csb[:, 256:384]
    triS = csb[:, 384:512]
    onesF = csb[:, 512:640]
    IOTA = 640
    EPSC = csb[:, IOTA + 7:IOTA + 8]

    es03 = ExitStack()
    es01 = ExitStack()
    A2 = es03.enter_context(nc.sbuf_tensor("A2", [128, D], F32))
    B2 = es03.enter_context(nc.sbuf_tensor("B2", [128, D], F32))
    M2 = es03.enter_context(nc.sbuf_tensor("M2", [128, D], F32))
    affT = es03.enter_context(nc.sbuf_tensor("affT", [128, NT * NE], F32))
    idxT = es03.enter_context(nc.sbuf_tensor("idxT", [128, NTO * NE], I32))
    yidx = es03.enter_context(nc.sbuf_tensor("yidx_s", [128, NT * 4], I32))
    xinit = es03.enter_context(nc.sbuf_tensor("xinit", [128, RW], BF16))
    A1 = [es01.enter_context(nc.sbuf_tensor("A1_%d" % i, [128, D], F32)) for i in range(2)]
    B1 = [es01.enter_context(nc.sbuf_tensor("B1_%d" % i, [128, D], F32)) for i in range(2)]

    def chk(n):
        if n > stop_after:
            P.muted = True

    P.add("sp", lambda e: e.dma_start(out=csb[:], in_=cst[:, :]), writes=["csb"], dma=True)
    P.add("sp", lambda e: e.dma_start(out=yidx[:], in_=yidx_d[:, :]), writes=["yidx"], dma=True)
    for nm, dst, src in (("ident_b", ident_b, identF), ("triF_b", triF_b, triF), ("triB_b", triB_b, triB),
                         ("triS_b", triS_b, triS), ("ones_b", ones_b, onesF)):
        P.add("dve", lambda e, dst=dst, src=src: e.tensor_copy(out=dst[:], in_=src), reads=["csb"], writes=[nm])
    P.add("dve", lambda e: e.memset(xinit[:], 0.0), writes=["xinit"])
    P.add("dve", lambda e: e.memset(xinit[:, MHI:MHI + 1], float(NTO)), reads=["xinit"], writes=["xinit"])
    P.add("dve", lambda e: e.tensor_copy(out=xinit[:, MLO:MLO + 1], in_=csb[:, IOTA:IOTA + 1]), reads=["xinit", "csb"], writes=["xinit"])
    P.add("pool", lambda e: e.dma_start(out=X1[TQ:TQ + 128, :], in_=xinit[:, 0:D]), reads=["xinit"], writes=["DR_x1"], dma=True)
    for ex_ in range(NE):
        for s_ in range(NST):
            P.add("sp", lambda e, ex_=ex_, s_=s_: e.dma_start(out=XE[ex_][s_ * 128:(s_ + 1) * 128, :], in_=xinit[:]),
                  reads=["xinit"], writes=["XE%d" % ex_], dma=True)

    pm = [es.enter_context(nc.psum_tensor("pm%d" % i, [128, 512], F32)) for i in range(2)]

    with ExitStack() as ph:
        def sbp(name, shape, dt=F32):
            return ph.enter_context(nc.sbuf_tensor(name, list(shape), dt))
        sc = sbp("sc", [128, 16])
        scs = sbp("scs", [128, 16])
        scbc = sbp("scbc", [128, 16, 128])
        wab = [sbp("wab%d" % i, [128, 8, 512]) for i in range(2)]
        bab = [sbp("bab%d" % i, [128, 512]) for i in range(2)]
        nw1 = sbp("nw1", [128, D])
        nw2 = sbp("nw2", [128, D])
        modt = [sbp("modt%d" % i, [128, 512]) for i in range(2)]
        rd = sbp("rd", [128, 2])
        rd2 = sbp("rd2", [128, 2])
        lg = sbp("lg", [128, 2])

        P.add("sp", lambda e: e.dma_start(out=sc[:], in_=scT[:, :]), writes=["sc"], dma=True)
        P.add("sp", lambda e: e.dma_start(out=nw1[:], in_=n1_bc[:, :]), writes=["nw1"], dma=True)
        P.add("sp", lambda e: e.dma_start(out=nw2[:], in_=n2_bc[:, :]), writes=["nw2"], dma=True)
        P.add("sp", lambda e: e.dma_start(out=rd[:], in_=rdl_bc[:, :]), writes=["rd"], dma=True)
        P.add("act", lambda e: e.activation(out=scs[:], in_=sc[:], func=AF.Silu), reads=["sc"], writes=["scs"])
        P.add("dve", lambda e: e.tensor_copy(out=scbc[:], in_=scs[:].unsqueeze(2).to_broadcast([128, 16, 128])),
              reads=["scs"], writes=["scbc"])
        P.add("act", lambda e: e.activation(out=rd2[:], in_=rd[:], func=AF.Exp, scale=-1.0), reads=["rd"], writes=["rd2"])
        P.add("act", lambda e: e.activation(out=lg[:], in_=rd2[:], func=AF.Ln, bias=1.0), reads=["rd2"], writes=["lg"])
        for d in range(2):
            cq = IOTA + 3 + d
            ck = IOTA + 1 + d
            P.add("act", lambda e, d=d, cq=cq: e.activation(out=DQ[:, d:d + 1], in_=lg[:, d:d + 1], func=AF.Exp, scale=csb[:, cq:cq + 1]),
                  reads=["lg", "csb"], writes=["DQ%d" % d])
            P.add("act", lambda e, d=d, ck=ck: e.activation(out=DK[:, d:d + 1], in_=lg[:, d:d + 1], func=AF.Exp, scale=csb[:, ck:ck + 1],
                                                           bias=csb[:, IOTA + 6:IOTA + 7]), reads=["lg", "csb"], writes=["DK%d" % d])
        P.add("act", lambda e: e.activation(out=ER[:], in_=lg[:], func=AF.Exp, scale=-128.0), reads=["lg"], writes=["ER"])

        jobs = [(n, 0) for n in range(12)] + [(n, 1) for n in range(4)]
        for ji, (n, which) in enumerate(jobs):
            bi = ji % 2
            for k in range(8):
                P.add("sp", lambda e, n=n, k=k, bi=bi: e.dma_start(out=wab[bi][:, k, :], in_=w_ada[k * 128:(k + 1) * 128, n * 512:(n + 1) * 512]),
                      writes=["wab%d" % bi], dma=True)
            P.add("sp", lambda e, n=n, bi=bi: e.dma_start(out=bab[bi][:], in_=b_ada_bc[0:128, n * 512:(n + 1) * 512]),
                  writes=["bab%d" % bi], dma=True)
            for k in range(8):
                P.add("pe", lambda e, k=k, bi=bi, which=which: e.matmul(pm[bi][:], lhsT=scbc[:, which * 8 + k, :], rhs=wab[bi][:, k, :],
                                                                        start=(k == 0), stop=(k == 7)),
                      reads=["scbc", "wab%d" % bi], writes=["pm%d" % bi])
            P.add("dve", lambda e, bi=bi: e.tensor_tensor(out=modt[bi][:], in0=pm[bi][:], in1=bab[bi][:], op=ALU.add),
                  reads=["pm%d" % bi, "bab%d" % bi], writes=["modt%d" % bi])
            m, half = n // 2, n % 2
            cs = slice(half * 512, (half + 1) * 512)
            if m == 0:
                P.add("dve", lambda e, bi=bi, cs=cs, which=which: e.tensor_copy(out=B1[which][:, cs], in_=modt[bi][:]),
                      reads=["modt%d" % bi], writes=["B1_%d" % which])
            elif m == 1:
                P.add("dve", lambda e, bi=bi, cs=cs, which=which: e.scalar_tensor_tensor(out=A1[which][:, cs], in0=modt[bi][:], scalar=1.0, in1=nw1[:, cs],
                                                                                         op0=ALU.add, op1=ALU.mult),
                      reads=["modt%d" % bi, "nw1"], writes=["A1_%d" % which])
            elif m == 2:
                P.add("dve", lambda e, bi=bi, cs=cs: e.tensor_copy(out=M2[:, cs], in_=modt[bi][:]), reads=["modt%d" % bi], writes=["M2"])
            elif m == 3:
                P.add("dve", lambda e, bi=bi, cs=cs: e.tensor_copy(out=B2[:, cs], in_=modt[bi][:]), reads=["modt%d" % bi], writes=["B2"])
            elif m == 4:
                P.add("dve", lambda e, bi=bi, cs=cs: e.scalar_tensor_tensor(out=A2[:, cs], in0=modt[bi][:], scalar=1.0, in1=nw2[:, cs],
                                                                            op0=ALU.add, op1=ALU.mult),
                      reads=["modt%d" % bi, "nw2"], writes=["A2"])
            else:
                P.add("dve", lambda e, bi=bi, cs=cs: e.tensor_copy(out=M5[:, cs], in_=modt[bi][:]), reads=["modt%d" % bi], writes=["M5"])
        P.barrier()

    chk(1)
    PA = ps("PA", [128, 1024], BF16)
    PQ = [ps("PQ%d" % i, [128, 512]) for i in range(5)]

    with ExitStack() as ph:
        def sbp(name, shape, dt=F32):
            return ph.enter_context(nc.sbuf_tensor(name, list(shape), dt))
        win = sbp("win", [128, 8, WC], BF16)
        gw = sbp("gw", [33, 128], BF16)
        gzaug = sbp("gzaug", [33, 128], BF16)
        xt = [sbp("xt%d" % i, [128, D]) for i in range(2)]
        rp = [sbp("rp%d" % i, [128, 128]) for i in range(2)]
        junk = sbp("junk", [128, D], BF16)
        ss = sbp("ss", [128, 4])
        htmp = sbp("htmp", [128, D])
        hb = sbp("hb", [128, D], BF16)
        hT = sbp("hT", [128, D], BF16)
        t1 = sbp("t1", [128, 128])
        spl = sbp("spl", [128, 192])
        epos = sbp("epos", [128, 128])
        eneg = sbp("eneg", [128, 128])
        rr = sbp("rr", [128, 256])
        ra = sbp("ra", [128, 128])
        rb = sbp("rb", [128, 128])
        qtok = [sbp("qtok%d" % i, [128, 512], BF16) for i in range(2)]
        ktok = [sbp("ktok%d" % i, [128, 512], BF16) for i in range(2)]
        qkts = [sbp("qkts%d" % i, [128, 1024], BF16) for i in range(2)]
        vvt = [sbp("vvt%d" % i, [128, 256], BF16) for i in range(2)]
        sgt = [sbp("sgt%d" % i, [128, 256], BF16) for i in range(2)]

        for k in range(8):
            P.add("pool", lambda e, k=k: e.dma_start(out=win[:, k, :], in_=w_in[k * 128:(k + 1) * 128, :]), writes=["win"], dma=True)
        P.add("pool", lambda e: e.dma_start(out=gw[:], in_=gwaug[:, :]), writes=["gw"], dma=True)
        P.add("dve", lambda e: e.memset(gzaug[:], 1.0), writes=["gzaug"])
        P.add("dve", lambda e: e.memset(spl[:], 0.0), writes=["spl"])
        for i in range(2):
            P.add("dve", lambda e, i=i: e.memset(qtok[i][:], 0.0), writes=["qtok%d" % i])
            P.add("dve", lambda e, i=i: e.memset(ktok[i][:], 0.0), writes=["ktok%d" % i])

        for tt in range(NTT):
            b = tt % 2
            isctx = tt < 2
            ci = 1 if isctx else 0
            X = "xt%d" % b
            QN, KN = "qtok%d" % b, "ktok%d" % b
            P.add("sp", lambda e, tt=tt, b=b: e.dma_start(out=xt[b][:], in_=xin[tt * 128:(tt + 1) * 128, :]), writes=[X], dma=True)
            P.add("sp", lambda e, tt=tt, b=b: e.dma_start(out=rp[b][:], in_=rope[tt * 128:(tt + 1) * 128, :]), writes=["rp%d" % b], dma=True)
            P.add("act", lambda e, b=b: e.activation(out=junk[:], in_=xt[b][:], func=AF.Square, accum_out=ss[:, 0:1]),
                  reads=[X], writes=["junk", "ss0"])
            P.add("act", lambda e: e.activation(out=ss[:, 1:2], in_=ss[:, 0:1], func=AF.Sqrt, scale=1.0 / D, bias=EPSC),
                  reads=["ss0", "csb"], writes=["ss1"])
            P.add("dve", lambda e: e.reciprocal(out=ss[:, 2:3], in_=ss[:, 1:2]), reads=["ss1"], writes=["ss2"])
            P.add("dve", lambda e, b=b, ci=ci: e.scalar_tensor_tensor(out=htmp[:], in0=xt[b][:], scalar=ss[:, 2:3], in1=A1[ci][:],
                                                                      op0=ALU.mult, op1=ALU.mult),
                  reads=[X, "ss2", "A1_%d" % ci], writes=["htmp"])
            P.add("dve", lambda e, ci=ci: e.tensor_tensor(out=hb[:], in0=htmp[:], in1=B1[ci][:], op=ALU.add),
                  reads=["htmp", "B1_%d" % ci], writes=["hb"])
            for k in range(8):
                P.add("pe", lambda e, k=k: e.transpose(out=PA[:, k * 128:(k + 1) * 128], in_=hb[:, k * 128:(k + 1) * 128], identity=ident_b[:]),
                      reads=["hb", "ident_b"], writes=["PA"])
            P.add("act", lambda e: e.activation(out=hT[:], in_=PA[:], func=AF.Copy), reads=["PA"], writes=["hT"])
            chk(1.1)

            for k in range(8):
                P.add("pe", lambda e, k=k: e.matmul(PQ[4][0:32, 0:128], lhsT=win[:, k, 896:928], rhs=hT[:, k * 128:(k + 1) * 128],
                                                    start=(k == 0), stop=(k == 7)), reads=["win", "hT"], writes=["PQ4"])
            P.add("act", lambda e: e.activation(out=gzaug[0:32, :], in_=PQ[4][0:32, 0:128], func=AF.Copy), reads=["PQ4"], writes=["gzaug"])
            P.add("pe", lambda e: e.matmul(PQ[4][:, 0:128], lhsT=gzaug[:], rhs=gw[:], start=True, stop=True), reads=["gzaug", "gw"], writes=["PQ4"])
            P.add("act", lambda e: e.activation(out=t1[:], in_=PQ[4][:, 0:128], func=AF.Exp, scale=-1.0), reads=["PQ4"], writes=["t1"])
            P.add("act", lambda e: e.activation(out=spl[:, 0:128], in_=t1[:], func=AF.Ln, bias=1.0), reads=["t1", "spl"], writes=["spl"])
            P.add("pe", lambda e: e.matmul(PQ[4][:, 0:64], lhsT=triF, rhs=spl[:, 0:64], start=True, stop=True), reads=["spl", "csb"], writes=["PQ4"])
            P.add("pe", lambda e: e.matmul(PQ[4][:, 64:128], lhsT=triB, rhs=spl[:, 64:128], start=True, stop=True), reads=["spl", "csb"], writes=["PQ4"])
            for d in range(2):
                P.add("pe", lambda e, d=d: e.matmul(PQ[3][:, d:d + 1], lhsT=spl[:, d * 64:d * 64 + 128], rhs=csb[:, 512:513], start=True, stop=True),
                      reads=["spl", "csb"], writes=["PQ3"])
            P.add("act", lambda e: e.activation(out=epos[:], in_=PQ[4][:, 0:128], func=AF.Exp, scale=-1.0 / 16), reads=["PQ4"], writes=["epos"])
            P.add("act", lambda e: e.activation(out=eneg[:], in_=PQ[4][:, 0:128], func=AF.Exp, scale=1.0 / 16), reads=["PQ4"], writes=["eneg"])
            P.add("act", lambda e, tt=tt: e.activation(out=ET[0:64, tt * 2:(tt + 1) * 2], in_=PQ[3][0:64, 0:2], func=AF.Exp, scale=-1.0 / 16),
                  reads=["PQ3"], writes=["ET"])

            chk(1.2)
            for k in range(8):
                P.add("pe", lambda e, k=k: e.matmul(PQ[0][:, 0:384], lhsT=hT[:, k * 128:(k + 1) * 128], rhs=win[:, k, 0:384], start=(k == 0), stop=(k == 7)),
                      reads=["win", "hT"], writes=["PQ0"])
            for d in range(2):
                P.add("dve", lambda e, d=d, b=b: e.scalar_tensor_tensor(out=qtok[b][:, d * 256:d * 256 + 64], in0=PQ[0][:, 0:64], scalar=0.125,
                                                                        in1=epos[:, d * 64:(d + 1) * 64], op0=ALU.mult, op1=ALU.mult),
                      reads=["PQ0", "epos"], writes=[QN])
                P.add("dve", lambda e, d=d, b=b: e.tensor_tensor(out=ktok[b][:, d * 256:d * 256 + 64], in0=PQ[0][:, 64:128], in1=eneg[:, d * 64:(d + 1) * 64], op=ALU.mult),
                      reads=["PQ0", "eneg"], writes=[KN])
            qk3 = lambda: PQ[0][:, 128:384].rearrange("p (h c) -> p h c", h=2)
            cosb = lambda b=b: rp[b][:, 0:64].unsqueeze(1).to_broadcast([128, 2, 64])
            sinb = lambda b=b: rp[b][:, 64:128].unsqueeze(1).to_broadcast([128, 2, 64])
            r3 = lambda t: t[:].rearrange("p (h c) -> p h c", h=2)
            R = "rp%d" % b
            P.add("dve", lambda e, cosb=cosb: e.tensor_tensor(out=r3(ra), in0=qk3()[:, :, 0:64], in1=cosb(), op=ALU.mult), reads=["PQ0", R], writes=["ra"])
            P.add("dve", lambda e, sinb=sinb: e.tensor_tensor(out=r3(rb), in0=qk3()[:, :, 64:128], in1=sinb(), op=ALU.mult), reads=["PQ0", R], writes=["rb"])
            P.add("dve", lambda e: e.tensor_tensor(out=r3(rr)[:, :, 0:64], in0=r3(ra), in1=r3(rb), op=ALU.subtract), reads=["ra", "rb"], writes=["rr"])
            P.add("dve", lambda e, sinb=sinb: e.tensor_tensor(out=r3(ra), in0=qk3()[:, :, 0:64], in1=sinb(), op=ALU.mult), reads=["PQ0", R, "rr"], writes=["ra"])
            P.add("dve", lambda e, cosb=cosb: e.tensor_tensor(out=r3(rb), in0=qk3()[:, :, 64:128], in1=cosb(), op=ALU.mult), reads=["PQ0", R, "rr"], writes=["rb"])
            P.add("dve", lambda e: e.tensor_tensor(out=r3(rr)[:, :, 64:128], in0=r3(ra), in1=r3(rb), op=ALU.add), reads=["ra", "rb"], writes=["rr"])
            for d in range(2):
                P.add("dve", lambda e, d=d, b=b: e.tensor_scalar(out=qtok[b][:, d * 256 + 128:(d + 1) * 256], in0=rr[:, 0:128], scalar1=DQ[:, d:d + 1], scalar2=None, op0=ALU.mult),
                      reads=["rr", "DQ%d" % d], writes=[QN])
                P.add("dve", lambda e, d=d, b=b: e.tensor_scalar(out=ktok[b][:, d * 256 + 128:(d + 1) * 256], in0=rr[:, 128:256], scalar1=DK[:, d:d + 1], scalar2=None, op0=ALU.mult),
                      reads=["rr", "DK%d" % d], writes=[KN])
            chk(1.3)
            for k in range(8):
                P.add("pe", lambda e, k=k: e.matmul(PQ[1][:], lhsT=hT[:, k * 128:(k + 1) * 128], rhs=win[:, k, 384:896], start=(k == 0), stop=(k == 7)),
                      reads=["win", "hT"], writes=["PQ1"])
            P.add("act", lambda e, b=b: e.activation(out=vvt[b][:], in_=PQ[1][:, 0:256], func=AF.Copy), reads=["PQ1"], writes=["vvt%d" % b])
            if not isctx:
                P.add("act", lambda e, b=b: e.activation(out=sgt[b][:], in_=PQ[1][:, 256:512], func=AF.Silu), reads=["PQ1"], writes=["sgt%d" % b])
            chk(1.4)
            for d in range(2):
                for j, (src, sn) in enumerate(((qtok[b], QN), (ktok[b], KN))):
                    for blk in range(2):
                        c0 = d * 512 + j * 256 + blk * 128
                        P.add("pe", lambda e, src=src, d=d, blk=blk, c0=c0: e.transpose(out=PA[:, c0:c0 + 128], in_=src[:, d * 256 + blk * 128: d * 256 + (blk + 1) * 128],
                                                                                      identity=ident_b[:]), reads=[sn, "ident_b"], writes=["PA"])
            P.add("act", lambda e, b=b: e.activation(out=qkts[b][:], in_=PA[:], func=AF.Copy), reads=["PA"], writes=["qkts%d" % b])
            P.add("sp", lambda e, b=b, tt=tt: e.dma_start(out=QKT[tt, :, :], in_=qkts[b][:]), reads=["qkts%d" % b], writes=["DR_qk"], dma=True)
            P.add("sp", lambda e, b=b, tt=tt: e.dma_start(out=KK[tt, :, :], in_=ktok[b][:]), reads=[KN], writes=["DR_kk"], dma=True)
            P.add("sp", lambda e, b=b, tt=tt: e.dma_start(out=VV[tt, :, :], in_=vvt[b][:]), reads=["vvt%d" % b], writes=["DR_vv"], dma=True)
            if not isctx:
                P.add("sp", lambda e, b=b, tt=tt: e.dma_start(out=SG[tt - 2, :, :], in_=sgt[b][:]), reads=["sgt%d" % b], writes=["DR_sg"], dma=True)
        P.barrier()
    es01.close()
    chk(2)

    with ExitStack() as ph:
        def sbp(name, shape, dt=F32):
            return ph.enter_context(nc.sbuf_tensor(name, list(shape), dt))
        hnw = sbp("hnw", [128, 256])
        qk = [sbp("qk%d" % i, [128, 512], BF16) for i in range(2)]
        kk = [sbp("kk%d" % i, [128, 256], BF16) for i in range(2)]
        vv = [sbp("vv%d" % i, [128, 256], BF16) for i in range(2)]
        S = sbp("S", [128, 256])
        Sb = sbp("Sb", [128, 256], BF16)
        Tt = [sbp("Tt%d" % i, [128, 128]) for i in range(2)]
        PTs = [sbp("PTs%d" % i, [128, 128], BF16) for i in range(2)]
        ofs = [sbp("ofs%d" % i, [128, 256]) for i in range(2)]
        osum = sbp("osum", [128, 256])
        sq = sbp("sq", [128, 256])
        st = sbp("st", [128, 64])
        sgl = [sbp("sgl%d" % i, [128, 256], BF16) for i in range(2)]
        yb = [sbp("yb%d" % i, [128, 256], BF16) for i in range(2)]

        P.add("sp", lambda e: e.dma_start(out=hnw[:], in_=hn_bc[:, :]), writes=["hnw"], dma=True)
        step = 0
        for d in range(2):
            P.add("dve", lambda e: e.memset(S[:], 0.0), writes=["S0", "S1"])
            P.add("dve", lambda e: e.memset(Sb[:], 0.0), writes=["Sb0", "Sb1"])
            seq = list(range(NTT)) if d == 0 else [1, 0] + list(range(NTT - 1, 1, -1))
            maskb = triF_b if d == 0 else triB_b
            maskn = "triF_b" if d == 0 else "triB_b"
            for tt in seq:
                b = step % 2
                step += 1
                isctx = tt < 2
                lt = tt - 2
                if not isctx:
                    P.add("sp", lambda e, d=d, tt=tt, b=b: e.dma_start(out=qk[b][:], in_=QKT[tt, :, d * 512:(d + 1) * 512]), reads=["DR_qk"], writes=["qk%d" % b], dma=True)
                P.add("sp", lambda e, d=d, tt=tt, b=b: e.dma_start(out=kk[b][:], in_=KK[tt, :, d * 256:(d + 1) * 256]), reads=["DR_kk"], writes=["kk%d" % b], dma=True)
                P.add("sp", lambda e, tt=tt, b=b: e.dma_start(out=vv[b][:], in_=VV[tt, :, :]), reads=["DR_vv"], writes=["vv%d" % b], dma=True)
                if not isctx and d == 1:
                    P.add("sp", lambda e, lt=lt, b=b: e.dma_start(out=ofs[b][:], in_=OF[lt, :, :]), reads=["DR_of"], writes=["ofs%d" % b], dma=True)
                    P.add("sp", lambda e, lt=lt, b=b: e.dma_start(out=sgl[b][:], in_=SG[lt, :, :]), reads=["DR_sg"], writes=["sgl%d" % b], dma=True)
                for hh in range(2):
                    dk = 64 if hh == 0 else 128
                    qc = slice(hh * 128, (hh + 1) * 128)
                    kc_ = slice(256 + hh * 128, 256 + (hh + 1) * 128)
                    hs = slice(hh * 128, (hh + 1) * 128)
                    ktc = slice(0, 64) if hh == 0 else slice(128, 256)
                    if not isctx:
                        P.add("pe", lambda e, b=b, dk=dk, qc=qc, kc_=kc_, hh=hh: e.matmul(PQ[hh][:, 0:128], lhsT=qk[b][0:dk, kc_], rhs=qk[b][0:dk, qc], start=True, stop=True),
                              reads=["qk%d" % b], writes=["PQ%d" % hh])
                        P.add("dve", lambda e, hh=hh, maskb=maskb: e.tensor_tensor(out=PTs[hh][:], in0=PQ[hh][:, 0:128], in1=maskb[:], op=ALU.mult),
                              reads=["PQ%d" % hh, maskn], writes=["PTs%d" % hh])
                        P.add("pe", lambda e, b=b, hh=hh, hs=hs: e.matmul(PQ[2][:, hs], lhsT=PTs[hh][:], rhs=vv[b][:, hs], start=True, stop=False),
                              reads=["PTs%d" % hh, "vv%d" % b], writes=["PQ2"])
                        P.add("pe", lambda e, b=b, dk=dk, qc=qc, hs=hs: e.matmul(PQ[2][:, hs], lhsT=qk[b][0:dk, qc], rhs=Sb[0:dk, hs], start=False, stop=True),
                              reads=["qk%d" % b, "Sb%d" % hh], writes=["PQ2"])
                    pun = "pm%d" % hh
                    P.add("pe", lambda e, b=b, dk=dk, ktc=ktc, hs=hs, hh=hh: e.matmul(pm[hh][0:dk, 0:128], lhsT=kk[b][:, ktc], rhs=vv[b][:, hs], start=True, stop=True),
                          reads=["kk%d" % b, "vv%d" % b], writes=[pun])
                    if hh == 0:
                        eap = lambda tt=tt, d=d: ET[0:64, tt * 2 + d: tt * 2 + d + 1]
                        en = "ET"
                    else:
                        eap = lambda d=d: ER[:, d:d + 1]
                        en = "ER"
                    P.add("dve", lambda e, hh=hh, dk=dk, hs=hs: e.tensor_tensor(out=Tt[hh][0:dk, :], in0=pm[hh][0:dk, 0:128], in1=S[0:dk, hs], op=ALU.add),
                          reads=[pun, "S%d" % hh], writes=["Tt%d" % hh])
                    P.add("act", lambda e, hh=hh, dk=dk, hs=hs, eap=eap: e.activation(out=S[0:dk, hs], in_=Tt[hh][0:dk, :], func=AF.Copy, scale=eap()),
                          reads=["Tt%d" % hh, en], writes=["S%d" % hh])
                    P.add("dve", lambda e, hh=hh, dk=dk, hs=hs, eap=eap: e.tensor_scalar(out=Sb[0:dk, hs], in0=Tt[hh][0:dk, :], scalar1=eap(), scalar2=None, op0=ALU.mult),
                          reads=["Tt%d" % hh, en], writes=["Sb%d" % hh])
                if isctx:
                    continue
                if d == 0:
                    P.add("act", lambda e, b=b: e.activation(out=ofs[b][:], in_=PQ[2][:, 0:256], func=AF.Copy), reads=["PQ2"], writes=["ofs%d" % b])
                    P.add("sp", lambda e, b=b, lt=lt: e.dma_start(out=OF[lt, :, :], in_=ofs[b][:]), reads=["ofs%d" % b], writes=["DR_of"], dma=True)
                    continue
                P.add("dve", lambda e, b=b: e.tensor_tensor(out=osum[:], in0=PQ[2][:, 0:256], in1=ofs[b][:], op=ALU.add), reads=["PQ2", "ofs%d" % b], writes=["osum"])
                o3 = lambda: osum[:].rearrange("p (h c) -> p h c", h=2)
                s3 = lambda: sq[:].rearrange("p (h c) -> p h c", h=2)
                P.add("dve", lambda e: e.tensor_reduce(out=st[:, 0:2], in_=o3(), axis=AX.X, op=ALU.add), reads=["osum"], writes=["st_s1"])
                P.add("dve", lambda e: e.tensor_tensor(out=sq[:], in0=osum[:], in1=osum[:], op=ALU.mult), reads=["osum"], writes=["sq"])
                P.add("dve", lambda e: e.tensor_reduce(out=st[:, 8:10], in_=s3(), axis=AX.X, op=ALU.add), reads=["sq"], writes=["st_s2"])
                P.add("dve", lambda e: e.tensor_scalar(out=st[:, 16:18], in0=st[:, 0:2], scalar1=1.0 / 128, scalar2=None, op0=ALU.mult), reads=["st_s1"], writes=["st_m"])
                P.add("dve", lambda e: e.memset(st[:, 16:17], 0.0), reads=["st_m"], writes=["st_m"])
                P.add("dve", lambda e: e.tensor_tensor(out=st[:, 24:26], in0=st[:, 16:18], in1=st[:, 16:18], op=ALU.mult), reads=["st_m"], writes=["st_mm"])
                P.add("dve", lambda e: e.scalar_tensor_tensor(out=st[:, 32:34], in0=st[:, 8:10], scalar=1.0 / 128, in1=st[:, 24:26], op0=ALU.mult, op1=ALU.subtract),
                      reads=["st_s2", "st_mm"], writes=["st_v"])
                P.add("act", lambda e: e.activation(out=st[:, 40:42], in_=st[:, 32:34], func=AF.Sqrt, bias=EPSC), reads=["st_v", "csb"], writes=["st_sd"])
                P.add("dve", lambda e: e.reciprocal(out=st[:, 48:50], in_=st[:, 40:42]), reads=["st_sd"], writes=["st_r"])
                P.add("dve", lambda e: e.scalar_tensor_tensor(out=st[:, 56:58], in0=st[:, 16:18], scalar=-1.0, in1=st[:, 48:50], op0=ALU.mult, op1=ALU.mult),
                      reads=["st_m", "st_r"], writes=["st_sh"])
                P.add("dve", lambda e: e.tensor_tensor(out=s3(), in0=o3(), in1=st[:, 48:50].unsqueeze(2).to_broadcast([128, 2, 128]), op=ALU.mult),
                      reads=["osum", "st_r", "sq"], writes=["sq"])
                P.add("dve", lambda e: e.tensor_tensor(out=o3(), in0=s3(), in1=st[:, 56:58].unsqueeze(2).to_broadcast([128, 2, 128]), op=ALU.add),
                      reads=["sq", "st_sh"], writes=["osum"])
                P.add("dve", lambda e: e.tensor_tensor(out=sq[:], in0=osum[:], in1=hnw[:], op=ALU.mult), reads=["osum", "hnw"], writes=["sq"])
                P.add("dve", lambda e, b=b: e.tensor_tensor(out=yb[b][:], in0=sq[:], in1=sgl[b][:], op=ALU.mult), reads=["sq", "sgl%d" % b], writes=["yb%d" % b])
                P.add("sp", lambda e, b=b, lt=lt: e.dma_start(out=YL[lt * 128:(lt + 1) * 128, :], in_=yb[b][:]), reads=["yb%d" % b], writes=["DR_yl"], dma=True)
        P.barrier()

    chk(3)
    P.add_cc(lambda e: e.collective_compute("AllGather", ALU.bypass, replica_groups=[list(range(8))], ins=[YL[:, :]], outs=[YA[:, :]]),
             reads=["DR_yl"], writes=["DR_ya"])
    P.barrier()

    chk(4)
    with ExitStack() as ph:
        def sbp(name, shape, dt=F32):
            return ph.enter_context(nc.sbuf_tensor(name, list(shape), dt))
        wo = sbp("wo", [128, 8, D], BF16)
        wr = sbp("wr", [128, 8, NE], BF16)
        yg = [sbp("yg%d" % i, [128, D], BF16) for i in range(2)]
        xl = [sbp("xl%d" % i, [128, D]) for i in range(2)]
        yT = sbp("yT", [128, D], BF16)
        sqB = sbp("sq2", [128, D])
        stB = sbp("st2", [128, 16])
        x1t = [sbp("x1t%d" % i, [128, D]) for i in range(2)]
        junk2 = sbp("junk2", [128, D], BF16)
        h2r = [sbp("h2r%d" % i, [128, RW], BF16) for i in range(2)]
        h2T = sbp("h2T", [128, D], BF16)
        ex = sbp("ex", [128, NE])
        gtmp = sbp("gtmp", [128, NE])
        for k in range(8):
            P.add("pool", lambda e, k=k: e.dma_start(out=wo[:, k, :], in_=w_out[k * 128:(k + 1) * 128, :]), writes=["wo"], dma=True)
            P.add("pool", lambda e, k=k: e.dma_start(out=wr[:, k, :], in_=w_router[k * 128:(k + 1) * 128, :]), writes=["wr"], dma=True)
        for i in range(2):
            P.add("dve", lambda e, i=i: e.memset(h2r[i][:, D:RW], 0.0), writes=["h2r%d" % i])
        for it in range(NT):
            b = it % 2
            own = it < NTO
            for r in range(4):
                P.add("pool", lambda e, it=it, r=r, b=b: e.indirect_dma_start(
                    out=yg[b][:, r * 256:(r + 1) * 256], out_offset=None, in_=YA[:, :],
                    in_offset=bass.IndirectOffsetOnAxis(ap=yidx[:, it * 4 + r: it * 4 + r + 1], axis=0)),
                    reads=["DR_ya", "yidx"], writes=["yg%d" % b], dma=True)
            P.add("sp", lambda e, it=it, b=b: e.dma_start(out=xl[b][:], in_=xrot[it * 128:(it + 1) * 128, :]), writes=["xl%d" % b], dma=True)
            for k in range(8):
                P.add("pe", lambda e, k=k, b=b: e.transpose(out=PA[:, k * 128:(k + 1) * 128], in_=yg[b][:, k * 128:(k + 1) * 128], identity=ident_b[:]),
                      reads=["yg%d" % b, "ident_b"], writes=["PA"])
            P.add("act", lambda e: e.activation(out=yT[:], in_=PA[:], func=AF.Copy), reads=["PA"], writes=["yT"])
            for nch in range(2):
                for k in range(8):
                    P.add("pe", lambda e, k=k, nch=nch: e.matmul(PQ[2 + nch][:], lhsT=yT[:, k * 128:(k + 1) * 128], rhs=wo[:, k, nch * 512:(nch + 1) * 512],
                                                                 start=(k == 0), stop=(k == 7)), reads=["yT", "wo"], writes=["PQ%d" % (2 + nch)])
                cs = slice(nch * 512, (nch + 1) * 512)
                P.add("dve", lambda e, nch=nch, cs=cs: e.tensor_tensor(out=sqB[:, cs], in0=PQ[2 + nch][:], in1=M2[:, cs], op=ALU.mult),
                      reads=["PQ%d" % (2 + nch), "M2"], writes=["sq"])
            P.add("dve", lambda e, b=b: e.tensor_tensor(out=x1t[b][:], in0=sqB[:], in1=xl[b][:], op=ALU.add), reads=["sq", "xl%d" % b], writes=["x1t%d" % b])
            if own:
                P.add("sp", lambda e, b=b, it=it: e.dma_start(out=X1[it * 128:(it + 1) * 128, :], in_=x1t[b][:]), reads=["x1t%d" % b], writes=["DR_x1"], dma=True)
            P.add("act", lambda e, b=b: e.activation(out=junk2[:], in_=x1t[b][:], func=AF.Square, accum_out=stB[:, 0:1]), reads=["x1t%d" % b], writes=["junk2", "st_s1"])
            P.add("act", lambda e: e.activation(out=stB[:, 1:2], in_=stB[:, 0:1], func=AF.Sqrt, scale=1.0 / D, bias=EPSC), reads=["st_s1", "csb"], writes=["st_q1"])
            P.add("dve", lambda e: e.reciprocal(out=stB[:, 2:3], in_=stB[:, 1:2]), reads=["st_q1"], writes=["st_q2"])
            P.add("dve", lambda e, b=b: e.scalar_tensor_tensor(out=sqB[:], in0=x1t[b][:], scalar=stB[:, 2:3], in1=A2[:], op0=ALU.mult, op1=ALU.mult),
                  reads=["x1t%d" % b, "st_q2", "A2", "sq"], writes=["sq"])
            H = "h2r%d" % b
            P.add("dve", lambda e, b=b: e.tensor_tensor(out=h2r[b][:, 0:D], in0=sqB[:], in1=B2[:], op=ALU.add), reads=["sq", "B2"], writes=[H])
            for k in range(8):
                P.add("pe", lambda e, k=k, b=b: e.transpose(out=PA[:, k * 128:(k + 1) * 128], in_=h2r[b][:, k * 128:(k + 1) * 128], identity=ident_b[:]),
                      reads=[H, "ident_b"], writes=["PA"])
            P.add("act", lambda e: e.activation(out=h2T[:], in_=PA[:], func=AF.Copy), reads=["PA"], writes=["h2T"])
            for k in range(8):
                P.add("pe", lambda e, k=k: e.matmul(PQ[4][:, 0:NE], lhsT=h2T[:, k * 128:(k + 1) * 128], rhs=wr[:, k, :], start=(k == 0), stop=(k == 7)),
                      reads=["h2T", "wr"], writes=["PQ4"])
            P.add("dve", lambda e: e.tensor_reduce(out=stB[:, 4:5], in_=PQ[4][:, 0:NE], axis=AX.X, op=ALU.max), reads=["PQ4"], writes=["st_mx"])
            P.add("dve", lambda e: e.tensor_scalar(out=stB[:, 5:6], in0=stB[:, 4:5], scalar1=-1.0, scalar2=None, op0=ALU.mult), reads=["st_mx"], writes=["st_nmx"])
            P.add("act", lambda e: e.activation(out=ex[:], in_=PQ[4][:, 0:NE], func=AF.Exp, bias=stB[:, 5:6], accum_out=stB[:, 6:7]), reads=["PQ4", "st_nmx"], writes=["ex", "st_sm"])
            P.add("dve", lambda e: e.reciprocal(out=stB[:, 7:8], in_=stB[:, 6:7]), reads=["st_sm"], writes=["st_rs"])
            asl = slice(it * NE, (it + 1) * NE)
            P.add("dve", lambda e, asl=asl: e.tensor_scalar(out=affT[:, asl], in0=ex[:], scalar1=stB[:, 7:8], scalar2=None, op0=ALU.mult),
                  reads=["ex", "st_rs"], writes=["affT"])
            if own:
                P.add("dve", lambda e, b=b, asl=asl: e.tensor_copy(out=h2r[b][:, MG:MG + 16], in_=affT[:, asl]), reads=["affT"], writes=[H])
                P.add("dve", lambda e, b=b, asl=asl: e.tensor_tensor(out=gtmp[:], in0=affT[:, asl], in1=h2r[b][:, MG:MG + 16], op=ALU.subtract), reads=["affT", H], writes=["gtmp"])
                P.add("dve", lambda e, b=b: e.tensor_copy(out=h2r[b][:, MG + 16:MG + 32], in_=gtmp[:]), reads=["gtmp"], writes=[H])
                P.add("dve", lambda e, b=b: e.tensor_tensor(out=gtmp[:], in0=gtmp[:], in1=h2r[b][:, MG + 16:MG + 32], op=ALU.subtract), reads=["gtmp", H], writes=["gtmp"])
                P.add("dve", lambda e, b=b: e.tensor_copy(out=h2r[b][:, MG + 32:MG + 48], in_=gtmp[:]), reads=["gtmp"], writes=[H])
                P.add("dve", lambda e, b=b, it=it: e.memset(h2r[b][:, MHI:MHI + 1], float(it)), reads=[H], writes=[H])
                P.add("dve", lambda e, b=b: e.tensor_copy(out=h2r[b][:, MLO:MLO + 1], in_=csb[:, IOTA:IOTA + 1]), reads=[H, "csb"], writes=[H])
                P.add("sp", lambda e, b=b, it=it: e.dma_start(out=H2[it, :, :], in_=h2r[b][:]), reads=[H], writes=["DR_h2"], dma=True)
        P.barrier()

    chk(5)
    with ExitStack() as ph:
        def sbp(name, shape, dt=F32):
            return ph.enter_context(nc.sbuf_tensor(name, list(shape), dt))
        NTE = NT * NE
        NOE = NTO * NE
        lo = sbp("lo", [128, NE])
        hi_ = sbp("hi", [128, NE])
        mid = sbp("mid", [128, NE])
        cmp_ = sbp("cmp", [128, NTE])
        cnt = sbp("cnt", [128, NE])
        ge = sbp("ge", [128, NE])
        d1 = sbp("d1", [128, NE])
        d2 = sbp("d2", [128, NE])
        maskb_ = sbp("maskb", [128, NOE], BF16)
        wn = sbp("wn", [128, NOE])
        tots = sbp("tots", [128, NOE])
        offs = sbp("offs", [128, NOE])
        pos = sbp("pos", [128, NOE])
        sel = sbp("sel", [128, NOE])
        hrow = [sbp("hrow%d" % i, [128, RW], BF16) for i in range(2)]

        a3 = lambda t: t[:].rearrange("p (t e) -> p t e", e=NE)
        P.add("dve", lambda e: e.memset(lo[:], 0.0), writes=["lo"])
        P.add("dve", lambda e: e.memset(hi_[:], 2.0), writes=["hi"])
        for it in range(40):
            P.add("dve", lambda e: e.tensor_tensor(out=mid[:], in0=lo[:], in1=hi_[:], op=ALU.add), reads=["lo", "hi"], writes=["mid"])
            P.add("dve", lambda e: e.tensor_scalar(out=mid[:], in0=mid[:], scalar1=0.5, scalar2=None, op0=ALU.mult), reads=["mid"], writes=["mid"])
            P.add("dve", lambda e: e.tensor_tensor(out=a3(cmp_), in0=a3(affT), in1=mid[:].unsqueeze(1).to_broadcast([128, NT, NE]), op=ALU.is_ge),
                  reads=["affT", "mid"], writes=["cmp"])
            P.add("dve", lambda e: e.tensor_reduce(out=cnt[:], in_=cmp_[:].rearrange("p (t e) -> p e t", e=NE), axis=AX.X, op=ALU.add), reads=["cmp"], writes=["cnt"])
            P.add("pe", lambda e: e.matmul(PQ[0][:, 0:NE], lhsT=onesF, rhs=cnt[:], start=True, stop=True), reads=["cnt", "csb"], writes=["PQ0"])
            P.add("dve", lambda e: e.tensor_scalar(out=ge[:], in0=PQ[0][:, 0:NE], scalar1=float(CAP) - 0.5, scalar2=None, op0=ALU.is_ge), reads=["PQ0"], writes=["ge"])
            P.add("dve", lambda e: e.tensor_tensor(out=d1[:], in0=mid[:], in1=lo[:], op=ALU.subtract), reads=["mid", "lo"], writes=["d1"])
            P.add("dve", lambda e: e.tensor_tensor(out=d2[:], in0=hi_[:], in1=mid[:], op=ALU.subtract), reads=["mid", "hi"], writes=["d2"])
            P.add("dve", lambda e: e.tensor_tensor(out=d1[:], in0=d1[:], in1=ge[:], op=ALU.mult), reads=["d1", "ge"], writes=["d1"])
            P.add("dve", lambda e: e.tensor_tensor(out=d2[:], in0=d2[:], in1=ge[:], op=ALU.mult), reads=["d2", "ge"], writes=["d2"])
            P.add("dve", lambda e: e.tensor_tensor(out=lo[:], in0=lo[:], in1=d1[:], op=ALU.add), reads=["lo", "d1"], writes=["lo"])
            P.add("dve", lambda e: e.tensor_tensor(out=hi_[:], in0=mid[:], in1=d2[:], op=ALU.add), reads=["mid", "d2"], writes=["hi"])
        P.add("dve", lambda e: e.tensor_tensor(out=cmp_[:, 0:NOE].rearrange("p (t e) -> p t e", e=NE), in0=affT[:, 0:NOE].rearrange("p (t e) -> p t e", e=NE),
                                               in1=lo[:].unsqueeze(1).to_broadcast([128, NTO, NE]), op=ALU.is_ge),
              reads=["affT", "lo"], writes=["cmp"])
        P.add("dve", lambda e: e.tensor_copy(out=maskb_[:], in_=cmp_[:, 0:NOE]), reads=["cmp"], writes=["maskb"])
        for c0 in range(0, NOE, 512):
            c1 = min(NOE, c0 + 512)
            w = c1 - c0
            P.add("pe", lambda e, c0=c0, c1=c1, w=w: e.matmul(PQ[0][:, 0:w], lhsT=triS_b[:], rhs=maskb_[:, c0:c1], start=True, stop=True),
                  reads=["maskb", "triS_b"], writes=["PQ0"])
            P.add("pe", lambda e, c0=c0, c1=c1, w=w: e.matmul(PQ[1][:, 0:w], lhsT=ones_b[:], rhs=maskb_[:, c0:c1], start=True, stop=True),
                  reads=["maskb", "ones_b"], writes=["PQ1"])
            P.add("act", lambda e, c0=c0, c1=c1, w=w: e.activation(out=wn[:, c0:c1], in_=PQ[0][:, 0:w], func=AF.Copy), reads=["PQ0"], writes=["wn"])
            P.add("act", lambda e, c0=c0, c1=c1, w=w: e.activation(out=tots[:, c0:c1], in_=PQ[1][:, 0:w], func=AF.Copy), reads=["PQ1"], writes=["tots"])
        P.add("dve", lambda e: e.memset(offs[:, 0:NE], 0.0), writes=["offs"])
        for t in range(1, NTO):
            P.add("dve", lambda e, t=t: e.tensor_tensor(out=offs[:, t * NE:(t + 1) * NE], in0=offs[:, (t - 1) * NE:t * NE], in1=tots[:, (t - 1) * NE:t * NE], op=ALU.add),
                  reads=["offs", "tots"], writes=["offs"])
        P.add("dve", lambda e: e.tensor_tensor(out=pos[:], in0=wn[:], in1=offs[:], op=ALU.add), reads=["wn", "offs"], writes=["pos"])
        P.add("dve", lambda e: e.tensor_scalar(out=sel[:], in0=pos[:], scalar1=float(CAPL) - 0.5, scalar2=None, op0=ALU.is_lt), reads=["pos"], writes=["sel"])
        P.add("dve", lambda e: e.tensor_tensor(out=sel[:], in0=sel[:], in1=cmp_[:, 0:NOE], op=ALU.mult), reads=["sel", "cmp"], writes=["sel"])
        P.add("dve", lambda e: e.tensor_scalar(out=pos[:], in0=pos[:], scalar1=csb[:, IOTA + 5:IOTA + 6], scalar2=None, op0=ALU.subtract), reads=["pos", "csb"], writes=["pos"])
        P.add("dve", lambda e: e.tensor_tensor(out=pos[:], in0=pos[:], in1=sel[:], op=ALU.mult), reads=["pos", "sel"], writes=["pos"])
        P.add("dve", lambda e: e.tensor_scalar(out=pos[:], in0=pos[:], scalar1=csb[:, IOTA + 5:IOTA + 6], scalar2=None, op0=ALU.add), reads=["pos", "csb"], writes=["pos"])
        P.add("dve", lambda e: e.tensor_copy(out=idxT[:], in_=pos[:]), reads=["pos"], writes=["idxT"])
        if debug:
            P.add("sp", lambda e: e.dma_start(out=dbg_aff[:, :], in_=affT[:]), reads=["affT"], writes=["dbg1"], dma=True)
            P.add("sp", lambda e: e.dma_start(out=dbg_idx[:, :], in_=idxT[:]), reads=["idxT"], writes=["dbg2"], dma=True)
        for lt in range(NTO):
            b = lt % 2
            P.add("sp", lambda e, lt=lt, b=b: e.dma_start(out=hrow[b][:], in_=H2[lt, :, :]), reads=["DR_h2"], writes=["hrow%d" % b], dma=True)
            for ex_ in range(NE):
                P.add("pool", lambda e, lt=lt, b=b, ex_=ex_: e.indirect_dma_start(
                    out=XE[ex_][:, :], out_offset=bass.IndirectOffsetOnAxis(ap=idxT[:, lt * NE + ex_: lt * NE + ex_ + 1], axis=0),
                    in_=hrow[b][:], in_offset=None), reads=["hrow%d" % b, "idxT"], writes=["XE%d" % ex_], dma=True)
        P.barrier()
    es03.close()
    chk(6)

    with ExitStack() as ph:
        def sbp(name, shape, dt=F32):
            return ph.enter_context(nc.sbuf_tensor(name, list(shape), dt))
        wg = [sbp("wg%d" % i, [128, 8, FF], BF16) for i in range(2)]
        wu = [sbp("wu%d" % i, [128, 8, FF], BF16) for i in range(2)]
        wd = [sbp("wd%d" % i, [128, NF, D], BF16) for i in range(2)]
        xrow = [sbp("xrow%d" % i, [128, RW], BF16) for i in range(2)]
        xeT = sbp("xeT", [128, 8, CAPL], BF16)
        gateT = sbp("gateT", [128, NST])
        gt2 = sbp("gt2", [128, 4])
        tokI = sbp("tokI", [128, NST], I32)
        sgx = sbp("sgx", [128, SC])
        hidT = sbp("hidT", [128, NF, SC], BF16)
        ysb = [sbp("ysb%d" % i, [128, D]) for i in range(2)]

        def load_w(ex_):
            wb = ex_ % 2
            for k in range(8):
                P.add("pool", lambda e, ex_=ex_, k=k, wb=wb: e.dma_start(out=wg[wb][:, k, :], in_=w_eg[ex_, k * 128:(k + 1) * 128, :]), writes=["wg%d" % wb], dma=True)
                P.add("pool", lambda e, ex_=ex_, k=k, wb=wb: e.dma_start(out=wu[wb][:, k, :], in_=w_eu[ex_, k * 128:(k + 1) * 128, :]), writes=["wu%d" % wb], dma=True)
            for f in range(NF):
                P.add("pool", lambda e, ex_=ex_, f=f, wb=wb: e.dma_start(out=wd[wb][:, f, :], in_=w_ed[ex_, f * 128:(f + 1) * 128, :]), writes=["wd%d" % wb], dma=True)

        load_w(0)
        yi = 0
        for ex_ in range(NE):
            wb = ex_ % 2
            if ex_ + 1 < NE:
                load_w(ex_ + 1)
            for s_ in range(NST):
                b = s_ % 2
                XR = "xrow%d" % b
                P.add("sp", lambda e, ex_=ex_, s_=s_, b=b: e.dma_start(out=xrow[b][:], in_=XE[ex_][s_ * 128:(s_ + 1) * 128, :]),
                      reads=["XE%d" % ex_], writes=[XR], dma=True)
                for k in range(8):
                    P.add("pe", lambda e, k=k, b=b: e.transpose(out=PA[:, k * 128:(k + 1) * 128], in_=xrow[b][:, k * 128:(k + 1) * 128], identity=ident_b[:]),
                          reads=[XR, "ident_b"], writes=["PA"])
                P.add("act", lambda e, s_=s_: e.activation(out=xeT[:, :, s_ * 128:(s_ + 1) * 128], in_=PA[:].rearrange("p (k c) -> p k c", k=8), func=AF.Copy),
                      reads=["PA"], writes=["xeT"])
                P.add("dve", lambda e, b=b, ex_=ex_: e.tensor_tensor(out=gt2[:, 0:1], in0=xrow[b][:, MG + ex_:MG + ex_ + 1], in1=xrow[b][:, MG + 16 + ex_:MG + 17 + ex_], op=ALU.add),
                      reads=[XR], writes=["gt2a"])
                P.add("dve", lambda e, b=b, ex_=ex_, s_=s_: e.tensor_tensor(out=gateT[:, s_:s_ + 1], in0=gt2[:, 0:1], in1=xrow[b][:, MG + 32 + ex_:MG + 33 + ex_], op=ALU.add),
                      reads=[XR, "gt2a"], writes=["gateT"])
                P.add("dve", lambda e, b=b: e.scalar_tensor_tensor(out=gt2[:, 1:2], in0=xrow[b][:, MHI:MHI + 1], scalar=128.0, in1=xrow[b][:, MLO:MLO + 1], op0=ALU.mult, op1=ALU.add),
                      reads=[XR], writes=["gt2b"])
                P.add("dve", lambda e, s_=s_: e.tensor_copy(out=tokI[:, s_:s_ + 1], in_=gt2[:, 1:2]), reads=["gt2b"], writes=["tokI"])
            for sc_ in range(NSC):
                ss_ = slice(sc_ * SC, (sc_ + 1) * SC)
                for f in range(NF):
                    pg = f % 2
                    for k in range(8):
                        P.add("pe", lambda e, k=k, f=f, pg=pg, ss_=ss_, wb=wb: e.matmul(PQ[pg][:, 0:SC], lhsT=wg[wb][:, k, f * 128:(f + 1) * 128], rhs=xeT[:, k, ss_],
                                                                                      start=(k == 0), stop=(k == 7)), reads=["wg%d" % wb, "xeT"], writes=["PQ%d" % pg])
                    for k in range(8):
                        P.add("pe", lambda e, k=k, f=f, pg=pg, ss_=ss_, wb=wb: e.matmul(PQ[2 + pg][:, 0:SC], lhsT=wu[wb][:, k, f * 128:(f + 1) * 128], rhs=xeT[:, k, ss_],
                                                                                      start=(k == 0), stop=(k == 7)), reads=["wu%d" % wb, "xeT"], writes=["PQ%d" % (2 + pg)])
                    P.add("act", lambda e, pg=pg: e.activation(out=sgx[:], in_=PQ[pg][:, 0:SC], func=AF.Silu), reads=["PQ%d" % pg], writes=["sgx"])
                    P.add("dve", lambda e, pg=pg, f=f: e.tensor_tensor(out=hidT[:, f, :], in0=sgx[:], in1=PQ[2 + pg][:, 0:SC], op=ALU.mult),
                          reads=["sgx", "PQ%d" % (2 + pg)], writes=["hidT"])
                for sl in range(SC // 128):
                    s_ = sc_ * (SC // 128) + sl
                    yb_ = yi % 2
                    yi += 1
                    for nch in range(2):
                        pp = "pm%d" % nch
                        for f in range(NF):
                            P.add("pe", lambda e, f=f, sl=sl, nch=nch, wb=wb: e.matmul(pm[nch][:], lhsT=hidT[:, f, sl * 128:(sl + 1) * 128], rhs=wd[wb][:, f, nch * 512:(nch + 1) * 512],
                                                                                      start=(f == 0), stop=(f == NF - 1)), reads=["hidT", "wd%d" % wb], writes=[pp])
                        cs = slice(nch * 512, (nch + 1) * 512)
                        P.add("dve", lambda e, nch=nch, cs=cs, s_=s_, yb_=yb_: e.scalar_tensor_tensor(out=ysb[yb_][:, cs], in0=pm[nch][:], scalar=gateT[:, s_:s_ + 1], in1=M5[:, cs],
                                                                                                    op0=ALU.mult, op1=ALU.mult),
                              reads=[pp, "gateT", "M5"], writes=["ysb%d" % yb_])
                    P.add("pool", lambda e, s_=s_, yb_=yb_: e.indirect_dma_start(
                        out=X1[:, :], out_offset=bass.IndirectOffsetOnAxis(ap=tokI[:, s_:s_ + 1], axis=0),
                        in_=ysb[yb_][:], in_offset=None, compute_op=ALU.add), reads=["ysb%d" % yb_, "tokI", "DR_x1"], writes=["DR_x1"], dma=True)
        P.barrier()

    chk(7)
    with ExitStack() as ph:
        def sbp(name, shape, dt=F32):
            return ph.enter_context(nc.sbuf_tensor(name, list(shape), dt))
        fnw = sbp("fnw", [128, D])
        xf = [sbp("xf%d" % i, [128, D]) for i in range(2)]
        of_ = [sbp("of%d" % i, [128, D]) for i in range(2)]
        junk3 = sbp("junk3", [128, D], BF16)
        s5 = sbp("s5", [128, 4])
        P.add("sp", lambda e: e.dma_start(out=fnw[:], in_=fn_bc[:, :]), writes=["fnw"], dma=True)
        for lt in range(NTO):
            b = lt % 2
            P.add("sp", lambda e, lt=lt, b=b: e.dma_start(out=xf[b][:], in_=X1[lt * 128:(lt + 1) * 128, :]), reads=["DR_x1"], writes=["xf%d" % b], dma=True)
            P.add("act", lambda e, b=b: e.activation(out=junk3[:], in_=xf[b][:], func=AF.Square, accum_out=s5[:, 0:1]), reads=["xf%d" % b], writes=["junk3", "s5a"])
            P.add("act", lambda e: e.activation(out=s5[:, 1:2], in_=s5[:, 0:1], func=AF.Sqrt, scale=1.0 / D, bias=EPSC), reads=["s5a", "csb"], writes=["s5b"])
            P.add("dve", lambda e: e.reciprocal(out=s5[:, 2:3], in_=s5[:, 1:2]), reads=["s5b"], writes=["s5c"])
            P.add("dve", lambda e, b=b: e.scalar_tensor_tensor(out=of_[b][:], in0=xf[b][:], scalar=s5[:, 2:3], in1=fnw[:], op0=ALU.mult, op1=ALU.mult),
                  reads=["xf%d" % b, "s5c", "fnw"], writes=["of%d" % b])
            P.add("sp", lambda e, lt=lt, b=b: e.dma_start(out=out[lt * 128:(lt + 1) * 128, :], in_=of_[b][:]), reads=["of%d" % b], writes=["OUT"], dma=True)

    P.emit()
    es.close()
    return nc


def _capl(T):
    CAP = 2 * T // NE
    return max(128, ((3 * CAP // 8) + 127) // 128 * 128)


def _consts(T):
    c = np.zeros((128, 1024), np.float32)
    p = np.arange(128)
    c[:, 0:128] = np.eye(128)
    c[:, 128:256] = (p[:, None] <= p[None, :])
    c[:, 256:384] = (p[:, None] >= p[None, :])
    c[:, 384:512] = (p[:, None] < p[None, :])
    c[:, 512:640] = 1.0
    c[:, 640] = p
    c[:, 641] = p + 1
    c[:, 642] = 128 - p
    c[:, 643] = -(p + 1)
    c[:, 644] = -(128 - p)
    c[:, 645] = _capl(T) + p
    c[:, 646] = math.log(128.0 ** -0.5)
    c[:, 647] = EPS
    return c


def _rope_table(T):
    rows = T // 64
    row = np.repeat(np.arange(rows), 64).astype(np.float32)
    col = np.tile(np.arange(64), rows).astype(np.float32)
    n_freq = 32
    inv = (np.float32(10000.0) ** (-np.arange(n_freq, dtype=np.float32) / n_freq)).astype(np.float32)
    ang = np.concatenate([row[:, None] * inv, col[:, None] * inv], axis=-1).astype(np.float32)
    tab = np.zeros((CTX + T, 128), np.float32)
    tab[:CTX, 0:64] = 1.0
    tab[CTX:, 0:64] = np.cos(ang)
    tab[CTX:, 64:128] = np.sin(ang)
    return tab


def make_shared(inp, T):
    f = lambda a: np.ascontiguousarray(np.asarray(a, dtype=np.float32))
    bc = lambda v: np.ascontiguousarray(np.broadcast_to(np.asarray(v, np.float32).reshape(1, -1), (128, np.asarray(v).size)))
    w_out = f(inp["w_out"])[0]
    perm = np.concatenate([np.concatenate([np.arange(r * 128, (r + 1) * 128), 512 + np.arange(r * 128, (r + 1) * 128)]) for r in range(4)])
    return {
        "rope": _rope_table(T),
        "w_ada": f(inp["w_ada"])[0],
        "b_ada_bc": bc(f(inp["b_ada"])[0]),
        "n1_bc": bc(f(inp["norm1_w"])[0]),
        "n2_bc": bc(f(inp["norm2_w"])[0]),
        "fn_bc": bc(f(inp["final_norm_w"])),
        "w_out": np.ascontiguousarray(w_out[perm]),
        "w_router": f(inp["w_router"])[0],
        "w_eg": f(inp["w_exp_gate"])[0],
        "w_eu": f(inp["w_exp_up"])[0],
        "w_ed": f(inp["w_exp_down"])[0],
        "cst": _consts(T),
    }


def make_inputs(inp, core, T, shared):
    f = lambda a: np.ascontiguousarray(np.asarray(a, dtype=np.float32))
    bc = lambda v: np.ascontiguousarray(np.broadcast_to(np.asarray(v, np.float32).reshape(1, -1), (128, np.asarray(v).size)))
    b, q = core // 4, core % 4
    NT = T // 128
    NTO = NT // 4
    x = f(inp["x"])[b, :T]
    ctx = f(inp["ctx"])[b]
    c = f(inp["c"])[b]
    cc = f(inp["c_ctx"])
    w_in = f(inp["w_in"])[0]
    h64 = slice(q * 64, (q + 1) * 64)
    h128 = slice(q * 128, (q + 1) * 128)
    w_in_r = np.concatenate([w_in[:, 0:256][:, h64], w_in[:, 256:512][:, h64], w_in[:, 1568:2080][:, h128], w_in[:, 2080:2592][:, h128],
                             w_in[:, 512:1024][:, h128], w_in[:, 2592:3104][:, h128], w_in[:, 1056:1568][:, h128], w_in[:, 3104:3616][:, h128],
                             w_in[:, 1024:1056]], axis=1)
    gw = f(inp["gla_gate_w"])[0]
    gb = f(inp["gla_gate_b"])[0]
    gwaug = np.zeros((33, 128), np.float32)
    gwaug[0:16, 0:64] = gw[0][:, h64]
    gwaug[16:32, 64:128] = gw[1][:, h64]
    gwaug[32, 0:64] = gb[0][h64]
    gwaug[32, 64:128] = gb[1][h64]
    scT = np.zeros((128, 16), np.float32)
    scT[:, 0:8] = c.reshape(8, 128).T
    scT[:, 8:16] = cc.reshape(8, 128).T
    rdl = f(inp["ret_decay_logit"])[0]
    order = (np.arange(NT) + q * NTO) % NT
    xrot = np.ascontiguousarray(x.reshape(NT, 128, D)[order].reshape(T, D))
    yidx = np.zeros((128, NT * 4), np.int32)
    for r in range(4):
        yidx[:, r::4] = ((4 * b + r) * T + order[None, :] * 128 + np.arange(128)[:, None]).astype(np.int32)
    d = dict(shared)
    tag = lambda a: np.ascontiguousarray(np.concatenate([a, np.full((1,) + a.shape[1:], float(core), a.dtype)], axis=0))
    for k in ("rope", "w_ada", "b_ada_bc", "w_out"):
        d[k] = tag(shared[k])
    for k in ("w_eg", "w_eu", "w_ed"):
        d[k] = np.ascontiguousarray(np.roll(shared[k], -core, axis=0))
    d["w_router"] = np.ascontiguousarray(np.roll(shared["w_router"], -core, axis=1))
    d.update({
        "xin": np.ascontiguousarray(np.concatenate([ctx, x], axis=0)),
        "xrot": xrot,
        "scT": scT,
        "hn_bc": bc(np.concatenate([f(inp["gla_norm_w"])[0][h128], f(inp["ret_norm_w"])[0][h128]])),
        "w_in": np.ascontiguousarray(w_in_r),
        "gwaug": gwaug,
        "rdl_bc": bc(np.array([rdl[0, q], rdl[1, q]], np.float32)),
        "yidx": yidx,
    })
    return d


def kernel(**inputs):
    T = 16384
    B = 2
    nc = build_program(T)
    shared = make_shared(inputs, T)
    in_maps = [make_inputs(inputs, core, T, shared) for core in range(8)]
    res = run_bass_kernel_spmd(nc, in_maps, core_ids=list(range(8)))
    TQ = T // 4
    full = np.zeros((B, T, D), np.float32)
    for core in range(8):
        b, q = core // 4, core % 4
        full[b, q * TQ:(q + 1) * TQ] = np.asarray(res.results[core]["out"], dtype=np.float32).reshape(TQ, D)
    return full
```
